# Optimizing a Trainium2 kernel written in Bass

```python
import jax, jax.numpy as jnp
from jax import lax
import numpy as np

D_MODEL = 1024
BATCH = 16
SEQ = 4096
DEPTH = 1

CTX_LEN = 256
GRID_W = 64
EPS = 1e-6
N_MOD = 6
GDN_HEADS = 8
GDN_HEAD_DIM = 128
GDN_WIDTH = GDN_HEADS * GDN_HEAD_DIM
GDN_CHUNK = 64
CONV_SIZE = 3
GLA_HEADS = 4
GLA_KEY_DIM = D_MODEL // 2
GLA_VALUE_DIM = D_MODEL
GLA_DK = GLA_KEY_DIM // GLA_HEADS
GLA_DV = GLA_VALUE_DIM // GLA_HEADS
GLA_GATE_RANK = 16
GLA_GATE_NORMALIZER = 16.0
GLA_CHUNK = 64
N_EXPERTS = 32
TOP_K = 4
D_EXPERT = D_MODEL
SWIGLU_LIMIT = 7.0
SWIGLU_ALPHA = 1.702
MOE_BLOCK = 128
IN_SPLITS = (GDN_WIDTH, GDN_WIDTH, GDN_WIDTH, GDN_WIDTH, 2 * GDN_HEADS, 2 * GDN_HEADS,
             GLA_KEY_DIM, GLA_KEY_DIM, GLA_VALUE_DIM, GLA_VALUE_DIM, 2 * GLA_GATE_RANK, 2 * D_MODEL)
D_IN = 4 * GDN_WIDTH + 4 * GDN_HEADS + 2 * GLA_KEY_DIM + 2 * GLA_VALUE_DIM + 2 * GLA_GATE_RANK + 2 * D_MODEL

kernel_name = 'hybrid_gdn_gla_moe_prefix_dit'

F32 = jnp.float32


def rms_norm(x, g):
    xf = x.astype(F32)
    y = xf * lax.rsqrt(jnp.mean(xf * xf, axis=-1, keepdims=True) + EPS)
    return (y * g.astype(F32)).astype(x.dtype)


def l2_normalize(t):
    tf = t.astype(F32)
    return tf * lax.rsqrt(jnp.sum(tf * tf, axis=-1, keepdims=True) + EPS)


def modulate(h, shift, scale):
    return h * (1.0 + scale) + shift


def split_in(p):
    idx = np.cumsum(IN_SPLITS)[:-1].tolist()
    return jnp.split(p, idx, axis=-1)


def to_heads(t, n_heads):
    b, l, _ = t.shape
    return t.reshape(b, l, n_heads, -1).transpose(0, 2, 1, 3)


def from_heads(t):
    return t.transpose(0, 2, 1, 3)


def depthwise_conv2d(img, w):
    ch = img.shape[-1]
    return lax.conv_general_dilated(img, w[:, :, None, :].astype(img.dtype), window_strides=(1, 1), padding='SAME',
                                    dimension_numbers=('NHWC', 'HWIO', 'NHWC'), feature_group_count=ch)


def conv_latent(t, conv_w):
    b, l, ch = t.shape
    rows = l // GRID_W
    return depthwise_conv2d(t.reshape(b, rows, GRID_W, ch), conv_w).reshape(b, l, ch)


def conv_context(t, conv_w):
    mid = CONV_SIZE // 2
    return depthwise_conv2d(t[:, None], conv_w[mid:mid + 1])[:, 0]


def gated_delta_chunked(q, k, v, g, beta, s0):
    b, h, l, dk = q.shape
    dv = v.shape[-1]
    c = GDN_CHUNK
    n = l // c
    q = q.astype(F32).reshape(b, h, n, c, dk) * (dk ** -0.5)
    k = k.astype(F32).reshape(b, h, n, c, dk)
    v = v.astype(F32).reshape(b, h, n, c, dv)
    beta = beta.astype(F32).reshape(b, h, n, c)
    g = jnp.cumsum(g.astype(F32).reshape(b, h, n, c), axis=-1)
    incl = jnp.tril(jnp.ones((c, c), bool))
    strict = jnp.tril(jnp.ones((c, c), bool), -1)
    decay = jnp.exp(jnp.where(incl, g[..., :, None] - g[..., None, :], -jnp.inf))
    kb = k * beta[..., None]
    m = jnp.where(strict, jnp.einsum('bhnid,bhnjd->bhnij', kb, k) * decay, 0.0)
    a = m + jnp.eye(c, dtype=F32)
    rhs = jnp.concatenate([v * beta[..., None], kb * jnp.exp(g)[..., None]], axis=-1)
    sol = lax.linalg.triangular_solve(a, rhs, left_side=True, lower=True, unit_diagonal=True)
    u, w = sol[..., :dv], sol[..., dv:]
    attn = jnp.einsum('bhnid,bhnjd->bhnij', q, k) * decay
    qg = q * jnp.exp(g)[..., None]
    kd = k * jnp.exp(g[..., -1:] - g)[..., None]
    gl = jnp.exp(g[..., -1])
    xs = tuple(jnp.moveaxis(t, 2, 0) for t in (u, w, attn, qg, kd, gl))

    def step(s, xs_n):
        u_n, w_n, attn_n, qg_n, kd_n, gl_n = xs_n
        v_new = u_n - jnp.einsum('bhck,bhkv->bhcv', w_n, s)
        o_n = jnp.einsum('bhck,bhkv->bhcv', qg_n, s) + jnp.einsum('bhij,bhjv->bhiv', attn_n, v_new)
        s = s * gl_n[..., None, None] + jnp.einsum('bhck,bhcv->bhkv', kd_n, v_new)
        return s, o_n

    s, o = lax.scan(step, s0.astype(F32), xs)
    return jnp.moveaxis(o, 0, 2).reshape(b, h, l, dv), s


def gla_chunked(q, k, v, log_a, s0):
    b, h, l, dk = q.shape
    dv = v.shape[-1]
    c = GLA_CHUNK
    n = l // c
    q = q.astype(F32).reshape(b, h, n, c, dk) * (dk ** -0.5)
    k = k.astype(F32).reshape(b, h, n, c, dk)
    v = v.astype(F32).reshape(b, h, n, c, dv)
    gcum = jnp.cumsum(log_a.astype(F32).reshape(b, h, n, c, dk), axis=-2)
    ref = gcum[..., c // 2:c // 2 + 1, :]
    incl = jnp.tril(jnp.ones((c, c), bool))
    attn = jnp.einsum('bhnik,bhnjk->bhnij', q * jnp.exp(gcum - ref), k * jnp.exp(ref - gcum))
    attn = jnp.where(incl, attn, 0.0)
    intra = jnp.einsum('bhnij,bhnjv->bhniv', attn, v)
    qg = q * jnp.exp(gcum)
    kd = k * jnp.exp(gcum[..., -1:, :] - gcum)
    gl = jnp.exp(gcum[..., -1, :])
    xs = tuple(jnp.moveaxis(t, 2, 0) for t in (intra, qg, kd, v, gl))

    def step(s, xs_n):
        intra_n, qg_n, kd_n, v_n, gl_n = xs_n
        o_n = intra_n + jnp.einsum('bhck,bhkv->bhcv', qg_n, s)
        s = s * gl_n[..., :, None] + jnp.einsum('bhck,bhcv->bhkv', kd_n, v_n)
        return s, o_n

    s, o = lax.scan(step, s0.astype(F32), xs)
    return jnp.moveaxis(o, 0, 2).reshape(b, h, l, dv), s


def flip_seq(ts, flip):
    return tuple(jnp.flip(t, axis=2) for t in ts) if flip else tuple(ts)


def bidirectional_prefix_scan(scan_fn, ctx_shared, lat_shared, ctx_gates, lat_gates, s0, need_ctx):
    lat_outs, ctx_outs = [], []
    for direction, flip in enumerate((False, True)):
        o_c, s_c = scan_fn(*flip_seq(ctx_shared, flip), *flip_seq(ctx_gates[direction], flip), s0)
        o_l, _ = scan_fn(*flip_seq(lat_shared, flip), *flip_seq(lat_gates[direction], flip), s_c)
        lat_outs.append(flip_seq((o_l,), flip)[0])
        if need_ctx:
            ctx_outs.append(flip_seq((o_c,), flip)[0])
    o_lat = lat_outs[0] + lat_outs[1]
    o_ctx = ctx_outs[0] + ctx_outs[1] if need_ctx else None
    return o_lat, o_ctx


def gdn_inputs(parts, qkv, a_log_f, a_log_b, dt_bias_f, dt_bias_b):
    q, k, v = jnp.split(qkv, 3, axis=-1)
    q = l2_normalize(to_heads(q, GDN_HEADS))
    k = l2_normalize(to_heads(k, GDN_HEADS))
    v = to_heads(v, GDN_HEADS)
    beta = jax.nn.sigmoid(parts[4].astype(F32)).transpose(0, 2, 1)
    a = parts[5].astype(F32).transpose(0, 2, 1)
    hh = GDN_HEADS
    g_f = -jnp.exp(a_log_f.astype(F32))[:, None] * jax.nn.softplus(a[:, :hh] + dt_bias_f.astype(F32)[:, None])
    g_b = -jnp.exp(a_log_b.astype(F32))[:, None] * jax.nn.softplus(a[:, hh:] + dt_bias_b.astype(F32)[:, None])
    return (q, k, v), ((g_f, beta[:, :hh]), (g_b, beta[:, hh:]))


def gla_log_gate(z, w, bias):
    return to_heads(jax.nn.log_sigmoid((z @ w + bias).astype(F32)) / GLA_GATE_NORMALIZER, GLA_HEADS)


def gla_inputs(parts, w_f, w_b, b_f, b_b):
    q = to_heads(parts[6], GLA_HEADS)
    k = to_heads(parts[7], GLA_HEADS)
    v = to_heads(parts[8], GLA_HEADS)
    lr = parts[10]
    g_f = gla_log_gate(lr[..., :GLA_GATE_RANK], w_f, b_f)
    g_b = gla_log_gate(lr[..., GLA_GATE_RANK:], w_b, b_b)
    return (q, k, v), ((g_f,), (g_b,))


def merge_branches(parts, o_a, o_b, gdn_norm_g, gla_norm_g, w_out_a, w_out_b, w_out):
    dtype = parts[0].dtype
    bsz, l = parts[0].shape[:2]
    za = parts[3].reshape(bsz, l, GDN_HEADS, GDN_HEAD_DIM).astype(F32)
    ya = (rms_norm(from_heads(o_a), gdn_norm_g) * jax.nn.silu(za)).astype(dtype).reshape(bsz, l, GDN_WIDTH) @ w_out_a
    rb = parts[9].reshape(bsz, l, GLA_HEADS, GLA_DV).astype(F32)
    yb = (rms_norm(from_heads(o_b), gla_norm_g) * jax.nn.silu(rb)).astype(dtype).reshape(bsz, l, GLA_VALUE_DIM) @ w_out_b
    gates = jax.nn.sigmoid(parts[11])
    y = gates[..., :D_MODEL] * ya + gates[..., D_MODEL:] * yb
    return y @ w_out


def token_mixer(h_lat, h_ctx, need_ctx, w_in, conv_w, a_log_f, a_log_b, dt_bias_f, dt_bias_b, gdn_norm_g,
                gla_gate_w_f, gla_gate_w_b, gla_gate_b_f, gla_gate_b_b, gla_norm_g, w_out_a, w_out_b, w_out):
    p_lat = h_lat @ w_in
    p_ctx = h_ctx @ w_in
    parts_lat, parts_ctx = split_in(p_lat), split_in(p_ctx)
    bsz = h_lat.shape[0]
    qkv_lat = jax.nn.silu(conv_latent(p_lat[..., :3 * GDN_WIDTH], conv_w))
    qkv_ctx = jax.nn.silu(conv_context(p_ctx[..., :3 * GDN_WIDTH], conv_w))
    a_lat, a_lat_g = gdn_inputs(parts_lat, qkv_lat, a_log_f, a_log_b, dt_bias_f, dt_bias_b)
    a_ctx, a_ctx_g = gdn_inputs(parts_ctx, qkv_ctx, a_log_f, a_log_b, dt_bias_f, dt_bias_b)
    s0_a = jnp.zeros((bsz, GDN_HEADS, GDN_HEAD_DIM, GDN_HEAD_DIM), F32)
    o_a_lat, o_a_ctx = bidirectional_prefix_scan(gated_delta_chunked, a_ctx, a_lat, a_ctx_g, a_lat_g, s0_a, need_ctx)
    b_lat, b_lat_g = gla_inputs(parts_lat, gla_gate_w_f, gla_gate_w_b, gla_gate_b_f, gla_gate_b_b)
    b_ctx, b_ctx_g = gla_inputs(parts_ctx, gla_gate_w_f, gla_gate_w_b, gla_gate_b_f, gla_gate_b_b)
    s0_b = jnp.zeros((bsz, GLA_HEADS, GLA_DK, GLA_DV), F32)
    o_b_lat, o_b_ctx = bidirectional_prefix_scan(gla_chunked, b_ctx, b_lat, b_ctx_g, b_lat_g, s0_b, need_ctx)
    merge_w = (gdn_norm_g, gla_norm_g, w_out_a, w_out_b, w_out)
    y_lat = merge_branches(parts_lat, o_a_lat, o_b_lat, *merge_w)
    y_ctx = merge_branches(parts_ctx, o_a_ctx, o_b_ctx, *merge_w) if need_ctx else None
    return y_lat, y_ctx


def moe_ffn(h, w_router, b_router, w_gu, b_gu, w_down, b_down):
    t, d = h.shape
    n_assign = t * TOP_K
    n_blocks = -(-(n_assign + N_EXPERTS * (MOE_BLOCK - 1)) // MOE_BLOCK)
    logits = (h @ w_router + b_router).astype(F32)
    top_logit, top_idx = lax.top_k(logits, TOP_K)
    top_p = jax.nn.softmax(top_logit, axis=-1)
    flat_e = top_idx.reshape(-1).astype(jnp.int32)
    order = jnp.argsort(flat_e)
    sorted_e = flat_e[order]
    sorted_tok = (order // TOP_K).astype(jnp.int32)
    sorted_p = top_p.reshape(-1)[order]
    counts = jnp.bincount(flat_e, length=N_EXPERTS).astype(jnp.int32)
    start = jnp.cumsum(counts) - counts
    padded = (counts + MOE_BLOCK - 1) // MOE_BLOCK * MOE_BLOCK
    pad_end = jnp.cumsum(padded)
    pad_start = pad_end - padded
    dest = pad_start[sorted_e] + jnp.arange(n_assign, dtype=jnp.int32) - start[sorted_e]
    slot_tok = jnp.full((n_blocks * MOE_BLOCK,), t, jnp.int32).at[dest].set(sorted_tok)
    slot_p = jnp.zeros((n_blocks * MOE_BLOCK,), F32).at[dest].set(sorted_p)
    block_start = jnp.arange(n_blocks, dtype=jnp.int32) * MOE_BLOCK
    block_expert = jnp.minimum(jnp.searchsorted(pad_end, block_start, side='right'), N_EXPERTS - 1)
    h_pad = jnp.concatenate([h, jnp.zeros((1, d), h.dtype)], axis=0)

    def expert_block(args):
        tok, p, e = args
        xb = h_pad[tok]
        gu = xb @ w_gu[e] + b_gu[e]
        glu = jnp.minimum(gu[:, :D_EXPERT], SWIGLU_LIMIT)
        lin = jnp.clip(gu[:, D_EXPERT:], -SWIGLU_LIMIT, SWIGLU_LIMIT)
        act = glu * jax.nn.sigmoid(SWIGLU_ALPHA * glu) * (lin + 1.0)
        return (act @ w_down[e] + b_down[e]) * p[:, None].astype(h.dtype)

    yb = lax.map(expert_block, (slot_tok.reshape(n_blocks, MOE_BLOCK), slot_p.reshape(n_blocks, MOE_BLOCK), block_expert))
    y = jax.ops.segment_sum(yb.reshape(-1, d), slot_tok, num_segments=t + 1)
    return y[:t].astype(h.dtype)


def setup_inputs(seed: int = 0) -> dict:
    key = jax.random.key(seed)
    ks = iter(jax.random.split(key, 40))
    nrm = lambda shape, s: jax.random.normal(next(ks), shape, F32) * s
    gain = lambda shape: 1.0 + nrm(shape, 0.02)
    dd = D_MODEL
    u_dt = jax.random.uniform(next(ks), (2, DEPTH, GDN_HEADS), F32)
    dt = jnp.exp(u_dt * (jnp.log(0.1) - jnp.log(1e-3)) + jnp.log(1e-3))
    dt_bias = dt + jnp.log(-jnp.expm1(-dt))
    a_log = jnp.log(jax.random.uniform(next(ks), (2, DEPTH, GDN_HEADS), F32, 1.0, 16.0))
    return {
        'x': nrm((BATCH, SEQ, dd), 1.0),
        'c': nrm((BATCH, dd), 1.0),
        'ctx': nrm((BATCH, CTX_LEN, dd), 1.0),
        'c_ctx': nrm((dd,), 1.0),
        'w_mod': nrm((DEPTH, dd, N_MOD * dd), 0.5 * dd ** -0.5),
        'b_mod': nrm((DEPTH, N_MOD * dd), 0.02),
        'norm_mix_g': gain((DEPTH, dd)),
        'norm_ffn_g': gain((DEPTH, dd)),
        'w_in': nrm((DEPTH, dd, D_IN), dd ** -0.5),
        'conv_w': nrm((DEPTH, CONV_SIZE, CONV_SIZE, 3 * GDN_WIDTH), 1.0 / CONV_SIZE),
        'a_log_f': a_log[0],
        'a_log_b': a_log[1],
        'dt_bias_f': dt_bias[0],
        'dt_bias_b': dt_bias[1],
        'gdn_norm_g': gain((DEPTH, GDN_HEAD_DIM)),
        'gla_gate_w_f': nrm((DEPTH, GLA_GATE_RANK, GLA_KEY_DIM), GLA_GATE_RANK ** -0.5),
        'gla_gate_w_b': nrm((DEPTH, GLA_GATE_RANK, GLA_KEY_DIM), GLA_GATE_RANK ** -0.5),
        'gla_gate_b_f': nrm((DEPTH, GLA_KEY_DIM), 0.1),
        'gla_gate_b_b': nrm((DEPTH, GLA_KEY_DIM), 0.1),
        'gla_norm_g': gain((DEPTH, GLA_DV)),
        'w_out_a': nrm((DEPTH, GDN_WIDTH, dd), GDN_WIDTH ** -0.5),
        'w_out_b': nrm((DEPTH, GLA_VALUE_DIM, dd), GLA_VALUE_DIM ** -0.5),
        'w_out': nrm((DEPTH, dd, dd), dd ** -0.5),
        'w_router': nrm((DEPTH, dd, N_EXPERTS), dd ** -0.5),
        'b_router': nrm((DEPTH, N_EXPERTS), 0.01),
        'w_gu': nrm((DEPTH, N_EXPERTS, dd, 2 * D_EXPERT), dd ** -0.5),
        'b_gu': nrm((DEPTH, N_EXPERTS, 2 * D_EXPERT), 0.01),
        'w_down': nrm((DEPTH, N_EXPERTS, D_EXPERT, dd), D_EXPERT ** -0.5),
        'b_down': nrm((DEPTH, N_EXPERTS, dd), 0.01),
        'final_norm_g': gain((dd,)),
    }


def reference(x, c, ctx, c_ctx, w_mod, b_mod, norm_mix_g, norm_ffn_g, w_in, conv_w, a_log_f, a_log_b, dt_bias_f,
              dt_bias_b, gdn_norm_g, gla_gate_w_f, gla_gate_w_b, gla_gate_b_f, gla_gate_b_b, gla_norm_g, w_out_a,
              w_out_b, w_out, w_router, b_router, w_gu, b_gu, w_down, b_down, final_norm_g):
    for layer in range(DEPTH):
        need_ctx = layer < DEPTH - 1
        mod_lat = [m[:, None, :] for m in jnp.split(jax.nn.silu(c) @ w_mod[layer] + b_mod[layer], N_MOD, axis=-1)]
        mod_ctx = jnp.split(jax.nn.silu(c_ctx) @ w_mod[layer] + b_mod[layer], N_MOD, axis=-1)
        sh1, sc1, g1, sh2, sc2, g2 = mod_lat
        csh1, csc1, cg1, csh2, csc2, cg2 = mod_ctx
        h_lat = modulate(rms_norm(x, norm_mix_g[layer]), sh1, sc1)
        h_ctx = modulate(rms_norm(ctx, norm_mix_g[layer]), csh1, csc1)
        y_lat, y_ctx = token_mixer(h_lat, h_ctx, need_ctx, w_in[layer], conv_w[layer], a_log_f[layer], a_log_b[layer],
                                   dt_bias_f[layer], dt_bias_b[layer], gdn_norm_g[layer], gla_gate_w_f[layer],
                                   gla_gate_w_b[layer], gla_gate_b_f[layer], gla_gate_b_b[layer], gla_norm_g[layer],
                                   w_out_a[layer], w_out_b[layer], w_out[layer])
        x = x + g1 * y_lat
        h = modulate(rms_norm(x, norm_ffn_g[layer]), sh2, sc2).reshape(-1, D_MODEL)
        moe_w = (w_router[layer], b_router[layer], w_gu[layer], b_gu[layer], w_down[layer], b_down[layer])
        if need_ctx:
            ctx = ctx + cg1 * y_ctx
            hc = modulate(rms_norm(ctx, norm_ffn_g[layer]), csh2, csc2).reshape(-1, D_MODEL)
            n_lat = h.shape[0]
            y = moe_ffn(jnp.concatenate([h, hc], axis=0), *moe_w)
            x = x + g2 * y[:n_lat].reshape(x.shape)
            ctx = ctx + cg2 * y[n_lat:].reshape(ctx.shape)
        else:
            x = x + g2 * moe_ffn(h, *moe_w).reshape(x.shape)
    return rms_norm(x, final_norm_g)
```

```python
import numpy as np
from contextlib import ExitStack
import concourse.bass as bass
import concourse.mybir as mybir
from concourse.bass_utils import run_bass_kernel_spmd

F32 = mybir.dt.float32
BF16 = mybir.dt.bfloat16
I32 = mybir.dt.int32
AF = mybir.ActivationFunctionType
ALU = mybir.AluOpType
AX = mybir.AxisListType

D = 1024
KC = 8
EPS = 1e-6
NH_A = 8
NH_B = 4
N_EXP = 32
TOPK = 4
LIMIT = 7.0
ALPHA = 1.702
SEM_LIMIT = 30000
OFF_QA, OFF_KA, OFF_VA, OFF_ZA = 0, 1024, 2048, 3072
OFF_BETA, OFF_DEC = 4096, 4112
OFF_QB, OFF_KB, OFF_VB, OFF_RB = 4128, 4640, 5152, 6176
OFF_LR, OFF_GATE = 7200, 7232
D_IN = 9280


class Res:
    __slots__ = ("name", "w", "r")

    def __init__(self, name):
        self.name = name
        self.w = None
        self.r = {}


class Eng:
    def __init__(self, kb, name, obj):
        self.kb, self.name, self.obj = kb, name, obj
        self.sem = None
        self.cnt = 0
        self.seen = {}
        self.nsem = 0

    def new_sem(self):
        self.sem = self.kb.es.enter_context(self.kb.nc.semaphore("e_%s_%d" % (self.name, self.nsem)))
        self.nsem += 1
        self.cnt = 0


class DSem:
    def __init__(self, sem):
        self.sem = sem
        self.total = 0


class KB:
    def __init__(self, nc, es, n_dsem=40):
        self.nc, self.es = nc, es
        self.eng = {
            "pe": Eng(self, "pe", nc.tensor),
            "act": Eng(self, "act", nc.scalar),
            "dve": Eng(self, "dve", nc.vector),
            "pool": Eng(self, "pool", nc.gpsimd),
            "sp": Eng(self, "sp", nc.sync),
        }
        for e in self.eng.values():
            e.new_sem()
        self.dsems_q = {"sp": [DSem(es.enter_context(nc.semaphore("dsp%d" % i))) for i in range(24)],
                        "pool": [DSem(es.enter_context(nc.semaphore("dpl%d" % i))) for i in range(24)]}
        self.dsems = self.dsems_q["sp"] + self.dsems_q["pool"]
        self.di = 0
        self.dq = {"sp": 0, "pool": 0}
        self.nres = 0
        self.out_tokens = []

    def res(self, name=None):
        self.nres += 1
        return Res(name or "r%d" % self.nres)

    def _waits(self, e, reads, writes, is_dma):
        deps = []
        for r in reads:
            if r.w is not None:
                deps.append((r.w, True))
        for r in writes:
            if r.w is not None:
                deps.append((r.w, False))
            for x in r.r.values():
                deps.append((x, False))
        for (sem, val, src), raw in deps:
            if (src is e) and not is_dma:
                if e.name == "pe":
                    continue
                assert val <= e.cnt or sem is not e.sem, "self-wait on pending token"
            if e.seen.get(id(sem), 0) >= val:
                continue
            e.obj.wait_ge(sem, val)
            e.seen[id(sem)] = val

    def op(self, en, fn, reads=(), writes=(), inc=True):
        e = self.eng[en]
        if e.cnt >= SEM_LIMIT:
            e.new_sem()
        self._waits(e, reads, writes, False)
        ins = fn(e.obj)
        if inc:
            e.cnt += 1
            ins.then_inc(e.sem, 1)
            tok = (e.sem, e.cnt, e)
        else:
            tok = (e.sem, e.cnt + 1, e)
        for r in writes:
            r.w = tok
            r.r = {}
        for r in reads:
            r.r[en] = tok
        return tok

    def dma(self, qn, fn, reads=(), writes=()):
        q = self.eng[qn]
        self._waits(q, reads, writes, True)
        pool_ = self.dsems_q[qn]
        s = pool_[self.dq[qn] % len(pool_)]
        self.dq[qn] += 1
        self.di += 1
        if s.total > 0 and q.seen.get(id(s.sem), 0) < s.total:
            q.obj.wait_ge(s.sem, s.total)
            q.seen[id(s.sem)] = s.total
        ins = fn(q.obj)
        s.total += 16
        assert s.total < 60000
        ins.then_inc(s.sem, 16)
        tok = (s.sem, s.total, None)
        for r in writes:
            r.w = tok
            r.r = {}
        for r in reads:
            r.r["dma%d" % (self.di % 64)] = tok
        return tok

    def barrier(self):
        toks = [(e.sem, e.cnt, e) for e in self.eng.values() if e.cnt > 0]
        toks += [(ds.sem, ds.total, None) for ds in self.dsems if ds.total > 0]
        for e in self.eng.values():
            for tok in toks:
                if tok[2] is not e:
                    self.wait_tok(e.name, tok)

    def wait_tok(self, en, tok):
        e = self.eng[en]
        sem, val, _ = tok
        if e.seen.get(id(sem), 0) < val:
            e.obj.wait_ge(sem, val)
            e.seen[id(sem)] = val


class Buf:
    def __init__(self, kb, t, name, single=False):
        self.kb, self.t, self.name = kb, t, name
        self._r = {}
        self.single = single

    def r(self, key=0):
        if self.single:
            key = 0
        if key not in self._r:
            self._r[key] = self.kb.res("%s/%s" % (self.name, key))
        return self._r[key]

    def __getitem__(self, idx):
        return self.t[idx]


def make_consts():
    c = {}
    idx = np.arange(128)
    same = (idx[:, None] // 64) == (idx[None, :] // 64)
    c["ident"] = np.eye(128, dtype=np.float32)
    c["tri_f"] = (same & (idx[:, None] <= idx[None, :])).astype(np.float32)
    c["tri_b"] = (same & (idx[:, None] >= idx[None, :])).astype(np.float32)
    c["blk"] = same.astype(np.float32)
    c["neg_f"] = np.where(c["tri_f"] > 0, 0.0, -30000.0).astype(np.float32)
    c["neg_b"] = np.where(c["tri_b"] > 0, 0.0, -30000.0).astype(np.float32)
    c["offd"] = (1.0 - np.eye(128)).astype(np.float32)
    c["ones"] = np.ones((128, 128), np.float32)
    sel = np.zeros((16, 16 * 128), np.float32)
    for r in range(16):
        sel[r, r * 128:(r + 1) * 128] = 1.0
    c["sel16"] = sel
    return c


CONST_ORDER = ["ident", "tri_f", "tri_b", "blk", "neg_f", "neg_b", "offd", "ones"]


def build(cfg):
    NB, SEQ, CTX, GW = cfg["NB"], cfg["SEQ"], cfg["CTX"], cfg["GW"]
    S = CTX + SEQ
    NT, NTC, NTL = S // 128, CTX // 128, SEQ // 128
    ROWS = SEQ // GW
    dbg = cfg.get("dbg", ())
    stop = cfg.get("stop", 99)
    nc = bass.Bass("TRN2", target_bir_lowering=False)

    def din(name, shape, dt=F32):
        return nc.dram_tensor(name, list(shape), dt, kind="ExternalInput").ap()

    x_d = din("x", [NB, SEQ, D])
    c_d = din("c", [NB, D])
    ctx_d = din("ctx", [NB, CTX, D])
    cctx_d = din("c_ctx", [1, D])
    wmod_d = din("w_mod", [D, 6 * D])
    bmod_d = din("b_mod", [1, 6 * D])
    gmix_d = din("norm_mix_g", [1, D])
    gffn_d = din("norm_ffn_g", [1, D])
    win_d = din("w_in", [D, D_IN])
    convw_d = din("conv_w", [9, 3 * D])
    alog_d = din("a_log", [1, 16])
    dtb_d = din("dt_bias", [1, 16])
    gdng_d = din("gdn_norm_g", [1, 128])
    glaw_d = din("gla_gate_w", [2, 16, 512])
    glab_d = din("gla_gate_b", [2, 512])
    glag_d = din("gla_norm_g", [1, 256])
    woa_d = din("w_out_a", [D, D])
    wob_d = din("w_out_b", [D, D])
    wo_d = din("w_out", [D, D])
    wr_d = din("w_router", [D, N_EXP])
    br_d = din("b_router", [1, N_EXP])
    wgu_d = din("w_gu", [N_EXP * D, 2 * D])
    bgu_d = din("b_gu", [N_EXP, 2 * D])
    wdn_d = din("w_down", [N_EXP * D, D])
    bdn_d = din("b_down", [N_EXP, D])
    fng_d = din("final_norm_g", [1, D])
    cst_d = din("consts", [128, len(CONST_ORDER) * 128])
    sel_d = din("sel16", [16, 16 * 128])
    _T = NB * SEQ
    _NBLK = -(-(_T * TOPK + N_EXP * 511) // 512)
    moec_d = din("moe_c", [128, 128 + 8 + _NBLK])
    tokid_d = din("tokid", [128, _T // 128], I32)
    out_d = nc.dram_tensor("out", [NB, SEQ, D], F32, kind="ExternalOutput").ap()
    dbg_d = {}
    for name, shape in cfg.get("dbg_shapes", {}).items():
        dbg_d[name] = nc.dram_tensor("dbg_" + name, list(shape), F32, kind="ExternalOutput").ap()

    es = ExitStack()
    kb = KB(nc, es)

    stk = [es]
    uniq = [0]

    def sb(name, shape, dt):
        uniq[0] += 1
        return Buf(kb, stk[-1].enter_context(nc.sbuf_tensor("s%d_%s" % (uniq[0], name), list(shape), dt)), name)

    class scope:
        def __enter__(self):
            self.s = ExitStack()
            stk.append(self.s)
            return self

        def __exit__(self, *a):
            kb.barrier()
            stk.pop()
            self.s.close()
            return False

    PS = [Buf(kb, es.enter_context(nc.psum_tensor("ps%d" % i, [128, 512], F32)), "ps%d" % i, single=True) for i in range(8)]

    cst = sb("cst", [128, len(CONST_ORDER) * 128], F32)
    cstb = sb("cstb", [128, len(CONST_ORDER) * 128], BF16)
    sel16 = sb("sel16", [16, 16 * 128], BF16)
    kb.dma("sp", lambda q: q.dma_start(out=cst[:, :], in_=cst_d), writes=[cst.r()])
    kb.dma("pool", lambda q: q.dma_start(out=cstb[:, :], in_=cst_d), writes=[cstb.r()])
    kb.dma("pool", lambda q: q.dma_start(out=sel16[:, :], in_=sel_d), writes=[sel16.r()])

    def C(name, bf=False, rows=slice(0, 128), cols=None):
        i = CONST_ORDER.index(name)
        t = cstb if bf else cst
        if cols is None:
            return t[rows, i * 128:(i + 1) * 128]
        return t[rows, i * 128 + cols.start:i * 128 + cols.stop]

    CR = [cst.r()]
    CRB = [cstb.r()]

    def mm(out, lhsT, rhs, start, stop_, reads, writes, inc=None):
        if inc is None:
            inc = stop_
        return kb.op("pe", lambda e: e.matmul(out, lhsT=lhsT, rhs=rhs, start=start, stop=stop_),
                     reads=reads, writes=writes, inc=inc)

    def row_to_cols(row_ap_fn, ncols, ps, dst_ap, reads, writes):
        for j in range(ncols):
            mm(ps[:, j:j + 1], row_ap_fn(j), C("ones", rows=slice(0, 1), cols=slice(0, 1)), True, True,
               reads + CR, [ps.r()])
        kb.op("dve", lambda e: e.tensor_copy(out=dst_ap, in_=ps[:, 0:ncols]), reads=[ps.r()], writes=writes)

    def row_bcast(row_ap, n, ps, dst_ap, reads, writes, eng="dve"):
        mm(ps[:, 0:n], C("ones", rows=slice(0, 1)), row_ap, True, True, reads + CR, [ps.r()])
        if eng == "dve":
            kb.op("dve", lambda e: e.tensor_copy(out=dst_ap, in_=ps[:, 0:n]), reads=[ps.r()], writes=writes)
        else:
            kb.op("act", lambda e: e.activation(out=dst_ap, in_=ps[:, 0:n], func=AF.Copy), reads=[ps.r()], writes=writes)

    bis = cfg.get("bis", 0)

    def early():
        for ds in kb.dsems:
            if ds.total > 0:
                kb.wait_tok("sp", (ds.sem, ds.total, None))
        while len(stk) > 1:
            stk.pop().close()
        es.close()
        return nc

    if bis == 1:
        return early()
    NSRC = NB + 1
    modcol = sb("modcol", [128, NSRC * 2 * KC], F32)
    gsc1 = sb("gsc1", [128, NSRC * KC], F32)
    fng_bc = sb("fng_bc", [128, D], F32)
    modbc_d = nc.dram_tensor("modbc_s", [NB, 128, 4 * D], F32, kind="Internal").ap()
    sc0 = scope()
    sc0.__enter__()
    rows = sb("rows", [1, 3 * 1024], F32)
    crow = sb("crow", [1, NSRC * D], F32)
    for b in range(NB):
        kb.dma("sp", lambda q, b=b: q.dma_start(out=crow[0:1, b * D:(b + 1) * D], in_=c_d[b:b + 1, :]), writes=[crow.r(b)])
    kb.dma("sp", lambda q: q.dma_start(out=crow[0:1, NB * D:(NB + 1) * D], in_=cctx_d), writes=[crow.r(NB)])
    ccol = sb("ccol", [128, NSRC * KC], F32)
    scol = sb("scol", [128, NSRC * KC], BF16)
    for s_ in range(NSRC):
        row_to_cols(lambda j, s_=s_: crow[0:1, s_ * D + j * 128:s_ * D + (j + 1) * 128], KC, PS[0],
                    ccol[:, s_ * KC:(s_ + 1) * KC], [crow.r(s_)], [ccol.r(s_)])
        kb.op("act", lambda e, s_=s_: e.activation(out=scol[:, s_ * KC:(s_ + 1) * KC], in_=ccol[:, s_ * KC:(s_ + 1) * KC], func=AF.Silu),
              reads=[ccol.r(s_)], writes=[scol.r(s_)])
    if bis == 2:
        return early()
    bmod = sb("bmod", [1, 6 * D], F32)
    kb.dma("sp", lambda q: q.dma_start(out=bmod[0:1, :], in_=bmod_d), writes=[bmod.r()])
    kb.dma("sp", lambda q: q.dma_start(out=rows[0:1, 0:D], in_=gmix_d), writes=[rows.r(0)])
    kb.dma("sp", lambda q: q.dma_start(out=rows[0:1, D:2 * D], in_=gffn_d), writes=[rows.r(1)])
    kb.dma("sp", lambda q: q.dma_start(out=rows[0:1, 2 * D:3 * D], in_=fng_d), writes=[rows.r(2)])
    gmixc = sb("gmixc", [128, KC], F32)
    row_to_cols(lambda j: rows[0:1, j * 128:(j + 1) * 128], KC, PS[0], gmixc[:, :], [rows.r(0)], [gmixc.r()])
    gffn_bc = sb("gffn_bc", [128, D], F32)
    for h in range(2):
        row_bcast(rows[0:1, D + h * 512:D + (h + 1) * 512], 512, PS[1], gffn_bc[:, h * 512:(h + 1) * 512], [rows.r(1)], [gffn_bc.r(h)])
        row_bcast(rows[0:1, 2 * D + h * 512:2 * D + (h + 1) * 512], 512, PS[1], fng_bc[:, h * 512:(h + 1) * 512], [rows.r(2)], [fng_bc.r(h)])

    modbc = [sb("modbc%d" % b, [128, 4 * D], F32) for b in range(NB)]
    wm = [sb("wm%d" % i, [128, KC, 512], BF16) for i in range(2)]
    mrow = [sb("mrow%d" % i, [1, 512], F32) for i in range(2)]
    wmod_v = wmod_d.rearrange("(kc p) n -> p kc n", p=128)
    for cb in range(12):
        w_ = wm[cb % 2]
        kb.dma("pool", lambda q, cb=cb, w_=w_: q.dma_start(out=w_[:, :, :], in_=wmod_v[:, :, cb * 512:(cb + 1) * 512]), writes=[w_.r()])
        v, half = cb // 2, cb % 2
        for s_ in range(NSRC):
            if v >= 2 and s_ == NB:
                continue
            ps = PS[2 + (s_ % 2)]
            for kc in range(KC):
                mm(ps[0:1, :], scol[:, s_ * KC + kc:s_ * KC + kc + 1], w_[:, kc, :], kc == 0, kc == KC - 1,
                   [scol.r(s_), w_.r()], [ps.r()])
            mr = mrow[s_ % 2]
            kb.op("dve", lambda e, ps=ps, mr=mr, cb=cb: e.tensor_tensor(out=mr[0:1, :], in0=ps[0:1, :], in1=bmod[0:1, cb * 512:(cb + 1) * 512], op=ALU.add),
                  reads=[ps.r(), bmod.r()], writes=[mr.r()])
            if v < 2:
                row_to_cols(lambda j, mr=mr: mr[0:1, j * 128:(j + 1) * 128], 4, PS[0],
                            modcol[:, s_ * 16 + v * 8 + half * 4:s_ * 16 + v * 8 + half * 4 + 4], [mr.r()], [modcol.r((s_, v, half))])
            else:
                row_bcast(mr[0:1, :], 512, PS[1], modbc[s_][:, (v - 2) * D + half * 512:(v - 2) * D + (half + 1) * 512],
                          [mr.r()], [modbc[s_].r((v - 2, half))])
    for s_ in range(NSRC):
        kb.op("dve", lambda e, s_=s_: e.scalar_tensor_tensor(out=gsc1[:, s_ * KC:(s_ + 1) * KC], in0=modcol[:, s_ * 16 + 8:s_ * 16 + 16], scalar=1.0,
                                                             in1=gmixc[:, :], op0=ALU.add, op1=ALU.mult),
              reads=[modcol.r((s_, 1, 0)), modcol.r((s_, 1, 1)), gmixc.r()], writes=[gsc1.r(s_)])
    for b in range(NB):
        for h in range(2):
            kb.op("dve", lambda e, b=b, h=h: e.scalar_tensor_tensor(out=modbc[b][:, 2 * D + h * 512:2 * D + (h + 1) * 512],
                                                                    in0=modbc[b][:, 2 * D + h * 512:2 * D + (h + 1) * 512], scalar=1.0,
                                                                    in1=gffn_bc[:, h * 512:(h + 1) * 512], op0=ALU.add, op1=ALU.mult),
                  reads=[modbc[b].r((2, h)), gffn_bc.r(h)], writes=[modbc[b].r((2, h))])

    def finish():
        for tok in kb.out_tokens:
            kb.wait_tok("sp", tok)
        for ds in kb.dsems:
            if ds.total > 0:
                kb.wait_tok("sp", (ds.sem, ds.total, None))
        es.close()
        return nc

    def dbg_out(name, src_ap, reads):
        if name in dbg_d:
            kb.out_tokens.append(kb.dma("pool", lambda q: q.dma_start(out=dbg_d[name], in_=src_ap), reads=reads))

    dbg_out("gsc1", gsc1[:, :], [gsc1.r(s_) for s_ in range(NSRC)])
    dbg_out("modbc0", modbc[0][:, :], [modbc[0].r((v, h)) for v in range(4) for h in range(2)])
    if bis == 3:
        return early()
    modbc_tok = []
    for b in range(NB):
        modbc_tok.append(kb.dma("sp", lambda q, b=b: q.dma_start(out=modbc_d[b], in_=modbc[b][:, :]),
                                reads=[modbc[b].r((v, h)) for v in range(4) for h in range(2)]))
    if bis == 4:
        return early()
    if bis == 5:
        kb.barrier()
        return early()
    sc0.__exit__()
    if stop <= 0:
        return finish()
    cwc = sb("cwc", [128, 24 * 9], F32)
    sc_cw = scope()
    sc_cw.__enter__()
    cwrow = sb("cwrow", [9, 3 * D], F32)
    kb.dma("sp", lambda q: q.dma_start(out=cwrow[:, :], in_=convw_d), writes=[cwrow.r()])
    for g_ in range(3):
        ps = PS[0]
        for j in range(8):
            ch = g_ * 8 + j
            mm(ps[:, j * 9:(j + 1) * 9], cwrow[0:9, ch * 128:(ch + 1) * 128], C("ident", rows=slice(0, 9), cols=slice(0, 9)), True, True,
               [cwrow.r()] + CR, [ps.r()])
        kb.op("dve", lambda e, g_=g_, ps=ps: e.tensor_copy(out=cwc[:, g_ * 72:(g_ + 1) * 72], in_=ps[:, 0:72]), reads=[ps.r()], writes=[cwc.r(g_)])
    sc_cw.__exit__()
    srow = sb("srow", [1, 32], F32)
    kb.dma("sp", lambda q: q.dma_start(out=srow[0:1, 0:16], in_=alog_d), writes=[srow.r(0)])
    kb.dma("sp", lambda q: q.dma_start(out=srow[0:1, 16:32], in_=dtb_d), writes=[srow.r(1)])
    gconst = sb("gconst", [128, 32], F32)
    row_bcast(srow[0:1, 0:32], 32, PS[1], gconst[:, :], [srow.r(0), srow.r(1)], [gconst.r()])
    kb.op("act", lambda e: e.activation(out=gconst[:, 0:16], in_=gconst[:, 0:16], func=AF.Exp), reads=[gconst.r()], writes=[gconst.r()])
    kb.op("dve", lambda e: e.tensor_scalar(out=gconst[:, 0:16], in0=gconst[:, 0:16], scalar1=-1.0, scalar2=None, op0=ALU.mult), reads=[gconst.r()], writes=[gconst.r()])
    nrow = sb("nrow", [1, 384], F32)
    kb.dma("sp", lambda q: q.dma_start(out=nrow[0:1, 0:128], in_=gdng_d), writes=[nrow.r(0)])
    kb.dma("sp", lambda q: q.dma_start(out=nrow[0:1, 128:384], in_=glag_d), writes=[nrow.r(1)])
    ngc = sb("ngc", [128, 3], F32)
    row_to_cols(lambda j: nrow[0:1, j * 128:(j + 1) * 128], 3, PS[0], ngc[:, :], [nrow.r(0), nrow.r(1)], [ngc.r()])
    win_v = win_d.rearrange("(kc p) n -> p kc n", p=128)
    wbd = sb("wbd", [128, KC, 32], BF16)
    wlr = sb("wlr", [128, KC, 32], BF16)
    kb.dma("pool", lambda q: q.dma_start(out=wbd[:, :, :], in_=win_v[:, :, OFF_BETA:OFF_BETA + 32]), writes=[wbd.r()])
    kb.dma("pool", lambda q: q.dma_start(out=wlr[:, :, :], in_=win_v[:, :, OFF_LR:OFF_LR + 32]), writes=[wlr.r()])

    if bis == 6:
        return early()
    gt_tmp = sb("gt_tmp", [128, 16], F32)

    TB = [(t0, min(512, S - t0)) for t0 in range(0, S, 512)]

    def tile_src(b, t):
        if t < NTC:
            return ctx_d[b, t * 128:(t + 1) * 128, :]
        return x_d[b, (t - NTC) * 128:(t - NTC + 1) * 128, :]

    def stage1(b):
        xt = [sb("xt%d" % i, [128, D], F32) for i in range(2)]
        xh = [sb("xh%d" % i, [128, D], BF16) for i in range(2)]
        junk = sb("junk", [128, D], BF16)
        st1 = [sb("st1_%d" % i, [128, 4], F32) for i in range(2)]
        for t in range(NT):
            i = t % 2
            src = NB if t < NTC else b
            kb.dma("sp", lambda q, t=t, i=i: q.dma_start(out=xt[i][:, :], in_=tile_src(b, t)), writes=[xt[i].r()])
            kb.op("act", lambda e, i=i: e.activation(out=junk[:, :], in_=xt[i][:, :], func=AF.Square, accum_out=st1[i][:, 0:1]),
                  reads=[xt[i].r()], writes=[junk.r(), st1[i].r()])
            kb.op("act", lambda e, i=i: e.activation(out=st1[i][:, 1:2], in_=st1[i][:, 0:1], func=AF.Sqrt, scale=1.0 / D, bias=EPS),
                  reads=[st1[i].r()], writes=[st1[i].r()])
            kb.op("dve", lambda e, i=i: e.reciprocal(out=st1[i][:, 2:3], in_=st1[i][:, 1:2]),
                  reads=[st1[i].r()], writes=[st1[i].r()])
            kb.op("act", lambda e, i=i: e.activation(out=xh[i][:, :], in_=xt[i][:, :], func=AF.Copy, scale=st1[i][:, 2:3]),
                  reads=[xt[i].r(), st1[i].r()], writes=[xh[i].r()])
            ps = PS[2 + i]
            psb = ps[:, :].bitcast(BF16)
            for kc in range(KC):
                kb.op("pe", lambda e, kc=kc, i=i, psb=psb: e.transpose(psb[:, kc * 128:(kc + 1) * 128], xh[i][:, kc * 128:(kc + 1) * 128], C("ident", bf=True)),
                      reads=[xh[i].r()] + CRB, writes=[ps.r()], inc=(kc == KC - 1))
            for kc in range(KC):
                sc_ = gsc1[:, src * KC + kc:src * KC + kc + 1]
                sh_ = modcol[:, src * 16 + kc:src * 16 + kc + 1]
                rds = [ps.r(), gsc1.r(src), modcol.r((src, 0, 0)), modcol.r((src, 0, 1))]
                if True:
                    kb.op("dve", lambda e, kc=kc, t=t, psb=psb, sc_=sc_, sh_=sh_: e.tensor_scalar(out=hT[:, kc, t * 128:(t + 1) * 128], in0=psb[:, kc * 128:(kc + 1) * 128],
                                                                                 scalar1=sc_, scalar2=sh_, op0=ALU.mult, op1=ALU.add),
                          reads=rds, writes=[hT.r((kc, t))])
                else:
                    kb.op("act", lambda e, kc=kc, t=t, psb=psb, sc_=sc_, sh_=sh_: e.activation(out=hT[:, kc, t * 128:(t + 1) * 128], in_=psb[:, kc * 128:(kc + 1) * 128],
                                                                              func=AF.Identity, scale=sc_, bias=sh_),
                          reads=rds, writes=[hT.r((kc, t))])

    def hT_reads(t0, n):
        return [hT.r((kc, t)) for kc in range(KC) for t in range(t0 // 128, (t0 + n) // 128)]

    def stage2_small(b):
        for t in range(NT):
            ps = PS[0]
            for kc in range(KC):
                mm(ps[:, 0:32], hT[:, kc, t * 128:(t + 1) * 128], wbd[:, kc, :], kc == 0, kc == KC - 1, hT_reads(t * 128, 128) + [wbd.r()], [ps.r()])
            kb.op("act", lambda e, t=t, ps=ps: e.activation(out=Gtok[:, t, 0:16], in_=ps[:, 0:16], func=AF.Sigmoid), reads=[ps.r()], writes=[Gtok.r(t)])
            kb.op("dve", lambda e, ps=ps: e.tensor_tensor(out=gt_tmp[:, :], in0=ps[:, 16:32], in1=gconst[:, 16:32], op=ALU.add), reads=[ps.r(), gconst.r()], writes=[gt_tmp.r()])
            kb.op("act", lambda e: e.activation(out=gt_tmp[:, :], in_=gt_tmp[:, :], func=AF.Exp), reads=[gt_tmp.r()], writes=[gt_tmp.r()])
            kb.op("act", lambda e: e.activation(out=gt_tmp[:, :], in_=gt_tmp[:, :], func=AF.Ln, bias=1.0), reads=[gt_tmp.r()], writes=[gt_tmp.r()])
            kb.op("dve", lambda e, t=t: e.tensor_tensor(out=Gtok[:, t, 16:32], in0=gt_tmp[:, :], in1=gconst[:, 0:16], op=ALU.mult), reads=[gt_tmp.r(), gconst.r()], writes=[Gtok.r(t)])

    def compute_betaT(b, betaT):
        for (t0, n) in TB:
            ps = PS[1]
            for kc in range(KC):
                mm(ps[0:16, 0:n], wbd[:, kc, 0:16], hT[:, kc, t0:t0 + n], kc == 0, kc == KC - 1, hT_reads(t0, n) + [wbd.r()], [ps.r()])
            kb.op("act", lambda e, t0=t0, n=n, ps=ps: e.activation(out=betaT[0:16, t0:t0 + n], in_=ps[0:16, 0:n], func=AF.Sigmoid), reads=[ps.r()], writes=[betaT.r()])

    def compute_lrT(b, lrT):
        for (t0, n) in TB:
            for d in range(2):
                ps = PS[d]
                for kc in range(KC):
                    mm(ps[0:16, 0:n], wlr[:, kc, d * 16:(d + 1) * 16], hT[:, kc, t0:t0 + n], kc == 0, kc == KC - 1, hT_reads(t0, n) + [wlr.r()], [ps.r()])
                kb.op("dve", lambda e, t0=t0, n=n, ps=ps, d=d: e.tensor_copy(out=lrT[d][0:16, t0:t0 + n], in_=ps[0:16, 0:n]), reads=[ps.r()], writes=[lrT[d].r()])

    ya_d = nc.dram_tensor("ya_s", [NB, D, SEQ], BF16, kind="Internal").ap()
    yb_d = nc.dram_tensor("yb_s", [NB, D, SEQ], BF16, kind="Internal").ap()
    LTB = [(t0, min(512, SEQ - t0)) for t0 in range(0, SEQ, 512)]

    def proj_chunk(wt, widx, dst, col0, ncols, tok0=0):
        k = 0
        for c0 in range(col0, col0 + ncols, 512):
            n = min(512, col0 + ncols - c0)
            ps = PS[2 + (k % 2)]
            for kc in range(KC):
                mm(ps[:, 0:n], wt[:, kc, widx, :], hT[:, kc, tok0 + c0:tok0 + c0 + n], kc == 0, kc == KC - 1,
                   hT_reads(tok0 + c0, n) + [wt.r(widx)], [ps.r()])
            if k % 2 == 0:
                kb.op("act", lambda e, ps=ps, c0=c0, n=n: e.activation(out=dst[:, c0:c0 + n], in_=ps[:, 0:n], func=AF.Copy), reads=[ps.r()], writes=[dst.r()])
            else:
                kb.op("dve", lambda e, ps=ps, c0=c0, n=n: e.tensor_copy(out=dst[:, c0:c0 + n], in_=ps[:, 0:n]), reads=[ps.r()], writes=[dst.r()])
            k += 1

    def conv(en, ch, pre, acc):
        cw = lambda tap: cwc[:, ch * 9 + tap:ch * 9 + tap + 1]
        rd = [pre.r(), acc.r(), cwc.r(ch // 8)]
        wr = [acc.r()]
        kb.op(en, lambda e: e.tensor_scalar(out=acc[:, 0:S], in0=pre[:, 0:S], scalar1=cw(4), scalar2=None, op0=ALU.mult), reads=rd, writes=wr)
        kb.op(en, lambda e: e.scalar_tensor_tensor(out=acc[:, 1:CTX], in0=pre[:, 0:CTX - 1], scalar=cw(3), in1=acc[:, 1:CTX], op0=ALU.mult, op1=ALU.add), reads=rd, writes=wr)
        kb.op(en, lambda e: e.scalar_tensor_tensor(out=acc[:, 0:CTX - 1], in0=pre[:, 1:CTX], scalar=cw(5), in1=acc[:, 0:CTX - 1], op0=ALU.mult, op1=ALU.add), reads=rd, writes=wr)
        P3 = pre[:, CTX:S].rearrange("p (r c) -> p r c", c=GW)
        A3 = acc[:, CTX:S].rearrange("p (r c) -> p r c", c=GW)
        for a in range(3):
            for bb in range(3):
                if a == 1 and bb == 1:
                    continue
                dr, dc = a - 1, bb - 1
                r0, r1 = max(0, -dr), ROWS - max(0, dr)
                c0, c1 = max(0, -dc), GW - max(0, dc)
                kb.op(en, lambda e, a=a, bb=bb, dr=dr, dc=dc, r0=r0, r1=r1, c0=c0, c1=c1: e.scalar_tensor_tensor(
                    out=A3[:, r0:r1, c0:c1], in0=P3[:, r0 + dr:r1 + dr, c0 + dc:c1 + dc], scalar=cw(a * 3 + bb), in1=A3[:, r0:r1, c0:c1],
                    op0=ALU.mult, op1=ALU.add), reads=rd, writes=wr)

    def l2norm(dst, scale, tmpb, tmpf):
        for (t0, n) in TB:
            kb.op("act", lambda e, t0=t0, n=n: e.activation(out=tmpb[:, 0:n], in_=dst[:, t0:t0 + n], func=AF.Square), reads=[dst.r()], writes=[tmpb.r()])
            ps = PS[2]
            mm(ps[:, 0:n], C("ones", bf=True), tmpb[:, 0:n], True, True, [tmpb.r()] + CRB, [ps.r()])
            kb.op("act", lambda e, n=n, ps=ps: e.activation(out=tmpf[:, 0:n], in_=ps[:, 0:n], func=AF.Ln, bias=EPS), reads=[ps.r()], writes=[tmpf.r()])
            kb.op("act", lambda e, n=n: e.activation(out=tmpf[:, 0:n], in_=tmpf[:, 0:n], func=AF.Exp, scale=-0.5), reads=[tmpf.r()], writes=[tmpf.r()])
            kb.op("dve", lambda e, t0=t0, n=n: e.scalar_tensor_tensor(out=dst[:, t0:t0 + n], in0=dst[:, t0:t0 + n], scalar=scale, in1=tmpf[:, 0:n], op0=ALU.mult, op1=ALU.mult),
                  reads=[dst.r(), tmpf.r()], writes=[dst.r()])

    def interleave(gens):
        gens = [g for g in gens if g is not None]
        while gens:
            for g in list(gens):
                try:
                    next(g)
                except StopIteration:
                    gens.remove(g)

    def gdn_all(b):
        betaT = sb("betaT", [16, S], BF16)
        compute_betaT(b, betaT)
        wch = sb("wch", [128, KC, 3, 128], BF16)
        wz = sb("wz", [128, KC, 1, 128], BF16)
        pre = sb("pre", [128, S], BF16)
        pre2 = sb("pre2", [128, S], BF16)
        oT = sb("oTa", [128, SEQ], BF16)
        qT = sb("qT", [128, S], BF16)
        kT = sb("kT", [128, S], BF16)
        vT = sb("vT", [128, S], BF16)
        zT = qT
        kbT = pre
        tmpb = sb("tmpb", [128, 512], BF16)
        tmpf = sb("tmpf", [128, 512], F32)
        tmpg = sb("tmpg", [128, 512], F32)
        Sf = [sb("Sf%d" % d_, [128, 128], F32) for d_ in range(2)]
        Sb = [sb("Sb%d" % d_, [128, 128], BF16) for d_ in range(2)]
        rbf = [sb("rbf%d" % d_, [128, 128], BF16) for d_ in range(2)]
        vnb = [sb("vnb%d" % d_, [128, 128], BF16) for d_ in range(2)]
        kbT1 = sb("kbT1", [128, S], BF16)
        WK = []
        for i in range(4):
            WK.append(dict(
                grep=sb("grep%d" % i, [128, 128], F32), tmp=sb("wtmp%d" % i, [128, 128], F32),
                E=sb("E%d" % i, [128, 128], BF16), Es=sb("Es%d" % i, [128, 128], BF16), erow=sb("erow%d" % i, [128, 128], BF16),
                L=[sb("L%d_%d" % (i, j), [128, 384], BF16) for j in range(2)],
                attnT=sb("attnT%d" % i, [128, 128], BF16), qgT=sb("qgT%d" % i, [128, 128], BF16), nkbgT=sb("nkbgT%d" % i, [128, 128], BF16),
                kd=sb("kd%d" % i, [128, 128], BF16), vb=sb("vb%d" % i, [128, 128], BF16), cols=sb("cols%d" % i, [128, 8], F32),
                TT=None))

        def load_qkv_w(hh):
            for j, off in enumerate((OFF_QA, OFF_KA, OFF_VA)):
                kb.dma("pool", lambda q, j=j, off=off: q.dma_start(out=wch[:, :, j, :], in_=win_v[:, :, off + hh * 128:off + (hh + 1) * 128]), writes=[wch.r(j)])

        load_qkv_w(0)
        for h in range(NH_A):
            kb.dma("pool", lambda q: q.dma_start(out=wz[:, :, 0, :], in_=win_v[:, :, OFF_ZA + h * 128:OFF_ZA + (h + 1) * 128]), writes=[wz.r(0)])
            pres = (pre, kbT1, pre2)
            for j in range(3):
                proj_chunk(wch, j, pres[j], 0, S)
            if h + 1 < NH_A:
                load_qkv_w(h + 1)
            for j, dst in enumerate((qT, kT, vT)):
                conv("dve", j * 8 + h, pres[j], dst)
                kb.op("act", lambda e, dst=dst: e.activation(out=dst[:, 0:S], in_=dst[:, 0:S], func=AF.Silu), reads=[dst.r()], writes=[dst.r()])
            l2norm(qT, float(128 ** -0.5), tmpb, tmpf)
            l2norm(kT, 1.0, tmpb, tmpf)
            if b == 0 and h == 0:
                dbg_out("qT", qT[:, :], [qT.r()])
                dbg_out("kT", kT[:, :], [kT.r()])
                dbg_out("vT", vT[:, :], [vT.r()])
            owr = set()
            kbTs = [kbT, kbT1]
            interleave([gdn_scan(b, h, d, locals()) for d in range(2)])
            proj_chunk(wz, 0, zT, 0, SEQ, tok0=CTX)
            for (t0, n) in LTB:
                kb.op("act", lambda e, t0=t0, n=n: e.activation(out=tmpb[:, 0:n], in_=oT[:, t0:t0 + n], func=AF.Square), reads=[oT.r()], writes=[tmpb.r()])
                ps = PS[2]
                mm(ps[:, 0:n], C("ones", bf=True), tmpb[:, 0:n], True, True, [tmpb.r()] + CRB, [ps.r()])
                kb.op("act", lambda e, n=n, ps=ps: e.activation(out=tmpf[:, 0:n], in_=ps[:, 0:n], func=AF.Ln, scale=1.0 / 128, bias=EPS), reads=[ps.r()], writes=[tmpf.r()])
                kb.op("act", lambda e, n=n: e.activation(out=tmpf[:, 0:n], in_=tmpf[:, 0:n], func=AF.Exp, scale=-0.5), reads=[tmpf.r()], writes=[tmpf.r()])
                kb.op("dve", lambda e, t0=t0, n=n: e.scalar_tensor_tensor(out=tmpf[:, 0:n], in0=oT[:, t0:t0 + n], scalar=ngc[:, 0:1], in1=tmpf[:, 0:n], op0=ALU.mult, op1=ALU.mult),
                      reads=[oT.r(), tmpf.r(), ngc.r()], writes=[tmpf.r()])
                kb.op("act", lambda e, t0=t0, n=n: e.activation(out=tmpg[:, 0:n], in_=zT[:, t0:t0 + n], func=AF.Silu), reads=[zT.r()], writes=[tmpg.r()])
                kb.op("dve", lambda e, n=n: e.tensor_tensor(out=tmpb[:, 0:n], in0=tmpf[:, 0:n], in1=tmpg[:, 0:n], op=ALU.mult), reads=[tmpf.r(), tmpg.r()], writes=[tmpb.r()])
                kb.dma("sp", lambda q, t0=t0, n=n: q.dma_start(out=ya_d[b, h * 128:(h + 1) * 128, t0:t0 + n], in_=tmpb[:, 0:n]), reads=[tmpb.r()], writes=[yaR])
            if b == 0:
                dbg_out("oT%d" % h, oT[:, 0:SEQ], [oT.r()])

    yaR = kb.res("ya_dram")
    ybR = kb.res("yb_dram")

    def gdn_scan(b, h, d, L_):
        betaT, qT, kT, vT, oT, owr = (L_[k] for k in ("betaT", "qT", "kT", "vT", "oT", "owr"))
        kbT = L_["kbTs"][d]
        WK = L_["WK"][2 * d:2 * d + 2]
        Sf, Sb, rbf, vnb = L_["Sf"][d], L_["Sb"][d], L_["rbf"][d], L_["vnb"][d]
        tri, neg = ("tri_f", "neg_f") if d == 0 else ("tri_b", "neg_b")
        if d == 0:
            order = list(range(NT))
        else:
            order = [NTC - 1 - t for t in range(NTC)] + [NT - 1 - t for t in range(NTL)]
        r = d * 8 + h
        for (t0, n) in TB:
            ps = PS[3]
            mm(ps[:, 0:n], sel16[0:16, r * 128:(r + 1) * 128], betaT[0:16, t0:t0 + n], True, True, [sel16.r(), betaT.r()], [ps.r()])
            kb.op("dve", lambda e, t0=t0, n=n, ps=ps: e.tensor_tensor(out=kbT[:, t0:t0 + n], in0=kT[:, t0:t0 + n], in1=ps[:, 0:n], op=ALU.mult), reads=[kT.r(), ps.r()], writes=[kbT.r()])
        kb.op("dve", lambda e: e.memset(Sf[:, :], 0.0), writes=[Sf.r()])
        kb.op("dve", lambda e: e.memset(Sb[:, :], 0.0), writes=[Sb.r()])

        def pre_gen(idx, t):
            W = WK[idx % 2]
            ts_ = slice(t * 128, (t + 1) * 128)
            gcol = Gtok[:, t, 16 + r:17 + r]
            bcol = Gtok[:, t, r:r + 1]
            cols, grep, tmp, E, Es, erow = W["cols"], W["grep"], W["tmp"], W["E"], W["Es"], W["erow"]
            bX, bY = (PS[2], PS[3]) if d == 0 else (PS[5], PS[6])
            kb.op("act", lambda e: e.activation(out=grep[:, :], in_=C("ones"), func=AF.Copy, scale=gcol), reads=[Gtok.r(t)] + CR, writes=[grep.r()])
            mm(bY[:, 448:449], C(tri), gcol, True, True, [Gtok.r(t)] + CR, [bY.r()])
            mm(bY[:, 449:450], C("blk"), gcol, True, True, [Gtok.r(t)] + CR, [bY.r()])
            mm(bX[:, 0:128], grep[:, :], C(tri), True, True, [grep.r()] + CR, [bX.r()])
            mm(bY[:, 450:451], grep[:, :], C("blk", cols=slice(0, 1)), True, True, [grep.r()] + CR, [bY.r()])
            mm(bY[:, 451:452], grep[:, :], C("blk", cols=slice(64, 65)), True, True, [grep.r()] + CR, [bY.r()])
            kb.op("dve", lambda e: e.tensor_copy(out=cols[:, 0:2], in_=bY[:, 448:450]), reads=[bY.r()], writes=[cols.r()])
            kb.op("dve", lambda e: e.tensor_tensor(out=cols[:, 2:3], in0=cols[:, 1:2], in1=cols[:, 0:1], op=ALU.subtract), reads=[cols.r()], writes=[cols.r()])
            kb.op("act", lambda e: e.activation(out=cols[:, 2:4], in_=cols[:, 2:4], func=AF.Exp) if False else e.activation(out=cols[:, 2:3], in_=cols[:, 2:3], func=AF.Exp), reads=[cols.r()], writes=[cols.r()])
            kb.op("act", lambda e: e.activation(out=cols[:, 3:4], in_=cols[:, 0:1], func=AF.Exp), reads=[cols.r()], writes=[cols.r()])
            kb.op("dve", lambda e: e.tensor_tensor(out=cols[:, 4:5], in0=cols[:, 3:4], in1=bcol, op=ALU.mult), reads=[cols.r(), Gtok.r(t)], writes=[cols.r()])
            kb.op("act", lambda e: e.activation(out=cols[:, 5:7], in_=bY[:, 450:452], func=AF.Exp), reads=[bY.r()], writes=[cols.r()])
            kb.op("dve", lambda e: e.scalar_tensor_tensor(out=tmp[:, :], in0=bX[:, 0:128], scalar=cols[:, 0:1], in1=C(neg), op0=ALU.subtract, op1=ALU.add),
                  reads=[bX.r(), cols.r()] + CR, writes=[tmp.r()])
            kb.op("act", lambda e: e.activation(out=E[:, :], in_=tmp[:, :], func=AF.Exp), reads=[tmp.r()], writes=[E.r()])
            kb.op("pool", lambda e: e.tensor_tensor(out=Es[:, :], in0=E[:, :], in1=C("offd", bf=True), op=ALU.mult), reads=[E.r()] + CRB, writes=[Es.r()])
            kb.op("act", lambda e: e.activation(out=erow[:, :], in_=bX[:, 0:128], func=AF.Exp), reads=[bX.r()], writes=[erow.r()])
            yield
            mm(bX[:, 128:256], kT[:, ts_], kbT[:, ts_], True, True, [kT.r(), kbT.r()], [bX.r()])
            mm(bX[:, 256:384], kT[:, ts_], qT[:, ts_], True, True, [kT.r(), qT.r()], [bX.r()])
            L0 = W["L"][0]
            kb.op("dve", lambda e: e.scalar_tensor_tensor(out=L0[:, 128:256], in0=bX[:, 128:256], scalar=-1.0, in1=Es[:, :], op0=ALU.mult, op1=ALU.mult),
                  reads=[bX.r(), Es.r()], writes=[L0.r("x")])
            kb.op("dve", lambda e: e.tensor_tensor(out=W["attnT"][:, :], in0=bX[:, 256:384], in1=E[:, :], op=ALU.mult), reads=[bX.r(), E.r()], writes=[W["attnT"].r()])
            bXb = bX[:, :].bitcast(BF16)
            bYb = bY[:, :].bitcast(BF16)
            kb.op("pe", lambda e: e.transpose(bYb[:, 768:896], L0[:, 128:256], C("ident", bf=True)), reads=[L0.r("x")] + CRB, writes=[bY.r()])
            kb.op("pe", lambda e: e.transpose(bXb[:, 768:896], kT[:, ts_], C("ident", bf=True)), reads=[kT.r()] + CRB, writes=[bX.r()])
            kb.op("pe", lambda e: e.transpose(bXb[:, 896:1024], vT[:, ts_], C("ident", bf=True)), reads=[vT.r()] + CRB, writes=[bX.r()])
            kb.op("dve", lambda e: e.tensor_copy(out=L0[:, 256:384], in_=bYb[:, 768:896]), reads=[bY.r()], writes=[L0.r("w")])
            kb.op("pool", lambda e: e.tensor_copy(out=L0[:, 0:128], in_=C("ident", bf=True)), reads=CRB, writes=[L0.r("q")])
            kb.op("dve", lambda e: e.tensor_scalar(out=W["kd"][:, :], in0=bXb[:, 768:896], scalar1=cols[:, 2:3], scalar2=None, op0=ALU.mult), reads=[bX.r(), cols.r()], writes=[W["kd"].r()])
            kb.op("dve", lambda e: e.tensor_scalar(out=W["vb"][:, :], in0=bXb[:, 896:1024], scalar1=bcol, scalar2=None, op0=ALU.mult), reads=[bX.r(), Gtok.r(t)], writes=[W["vb"].r()])
            kb.op("pool", lambda e: e.tensor_tensor(out=W["qgT"][:, :], in0=qT[:, ts_], in1=erow[:, :], op=ALU.mult), reads=[qT.r(), erow.r()], writes=[W["qgT"].r()])
            kb.op("dve", lambda e: e.scalar_tensor_tensor(out=W["nkbgT"][:, :], in0=kbT[:, ts_], scalar=-1.0, in1=erow[:, :], op0=ALU.mult, op1=ALU.mult),
                  reads=[kbT.r(), erow.r()], writes=[W["nkbgT"].r()])
            yield
            for hop in range(6):
                Li, Lo = W["L"][hop % 2], W["L"][(hop + 1) % 2]
                lr_ = [Li.r("q"), Li.r("x"), Li.r("w")]
                mm(bY[:, 0:128], C("ident", bf=True), Li[:, 0:128], True, False, lr_ + CRB, [bY.r()], inc=False)
                mm(bY[:, 0:128], Li[:, 256:384], Li[:, 0:128], False, True, lr_, [bY.r()], inc=(hop == 5))
                if hop < 5:
                    mm(bY[:, 256:384], Li[:, 128:256], Li[:, 256:384], True, True, lr_, [bY.r()], inc=(hop == 4))
                if hop < 4:
                    mm(bY[:, 128:256], Li[:, 256:384], Li[:, 128:256], True, True, lr_, [bY.r()], inc=True)
                hi = 128 if hop == 5 else 384
                wr = [Lo.r("q"), Lo.r("x"), Lo.r("w")]
                if hop % 2 == 0:
                    kb.op("act", lambda e, Lo=Lo, hi=hi: e.activation(out=Lo[:, 0:hi], in_=bY[:, 0:hi], func=AF.Copy), reads=[bY.r()], writes=wr)
                else:
                    kb.op("dve", lambda e, Lo=Lo, hi=hi: e.tensor_copy(out=Lo[:, 0:hi], in_=bY[:, 0:hi]), reads=[bY.r()], writes=wr)
                yield
            W["TT"] = W["L"][0]

        def scan_gen(idx, t):
            W = WK[idx % 2]
            TT = W["L"][0]
            cols = W["cols"]
            pS = PS[4] if d == 0 else PS[7]
            for c in ((0, 1) if d == 0 else (1, 0)):
                sl = slice(c * 64, c * 64 + 64)
                mm(pS[:, 0:128], C("ident", bf=True), W["vb"][:, :], True, False, [W["vb"].r()] + CRB, [pS.r("r")], inc=False)
                mm(pS[:, 0:128], W["nkbgT"][:, :], Sb[:, :], False, True, [W["nkbgT"].r(), Sb.r()], [pS.r("r")])
                kb.op("act", lambda e, sl=sl: e.activation(out=rbf[sl, :], in_=pS[sl, 0:128], func=AF.Copy), reads=[pS.r("r")], writes=[rbf.r()])
                yield
                mm(pS[:, 128:256], TT[sl, 0:128], rbf[sl, :], True, True, [TT.r("q"), rbf.r()], [pS.r("v")])
                kb.op("dve", lambda e, sl=sl: e.tensor_copy(out=vnb[sl, :], in_=pS[sl, 128:256]), reads=[pS.r("v")], writes=[vnb.r()])
                yield
                if t >= NTC:
                    pO = pS
                    mm(pO[:, 256:320], Sb[:, :], W["qgT"][:, sl], True, False, [Sb.r(), W["qgT"].r()], [pO.r()], inc=False)
                    mm(pO[:, 256:320], vnb[sl, :], W["attnT"][sl, sl], False, True, [vnb.r(), W["attnT"].r()], [pO.r()])
                    o0 = (t - NTC) * 128 + c * 64
                    if o0 not in owr:
                        owr.add(o0)
                        kb.op("act", lambda e, o0=o0: e.activation(out=oT[:, o0:o0 + 64], in_=pO[:, 256:320], func=AF.Copy), reads=[pO.r()], writes=[oT.r()])
                    else:
                        kb.op("dve", lambda e, o0=o0: e.tensor_tensor(out=oT[:, o0:o0 + 64], in0=pO[:, 256:320], in1=oT[:, o0:o0 + 64], op=ALU.add), reads=[pO.r(), oT.r()], writes=[oT.r()])
                mm(pS[:, 320:448], W["kd"][sl, :], vnb[sl, :], True, True, [W["kd"].r(), vnb.r()], [pS.r("s")])
                kb.op("dve", lambda e, c=c: e.scalar_tensor_tensor(out=Sf[:, :], in0=Sf[:, :], scalar=cols[:, 5 + c:6 + c], in1=pS[:, 320:448], op0=ALU.mult, op1=ALU.add),
                      reads=[Sf.r(), cols.r(), pS.r("s")], writes=[Sf.r()])
                kb.op("act", lambda e: e.activation(out=Sb[:, :], in_=Sf[:, :], func=AF.Copy), reads=[Sf.r()], writes=[Sb.r()])
                yield

        yield
        for _ in pre_gen(0, order[0]):
            yield
        for idx, t in enumerate(order):
            nxt = pre_gen(idx + 1, order[idx + 1]) if idx + 1 < len(order) else None
            gens = [g for g in (scan_gen(idx, t), nxt) if g is not None]
            while gens:
                for g in list(gens):
                    try:
                        next(g)
                        yield
                    except StopIteration:
                        gens.remove(g)

    def gla_all(b):
        gwb_t = sb("gwb", [48, 512], BF16)

        class _Rows:
            def __init__(s_, buf, base): s_.buf, s_.base = buf, base
            def __getitem__(s_, idx): return s_.buf[slice(idx[0].start + s_.base, idx[0].stop + s_.base), idx[1]]
            def r(s_, key=0): return s_.buf.r()
        gwb = [_Rows(gwb_t, 0), _Rows(gwb_t, 32)]
        gb_bc = [sb("gb_bc%d" % d, [128, 512], F32) for d in range(2)]
        with scope():
            gbrow = sb("gbrow", [1, 1024], F32)
            for d in range(2):
                kb.dma("pool", lambda q, d=d: q.dma_start(out=gwb[d][0:16, 0:512], in_=glaw_d[d]), writes=[gwb[d].r()])
                kb.dma("sp", lambda q, d=d: q.dma_start(out=gbrow[0:1, d * 512:(d + 1) * 512], in_=glab_d[d:d + 1, :]), writes=[gbrow.r(d)])
                row_bcast(gbrow[0:1, d * 512:(d + 1) * 512], 512, PS[1], gb_bc[d][:, :], [gbrow.r(d)], [gb_bc[d].r()])
        lrT_t = sb("lrT", [48, S], BF16)
        lrT = [_Rows(lrT_t, 0), _Rows(lrT_t, 32)]
        compute_lrT(b, lrT)
        wch = sb("wchb", [128, KC, 6, 128], BF16)
        qT = sb("qbT", [128, S], BF16)
        kT = sb("kbT_", [128, S], BF16)
        vT = sb("vbT", [128, 2 * S], BF16)
        rT = vT
        oT = sb("oTb", [128, 2 * SEQ], BF16)
        tmpb = sb("g_tmpb", [128, 512], BF16)
        tmpb2 = sb("g_tmpb2", [128, 512], BF16)
        tmpf = sb("g_tmpf", [128, 512], F32)
        tmpg = sb("g_tmpg", [128, 512], F32)
        Sf = [sb("g_Sf%d" % d_, [128, 256], F32) for d_ in range(2)]
        Sb = [sb("g_Sb%d" % d_, [128, 256], BF16) for d_ in range(2)]
        WK = []
        for i in range(4):
            WK.append(dict(
                la=sb("la%d" % i, [128, 128], F32), tmpx=sb("tmpx%d" % i, [128, 128], F32), edk=sb("edk%d" % i, [128, 128], F32),
                gcT=sb("gcT%d" % i, [128, 128], F32), eg=sb("eg%d" % i, [128, 128], BF16), glc=sb("glc%d" % i, [128, 2], F32),
                tq=sb("tq%d" % i, [128, 128], F32), eq=sb("eq%d" % i, [128, 128], BF16), ek=sb("ek%d" % i, [128, 128], BF16),
                qgT=sb("gqgT%d" % i, [128, 128], BF16), qrT=sb("qrT%d" % i, [128, 128], BF16), krT=sb("krT%d" % i, [128, 128], BF16),
                attnT=sb("gattnT%d" % i, [128, 128], BF16), kd=sb("gkd%d" % i, [128, 128], BF16), vtok=sb("vtok%d" % i, [128, 256], BF16)))
        for hb in range(NH_B):
            offs = [OFF_QB + hb * 128, OFF_KB + hb * 128, OFF_VB + hb * 256, OFF_VB + hb * 256 + 128, OFF_RB + hb * 256, OFF_RB + hb * 256 + 128]
            for j, off in enumerate(offs):
                kb.dma("pool", lambda q, j=j, off=off: q.dma_start(out=wch[:, :, j, :], in_=win_v[:, :, off:off + 128]), writes=[wch.r(j)])
            proj_chunk(wch, 0, qT, 0, S)
            kb.op("dve", lambda e: e.tensor_scalar(out=qT[:, 0:S], in0=qT[:, 0:S], scalar1=float(128 ** -0.5), scalar2=None, op0=ALU.mult), reads=[qT.r()], writes=[qT.r()])
            proj_chunk(wch, 1, kT, 0, S)
            vT0 = Buf(kb, vT.t, "vT0"); vT0._r = vT._r
            for hv in range(2):
                class _V:
                    def __init__(s_, base): s_.base = base
                    def __getitem__(s_, idx): return vT[idx[0], slice(idx[1].start + s_.base, idx[1].stop + s_.base)]
                    def r(s_, key=0): return vT.r()
                proj_chunk(wch, 2 + hv, _V(hv * S), 0, S)
            owr = set()
            interleave([gla_scan(b, hb, d, locals()) for d in range(2)])
            for hv in range(2):
                class _R:
                    def __init__(s_, base): s_.base = base
                    def __getitem__(s_, idx): return rT[idx[0], slice(idx[1].start + s_.base, idx[1].stop + s_.base)]
                    def r(s_, key=0): return rT.r()
                proj_chunk(wch, 4 + hv, _R(hv * SEQ), 0, SEQ, tok0=CTX)
            for (t0, n) in LTB:
                ps = PS[2]
                tb_ = (tmpb, tmpb2)
                for hv in range(2):
                    kb.op("act", lambda e, t0=t0, n=n, hv=hv: e.activation(out=tb_[hv][:, 0:n], in_=oT[:, hv * SEQ + t0:hv * SEQ + t0 + n], func=AF.Square), reads=[oT.r()], writes=[tb_[hv].r()])
                for hv in range(2):
                    mm(ps[:, 0:n], C("ones", bf=True), tb_[hv][:, 0:n], hv == 0, hv == 1, [tb_[hv].r()] + CRB, [ps.r()])
                kb.op("act", lambda e, n=n, ps=ps: e.activation(out=tmpf[:, 0:n], in_=ps[:, 0:n], func=AF.Ln, scale=1.0 / 256, bias=EPS), reads=[ps.r()], writes=[tmpf.r()])
                kb.op("act", lambda e, n=n: e.activation(out=tmpf[:, 0:n], in_=tmpf[:, 0:n], func=AF.Exp, scale=-0.5), reads=[tmpf.r()], writes=[tmpf.r()])
                for hv in range(2):
                    kb.op("dve", lambda e, t0=t0, n=n, hv=hv: e.scalar_tensor_tensor(out=tmpg[:, 0:n], in0=oT[:, hv * SEQ + t0:hv * SEQ + t0 + n], scalar=ngc[:, 1 + hv:2 + hv], in1=tmpf[:, 0:n], op0=ALU.mult, op1=ALU.mult),
                          reads=[oT.r(), tmpf.r(), ngc.r()], writes=[tmpg.r()])
                    kb.op("act", lambda e, t0=t0, n=n, hv=hv: e.activation(out=tb_[hv][:, 0:n], in_=rT[:, hv * SEQ + t0:hv * SEQ + t0 + n], func=AF.Silu), reads=[rT.r()], writes=[tb_[hv].r()])
                    kb.op("dve", lambda e, n=n, hv=hv: e.tensor_tensor(out=tb_[hv][:, 0:n], in0=tmpg[:, 0:n], in1=tb_[hv][:, 0:n], op=ALU.mult), reads=[tmpg.r(), tb_[hv].r()], writes=[tb_[hv].r()])
                    kb.dma("sp", lambda q, t0=t0, n=n, hv=hv: q.dma_start(out=yb_d[b, hb * 256 + hv * 128:hb * 256 + (hv + 1) * 128, t0:t0 + n], in_=tb_[hv][:, 0:n]), reads=[tb_[hv].r()], writes=[ybR])
            if b == 0:
                dbg_out("obT%d" % hb, oT[:, :], [oT.r()])

    def gla_scan(b, hb, d, L_):
        lrT, qT, kT, vT, oT, gwb, gb_bc, owr = (L_[k] for k in ("lrT", "qT", "kT", "vT", "oT", "gwb", "gb_bc", "owr"))
        WK = L_["WK"][2 * d:2 * d + 2]
        Sf, Sb = L_["Sf"][d], L_["Sb"][d]
        tri = "tri_f" if d == 0 else "tri_b"
        if d == 0:
            order = list(range(NT))
        else:
            order = [NTC - 1 - t for t in range(NTC)] + [NT - 1 - t for t in range(NTL)]
        kb.op("dve", lambda e: e.memset(Sf[:, :], 0.0), writes=[Sf.r()])
        kb.op("dve", lambda e: e.memset(Sb[:, :], 0.0), writes=[Sb.r()])
        pA, pC = (PS[2], PS[3]) if d == 0 else (PS[5], PS[6])
        pT = pC

        def pre_gen(idx, t):
            W = WK[idx % 2]
            ts_ = slice(t * 128, (t + 1) * 128)
            la = W["la"]
            mm(pA[:, 0:128], lrT[d][0:16, ts_], gwb[d][0:16, hb * 128:(hb + 1) * 128], True, True, [lrT[d].r(), gwb[d].r()], [pA.r()])
            kb.op("dve", lambda e: e.tensor_tensor(out=la[:, :], in0=pA[:, 0:128], in1=gb_bc[d][:, hb * 128:(hb + 1) * 128], op=ALU.add), reads=[pA.r(), gb_bc[d].r()], writes=[la.r()])
            kb.op("act", lambda e: e.activation(out=la[:, :], in_=la[:, :], func=AF.Exp, scale=-1.0), reads=[la.r()], writes=[la.r()])
            kb.op("act", lambda e: e.activation(out=la[:, :], in_=la[:, :], func=AF.Ln, bias=1.0), reads=[la.r()], writes=[la.r()])
            kb.op("dve", lambda e: e.tensor_scalar(out=la[:, :], in0=la[:, :], scalar1=-1.0 / 16.0, scalar2=None, op0=ALU.mult), reads=[la.r()], writes=[la.r()])
            yield
            mm(pA[:, 0:128], C(tri), la[:, :], True, True, [la.r()] + CR, [pA.r()])
            mm(pA[:, 128:256], C("blk"), la[:, :], True, True, [la.r()] + CR, [pA.r()])
            mm(pA[:, 256:384], la[:, :], C(tri), True, True, [la.r()] + CR, [pA.r()])
            mm(pA[:, 384:385], la[:, :], C("blk", cols=slice(0, 1)), True, True, [la.r()] + CR, [pA.r()])
            mm(pA[:, 385:386], la[:, :], C("blk", cols=slice(64, 65)), True, True, [la.r()] + CR, [pA.r()])
            kb.op("act", lambda e: e.activation(out=W["tmpx"][:, :], in_=pA[:, 0:128], func=AF.Copy), reads=[pA.r()], writes=[W["tmpx"].r()])
            kb.op("dve", lambda e: e.tensor_tensor(out=W["tmpx"][:, :], in0=pA[:, 128:256], in1=W["tmpx"][:, :], op=ALU.subtract), reads=[pA.r(), W["tmpx"].r()], writes=[W["tmpx"].r()])
            kb.op("act", lambda e: e.activation(out=W["edk"][:, :], in_=W["tmpx"][:, :], func=AF.Exp), reads=[W["tmpx"].r()], writes=[W["edk"].r()])
            kb.op("dve", lambda e: e.tensor_copy(out=W["gcT"][:, :], in_=pA[:, 256:384]), reads=[pA.r()], writes=[W["gcT"].r()])
            kb.op("act", lambda e: e.activation(out=W["eg"][:, :], in_=pA[:, 256:384], func=AF.Exp), reads=[pA.r()], writes=[W["eg"].r()])
            kb.op("act", lambda e: e.activation(out=W["glc"][:, 0:2], in_=pA[:, 384:386], func=AF.Exp), reads=[pA.r()], writes=[W["glc"].r()])
            yield
            kb.op("pool", lambda e: e.tensor_tensor(out=W["qgT"][:, :], in0=qT[:, ts_], in1=W["eg"][:, :], op=ALU.mult), reads=[qT.r(), W["eg"].r()], writes=[W["qgT"].r()])
            for c in range(2):
                sl = slice(c * 64, c * 64 + 64)
                ref_ = W["gcT"][:, c * 64 + 32:c * 64 + 33]
                kb.op("dve", lambda e, sl=sl, ref_=ref_: e.tensor_scalar(out=W["tq"][:, sl], in0=W["gcT"][:, sl], scalar1=ref_, scalar2=None, op0=ALU.subtract), reads=[W["gcT"].r()], writes=[W["tq"].r()])
            kb.op("act", lambda e: e.activation(out=W["eq"][:, :], in_=W["tq"][:, :], func=AF.Exp), reads=[W["tq"].r()], writes=[W["eq"].r()])
            kb.op("act", lambda e: e.activation(out=W["ek"][:, :], in_=W["tq"][:, :], func=AF.Exp, scale=-1.0), reads=[W["tq"].r()], writes=[W["ek"].r()])
            kb.op("pool", lambda e: e.tensor_tensor(out=W["qrT"][:, :], in0=qT[:, ts_], in1=W["eq"][:, :], op=ALU.mult), reads=[qT.r(), W["eq"].r()], writes=[W["qrT"].r()])
            kb.op("pool", lambda e: e.tensor_tensor(out=W["krT"][:, :], in0=kT[:, ts_], in1=W["ek"][:, :], op=ALU.mult), reads=[kT.r(), W["ek"].r()], writes=[W["krT"].r()])
            yield
            mm(pC[:, 0:128], W["krT"][:, :], W["qrT"][:, :], True, True, [W["krT"].r(), W["qrT"].r()], [pC.r()])
            pTb = pT[:, :].bitcast(BF16)
            kb.op("pe", lambda e: e.transpose(pTb[:, 256:384], kT[:, ts_], C("ident", bf=True)), reads=[kT.r()] + CRB, writes=[pT.r()])
            for hv in range(2):
                kb.op("pe", lambda e, hv=hv: e.transpose(pTb[:, 384 + hv * 128:512 + hv * 128], vT[:, hv * S + t * 128:hv * S + (t + 1) * 128], C("ident", bf=True)), reads=[vT.r()] + CRB, writes=[pT.r()])
            kb.op("dve", lambda e: e.tensor_tensor(out=W["attnT"][:, :], in0=pC[:, 0:128], in1=C(tri, bf=True), op=ALU.mult), reads=[pC.r()] + CRB, writes=[W["attnT"].r()])
            kb.op("dve", lambda e: e.tensor_tensor(out=W["kd"][:, :], in0=pTb[:, 256:384], in1=W["edk"][:, :], op=ALU.mult), reads=[pT.r(), W["edk"].r()], writes=[W["kd"].r()])
            kb.op("dve", lambda e: e.tensor_copy(out=W["vtok"][:, :], in_=pTb[:, 384:640]), reads=[pT.r()], writes=[W["vtok"].r()])
            yield

        def scan_gen(idx, t):
            W = WK[idx % 2]
            pS = PS[4] if d == 0 else PS[7]
            pO = pS
            for c in ((0, 1) if d == 0 else (1, 0)):
                sl = slice(c * 64, c * 64 + 64)
                if t >= NTC:
                    for hv in range(2):
                        mm(pO[:, 256 + hv * 64:256 + (hv + 1) * 64], Sb[:, hv * 128:(hv + 1) * 128], W["qgT"][:, sl], True, False, [Sb.r(), W["qgT"].r()], [pO.r()], inc=False)
                        mm(pO[:, 256 + hv * 64:256 + (hv + 1) * 64], W["vtok"][sl, hv * 128:(hv + 1) * 128], W["attnT"][sl, sl], False, True, [W["vtok"].r(), W["attnT"].r()], [pO.r()])
                    o0 = (t - NTC) * 128 + c * 64
                    for hv in range(2):
                        if (o0, hv) not in owr:
                            owr.add((o0, hv))
                            kb.op("act", lambda e, o0=o0, hv=hv: e.activation(out=oT[:, hv * SEQ + o0:hv * SEQ + o0 + 64], in_=pO[:, 256 + hv * 64:256 + (hv + 1) * 64], func=AF.Copy), reads=[pO.r()], writes=[oT.r()])
                        else:
                            kb.op("dve", lambda e, o0=o0, hv=hv: e.tensor_tensor(out=oT[:, hv * SEQ + o0:hv * SEQ + o0 + 64], in0=pO[:, 256 + hv * 64:256 + (hv + 1) * 64], in1=oT[:, hv * SEQ + o0:hv * SEQ + o0 + 64], op=ALU.add),
                                  reads=[pO.r(), oT.r()], writes=[oT.r()])
                mm(pS[:, 0:256], W["kd"][sl, :], W["vtok"][sl, :], True, True, [W["kd"].r(), W["vtok"].r()], [pS.r()])
                kb.op("dve", lambda e, c=c: e.scalar_tensor_tensor(out=Sf[:, :], in0=Sf[:, :], scalar=W["glc"][:, c:c + 1], in1=pS[:, 0:256], op0=ALU.mult, op1=ALU.add),
                      reads=[Sf.r(), W["glc"].r(), pS.r()], writes=[Sf.r()])
                kb.op("act", lambda e: e.activation(out=Sb[:, :], in_=Sf[:, :], func=AF.Copy), reads=[Sf.r()], writes=[Sb.r()])
                yield

        yield
        for _ in pre_gen(0, order[0]):
            yield
        for idx, t in enumerate(order):
            nxt = pre_gen(idx + 1, order[idx + 1]) if idx + 1 < len(order) else None
            gens = [g_ for g_ in (scan_gen(idx, t), nxt) if g_ is not None]
            while gens:
                for g_ in list(gens):
                    try:
                        next(g_)
                        yield
                    except StopIteration:
                        gens.remove(g_)

    T = NB * SEQ
    TT_ = T // 128
    BLK = 512
    NBLK = -(-(T * TOPK + N_EXP * (BLK - 1)) // BLK)
    NSLOT = NBLK * BLK
    x1_d = nc.dram_tensor("x1_s", [T, D], F32, kind="Internal").ap()
    h2_d = nc.dram_tensor("h2_s", [T + 1, D], BF16, kind="Internal").ap()
    ys_d = nc.dram_tensor("ys_s", [NSLOT, D], BF16, kind="Internal").ap()
    sinfo_d = nc.dram_tensor("sinfo_s", [NSLOT, 2], I32, kind="Internal").ap()
    x1R, h2R, ysR, siR = kb.res("x1"), kb.res("h2"), kb.res("ys"), kb.res("si")
    lg_all = sb("lg_all", [128, TT_, N_EXP], F32)
    wr_sb = sb("wr_sb", [128, KC, N_EXP], F32)
    kb.dma("sp", lambda q: q.dma_start(out=wr_sb[:, :, :], in_=wr_d.rearrange("(kc p) n -> p kc n", p=128)), writes=[wr_sb.r()])
    brrow = sb("brrow", [1, N_EXP], F32)
    kb.dma("sp", lambda q: q.dma_start(out=brrow[0:1, :], in_=br_d), writes=[brrow.r()])
    br_bc = sb("br_bc", [128, N_EXP], F32)
    row_bcast(brrow[0:1, :], N_EXP, PS[1], br_bc[:, :], [brrow.r()], [br_bc.r()])

    ym_d = nc.dram_tensor("ym_s", [NB, D, SEQ], BF16, kind="Internal").ap()
    ymR = kb.res("ym_dram")

    def merge_seq(b):
        wo = sb("wo", [128, KC, D], BF16)
        for kc in range(KC):
            kb.dma("pool", lambda q, kc=kc: q.dma_start(out=wo[:, kc, :], in_=wo_d[kc * 128:(kc + 1) * 128, :]), writes=[wo.r()])
        woa_v = woa_d.rearrange("(kc p) n -> p kc n", p=128)
        wob_v = wob_d.rearrange("(kc p) n -> p kc n", p=128)
        mbc = sb("mbc", [128, 3 * D], F32)
        kb.dma("sp", lambda q: q.dma_start(out=mbc[:, :], in_=modbc_d[b, :, 0:3 * D]), reads=[], writes=[mbc.r()])
        ya_v = ya_d[b].rearrange("(kc p) t -> p kc t", p=128)
        yb_v = yb_d[b].rearrange("(kc p) t -> p kc t", p=128)
        ym_v = ym_d[b].rearrange("(kc p) t -> p kc t", p=128)
        with scope():
            wmm = [sb("wmm%d" % i, [128, KC, 4, 128], BF16) for i in range(2)]
            ya_sb = [sb("ya_sb%d" % i, [128, KC, 512], BF16) for i in range(2)]
            yb_sb = [sb("yb_sb%d" % i, [128, KC, 512], BF16) for i in range(2)]
            ga = sb("ga", [128, 512], F32)
            gb = sb("gb", [128, 512], F32)
            t1 = sb("t1", [128, 512], F32)
            t2 = sb("t2", [128, 512], F32)
            ymc = [sb("ymc%d" % i, [128, 512], BF16) for i in range(2)]
            it = 0
            for m in range(KC):
                wm_ = wmm[m % 2]
                kb.dma("pool", lambda q, m=m, wm_=wm_: q.dma_start(out=wm_[:, :, 0, :], in_=win_v[:, :, OFF_GATE + m * 128:OFF_GATE + (m + 1) * 128]), writes=[wm_.r()])
                kb.dma("pool", lambda q, m=m, wm_=wm_: q.dma_start(out=wm_[:, :, 1, :], in_=win_v[:, :, OFF_GATE + D + m * 128:OFF_GATE + D + (m + 1) * 128]), writes=[wm_.r()])
                kb.dma("pool", lambda q, m=m, wm_=wm_: q.dma_start(out=wm_[:, :, 2, :], in_=woa_v[:, :, m * 128:(m + 1) * 128]), writes=[wm_.r()])
                kb.dma("pool", lambda q, m=m, wm_=wm_: q.dma_start(out=wm_[:, :, 3, :], in_=wob_v[:, :, m * 128:(m + 1) * 128]), writes=[wm_.r()])
                for (t0, n) in LTB:
                    ya_, yb_, ymc_ = ya_sb[it % 2], yb_sb[it % 2], ymc[it % 2]
                    it += 1
                    kb.dma("sp", lambda q, t0=t0, n=n, ya_=ya_: q.dma_start(out=ya_[:, :, 0:n], in_=ya_v[:, :, t0:t0 + n]), reads=[yaR], writes=[ya_.r()])
                    kb.dma("sp", lambda q, t0=t0, n=n, yb_=yb_: q.dma_start(out=yb_[:, :, 0:n], in_=yb_v[:, :, t0:t0 + n]), reads=[ybR], writes=[yb_.r()])
                    hr = hT_reads(CTX + t0, n)
                    pa, pb, pga, pgb = PS[4], PS[5], PS[6], PS[7]
                    for kc in range(KC):
                        mm(pga[:, 0:n], wm_[:, kc, 0, :], hT[:, kc, CTX + t0:CTX + t0 + n], kc == 0, kc == KC - 1, hr + [wm_.r()], [pga.r()])
                    for kc in range(KC):
                        mm(pgb[:, 0:n], wm_[:, kc, 1, :], hT[:, kc, CTX + t0:CTX + t0 + n], kc == 0, kc == KC - 1, hr + [wm_.r()], [pgb.r()])
                    for kc in range(KC):
                        mm(pa[:, 0:n], wm_[:, kc, 2, :], ya_[:, kc, 0:n], kc == 0, kc == KC - 1, [wm_.r(), ya_.r()], [pa.r()])
                    for kc in range(KC):
                        mm(pb[:, 0:n], wm_[:, kc, 3, :], yb_[:, kc, 0:n], kc == 0, kc == KC - 1, [wm_.r(), yb_.r()], [pb.r()])
                    kb.op("act", lambda e, n=n, pga=pga: e.activation(out=ga[:, 0:n], in_=pga[:, 0:n], func=AF.Sigmoid), reads=[pga.r()], writes=[ga.r()])
                    kb.op("act", lambda e, n=n, pgb=pgb: e.activation(out=gb[:, 0:n], in_=pgb[:, 0:n], func=AF.Sigmoid), reads=[pgb.r()], writes=[gb.r()])
                    kb.op("dve", lambda e, n=n, pa=pa: e.tensor_tensor(out=t1[:, 0:n], in0=pa[:, 0:n], in1=ga[:, 0:n], op=ALU.mult), reads=[pa.r(), ga.r()], writes=[t1.r()])
                    kb.op("dve", lambda e, n=n, pb=pb: e.tensor_tensor(out=t2[:, 0:n], in0=pb[:, 0:n], in1=gb[:, 0:n], op=ALU.mult), reads=[pb.r(), gb.r()], writes=[t2.r()])
                    kb.op("pool", lambda e, n=n, ymc_=ymc_: e.tensor_tensor(out=ymc_[:, 0:n], in0=t1[:, 0:n], in1=t2[:, 0:n], op=ALU.add), reads=[t1.r(), t2.r()], writes=[ymc_.r()])
                    kb.dma("sp", lambda q, m=m, t0=t0, n=n, ymc_=ymc_: q.dma_start(out=ym_d[b, m * 128:(m + 1) * 128, t0:t0 + n], in_=ymc_[:, 0:n]), reads=[ymc_.r()], writes=[ymR])
        ymT2 = [sb("ymT%d" % i, [128, KC, 512], BF16) for i in range(2)]
        xr = sb("xr", [128, D], F32)
        x1t = sb("x1t", [128, D], F32)
        h2f = sb("h2f", [128, D], F32)
        h2b = sb("h2b", [128, D], BF16)
        h2T = sb("h2T", [128, D], F32)
        jk = sb("jk", [128, D], BF16)
        st = sb("st", [128, 4], F32)
        for bi, (t0, n) in enumerate(LTB):
            ymT = ymT2[bi % 2]
            kb.dma("sp", lambda q, t0=t0, n=n, ymT=ymT: q.dma_start(out=ymT[:, :, 0:n], in_=ym_v[:, :, t0:t0 + n]), reads=[ymR], writes=[ymT.r()])
            for tt in range(n // 128):
                tok0 = t0 + tt * 128
                gt = b * SEQ + tok0
                tile_i = gt // 128
                kb.dma("sp", lambda q, tok0=tok0: q.dma_start(out=xr[:, :], in_=x_d[b, tok0:tok0 + 128, :]), writes=[xr.r()])
                for half in range(2):
                    ps = PS[2 + half]
                    for kc in range(KC):
                        mm(ps[:, :], ymT[:, kc, tt * 128:(tt + 1) * 128], wo[:, kc, half * 512:(half + 1) * 512], kc == 0, kc == KC - 1, [ymT.r(), wo.r()], [ps.r()])
                    kb.op("dve", lambda e, ps=ps, half=half: e.tensor_tensor(out=x1t[:, half * 512:(half + 1) * 512], in0=ps[:, :], in1=mbc[:, half * 512:(half + 1) * 512], op=ALU.mult),
                          reads=[ps.r(), mbc.r()], writes=[x1t.r()])
                kb.op("pool", lambda e: e.tensor_tensor(out=x1t[:, :], in0=x1t[:, :], in1=xr[:, :], op=ALU.add), reads=[x1t.r(), xr.r()], writes=[x1t.r()])
                kb.dma("sp", lambda q, gt=gt: q.dma_start(out=x1_d[gt:gt + 128, :], in_=x1t[:, :]), reads=[x1t.r()], writes=[x1R])
                kb.op("act", lambda e: e.activation(out=jk[:, :], in_=x1t[:, :], func=AF.Square, accum_out=st[:, 0:1]), reads=[x1t.r()], writes=[jk.r(), st.r()])
                kb.op("act", lambda e: e.activation(out=st[:, 1:2], in_=st[:, 0:1], func=AF.Sqrt, scale=1.0 / D, bias=EPS), reads=[st.r()], writes=[st.r()])
                kb.op("dve", lambda e: e.reciprocal(out=st[:, 2:3], in_=st[:, 1:2]), reads=[st.r()], writes=[st.r()])
                kb.op("dve", lambda e: e.scalar_tensor_tensor(out=h2f[:, :], in0=x1t[:, :], scalar=st[:, 2:3], in1=mbc[:, 2 * D:3 * D], op0=ALU.mult, op1=ALU.mult),
                      reads=[x1t.r(), st.r(), mbc.r()], writes=[h2f.r()])
                kb.op("pool", lambda e: e.tensor_tensor(out=h2f[:, :], in0=h2f[:, :], in1=mbc[:, D:2 * D], op=ALU.add), reads=[h2f.r(), mbc.r()], writes=[h2f.r()])
                kb.op("act", lambda e: e.activation(out=h2b[:, :], in_=h2f[:, :], func=AF.Copy), reads=[h2f.r()], writes=[h2b.r()])
                kb.dma("sp", lambda q, gt=gt: q.dma_start(out=h2_d[gt:gt + 128, :], in_=h2b[:, :]), reads=[h2b.r()], writes=[h2R])
                for half in range(2):
                    ps = PS[half]
                    for j in range(4):
                        kc = half * 4 + j
                        kb.op("pe", lambda e, ps=ps, j=j, kc=kc: e.transpose(ps[:, j * 128:(j + 1) * 128], h2f[:, kc * 128:(kc + 1) * 128], C("ident")),
                              reads=[h2f.r()] + CR, writes=[ps.r()], inc=(j == 3))
                    kb.op("act" if half == 0 else "dve", (lambda e, ps=ps, half=half: e.activation(out=h2T[:, half * 512:(half + 1) * 512], in_=ps[:, :], func=AF.Copy)) if half == 0 else
                          (lambda e, ps=ps, half=half: e.tensor_copy(out=h2T[:, half * 512:(half + 1) * 512], in_=ps[:, :])), reads=[ps.r()], writes=[h2T.r()])
                ps = PS[2]
                for kc in range(KC):
                    mm(ps[:, 0:N_EXP], h2T[:, kc * 128:(kc + 1) * 128], wr_sb[:, kc, :], kc == 0, kc == KC - 1, [h2T.r(), wr_sb.r()], [ps.r()])
                kb.op("dve", lambda e, ps=ps, tile_i=tile_i: e.tensor_tensor(out=lg_all[:, tile_i, :], in0=ps[:, 0:N_EXP], in1=br_bc[:, :], op=ALU.add), reads=[ps.r(), br_bc.r()], writes=[lg_all.r(tile_i)])

    seq_scope = scope()
    seq_scope.__enter__()
    hT = sb("hT", [128, KC, S], BF16)
    Gtok = sb("Gtok", [128, NT, 32], F32)
    for b in range(NB):
        with scope():
            stage1(b)
        if bis == 7:
            return early()
        stage2_small(b)
        if bis == 8:
            return early()
        if b == 0:
            dbg_out("hT", hT[:, 0, :], [hT.r((0, t)) for t in range(NT)])
            dbg_out("Gtok", Gtok[:, :, :], [Gtok.r(t) for t in range(NT)])
        if stop <= 1:
            continue
        with scope():
            gdn_all(b)
        if stop <= 2:
            continue
        with scope():
            gla_all(b)
        if stop <= 3:
            continue
        with scope():
            merge_seq(b)
    seq_scope.__exit__()
    dbg_out("lg", lg_all[:, :, :], [lg_all.r(i) for i in range(TT_)])
    if "x1" in dbg_d:
        kb.out_tokens.append(kb.dma("sp", lambda q: q.dma_start(out=dbg_d["x1"], in_=x1_d), reads=[x1R]))
    if stop <= 4:
        return finish()
    NMC = 128 + 8 + NBLK
    mc = sb("mc", [128, NMC], F32)
    kb.dma("sp", lambda q: q.dma_start(out=mc[:, :], in_=moec_d), writes=[mc.r()])
    ustr = sb("ustr", [128, 128], BF16)
    kb.op("dve", lambda e: e.tensor_copy(out=ustr[:, :], in_=mc[:, 0:128]), reads=[mc.r()], writes=[ustr.r()])
    tokid = sb("tokid", [128, TT_], I32)
    kb.dma("sp", lambda q: q.dma_start(out=tokid[:, :], in_=tokid_d), writes=[tokid.r()])
    dsl_i = sb("dsl_i", [128, TT_ * 4], I32)
    be_bc = sb("be_bc", [128, NBLK], F32)
    be_i = sb("be_i", [128, NBLK], I32)
    scA = scope()
    scA.__enter__()
    sel_all = sb("sel_all", [128, TT_, N_EXP], F32)
    P_all = sb("P_all", [128, TT_, N_EXP], F32)
    dest_all = sb("dest_all", [128, TT_, N_EXP], F32)
    top8 = sb("top8", [128, TT_, 8], F32)
    dsl = sb("dsl", [128, TT_ * 4], F32)
    pk = sb("pk", [128, TT_ * 4], F32)
    run = sb("run", [128, N_EXP], F32)
    t32 = sb("t32", [128, N_EXP], F32)
    selb = sb("selb", [128, N_EXP], BF16)
    den = sb("den", [128, 2], F32)
    kb.op("dve", lambda e: e.memset(run[:, :], 0.0), writes=[run.r()])
    for ti in range(TT_):
        lg = lg_all[:, ti, :]
        rl = [lg_all.r(ti)]
        kb.op("dve", lambda e, ti=ti, lg=lg: e.max(out=top8[:, ti, :], in_=lg), reads=rl, writes=[top8.r(ti)])
        kb.op("dve", lambda e, ti=ti, lg=lg: e.tensor_scalar(out=sel_all[:, ti, :], in0=lg, scalar1=top8[:, ti, 3:4], scalar2=None, op0=ALU.is_ge), reads=rl + [top8.r(ti)], writes=[sel_all.r(ti)])
        kb.op("dve", lambda e, ti=ti, lg=lg: e.tensor_scalar(out=t32[:, :], in0=lg, scalar1=top8[:, ti, 0:1], scalar2=None, op0=ALU.subtract), reads=rl + [top8.r(ti)], writes=[t32.r()])
        kb.op("act", lambda e: e.activation(out=t32[:, :], in_=t32[:, :], func=AF.Exp), reads=[t32.r()], writes=[t32.r()])
        kb.op("dve", lambda e, ti=ti: e.tensor_tensor(out=t32[:, :], in0=t32[:, :], in1=sel_all[:, ti, :], op=ALU.mult), reads=[t32.r(), sel_all.r(ti)], writes=[t32.r()])
        kb.op("dve", lambda e: e.reduce_sum(out=den[:, 0:1], in_=t32[:, :], axis=AX.X), reads=[t32.r()], writes=[den.r()])
        kb.op("dve", lambda e: e.reciprocal(out=den[:, 1:2], in_=den[:, 0:1]), reads=[den.r()], writes=[den.r()])
        kb.op("dve", lambda e, ti=ti: e.tensor_scalar(out=P_all[:, ti, :], in0=t32[:, :], scalar1=den[:, 1:2], scalar2=None, op0=ALU.mult), reads=[t32.r(), den.r()], writes=[P_all.r(ti)])
        kb.op("act", lambda e, ti=ti: e.activation(out=selb[:, :], in_=sel_all[:, ti, :], func=AF.Copy), reads=[sel_all.r(ti)], writes=[selb.r()])
        ps = PS[ti % 2]
        mm(ps[:, 0:32], ustr[:, :], selb[:, :], True, True, [ustr.r(), selb.r()], [ps.r()])
        mm(ps[:, 32:64], C("ones", bf=True), selb[:, :], True, True, [selb.r()] + CRB, [ps.r()])
        kb.op("dve", lambda e, ti=ti, ps=ps: e.tensor_tensor(out=dest_all[:, ti, :], in0=ps[:, 0:32], in1=run[:, :], op=ALU.add), reads=[ps.r(), run.r()], writes=[dest_all.r(ti)])
        kb.op("dve", lambda e, ps=ps: e.tensor_tensor(out=run[:, :], in0=ps[:, 32:64], in1=run[:, :], op=ALU.add), reads=[ps.r(), run.r()], writes=[run.r()])
    padded = sb("padded", [128, N_EXP], F32)
    pstart = sb("pstart", [128, N_EXP], F32)
    pend = sb("pend", [128, N_EXP], F32)
    padT = sb("padT", [32, 128], F32)
    pendT = sb("pendT", [32, 128], F32)
    cmpb = sb("cmpb", [32, NBLK], F32)
    kb.op("dve", lambda e: e.tensor_scalar(out=padded[:, :], in0=run[:, :], scalar1=0.0, scalar2=float(BLK), op0=ALU.is_gt, op1=ALU.mult), reads=[run.r()], writes=[padded.r()])
    for j_ in range(1, -(-T // BLK)):
        kb.op("dve", lambda e, j_=j_: e.tensor_scalar(out=t32[:, :], in0=run[:, :], scalar1=float(j_ * BLK), scalar2=float(BLK), op0=ALU.is_gt, op1=ALU.mult), reads=[run.r()], writes=[t32.r()])
        kb.op("dve", lambda e: e.tensor_tensor(out=padded[:, :], in0=padded[:, :], in1=t32[:, :], op=ALU.add), reads=[padded.r(), t32.r()], writes=[padded.r()])
    ps = PS[0]
    kb.op("pe", lambda e: e.transpose(ps[0:32, 0:128], padded[:, :], C("ident")), reads=[padded.r()] + CR, writes=[ps.r()])
    kb.op("dve", lambda e: e.tensor_copy(out=padT[:, :], in_=ps[0:32, 0:128]), reads=[ps.r()], writes=[padT.r()])
    mm(ps[:, 0:32], padT[0:32, :], C("tri_f", rows=slice(0, 32), cols=slice(0, 32)), True, True, [padT.r()] + CR, [ps.r()])
    kb.op("dve", lambda e: e.tensor_copy(out=pend[:, :], in_=ps[:, 0:32]), reads=[ps.r()], writes=[pend.r()])
    kb.op("dve", lambda e: e.tensor_tensor(out=pstart[:, :], in0=pend[:, :], in1=padded[:, :], op=ALU.subtract), reads=[pend.r(), padded.r()], writes=[pstart.r()])
    kb.op("pe", lambda e: e.transpose(ps[0:32, 0:128], pend[:, :], C("ident")), reads=[pend.r()] + CR, writes=[ps.r()])
    kb.op("dve", lambda e: e.tensor_copy(out=pendT[:, :], in_=ps[0:32, 0:128]), reads=[ps.r()], writes=[pendT.r()])
    kb.op("dve", lambda e: e.tensor_scalar(out=cmpb[:, :], in0=mc[0:32, 136:136 + NBLK], scalar1=pendT[0:32, 0:1], scalar2=None, op0=ALU.is_ge), reads=[mc.r(), pendT.r()], writes=[cmpb.r()])
    mm(ps[:, 0:NBLK], C("ones", rows=slice(0, 32)), cmpb[0:32, :], True, True, [cmpb.r()] + CR, [ps.r()])
    kb.op("dve", lambda e: e.tensor_scalar(out=be_bc[:, :], in0=ps[:, 0:NBLK], scalar1=float(N_EXP - 1), scalar2=None, op0=ALU.min), reads=[ps.r()], writes=[be_bc.r()])
    kb.op("dve", lambda e: e.tensor_copy(out=be_i[:, :], in_=be_bc[:, :]), reads=[be_bc.r()], writes=[be_i.r()])
    kb.op("dve", lambda e: e.tensor_scalar(out=be_bc[:, :], in0=be_bc[:, :], scalar1=float(D), scalar2=None, op0=ALU.mult), reads=[be_bc.r(), be_i.r()], writes=[be_bc.r()])
    for ti in range(TT_):
        kb.op("dve", lambda e, ti=ti: e.tensor_tensor(out=dest_all[:, ti, :], in0=dest_all[:, ti, :], in1=pstart[:, :], op=ALU.add), reads=[dest_all.r(ti), pstart.r()], writes=[dest_all.r(ti)])
        for k in range(TOPK):
            kb.op("dve", lambda e, ti=ti, k=k: e.scalar_tensor_tensor(out=t32[:, :], in0=lg_all[:, ti, :], scalar=top8[:, ti, k:k + 1], in1=dest_all[:, ti, :], op0=ALU.is_equal, op1=ALU.mult,
                                                                      accum_out=dsl[:, ti * 4 + k:ti * 4 + k + 1]), reads=[lg_all.r(ti), top8.r(ti), dest_all.r(ti)], writes=[t32.r(), dsl.r()])
            kb.op("dve", lambda e, ti=ti, k=k: e.scalar_tensor_tensor(out=t32[:, :], in0=lg_all[:, ti, :], scalar=top8[:, ti, k:k + 1], in1=P_all[:, ti, :], op0=ALU.is_equal, op1=ALU.mult,
                                                                      accum_out=pk[:, ti * 4 + k:ti * 4 + k + 1]), reads=[lg_all.r(ti), top8.r(ti), P_all.r(ti)], writes=[t32.r(), pk.r()])
    kb.op("dve", lambda e: e.tensor_copy(out=dsl_i[:, :], in_=dsl[:, :]), reads=[dsl.r()], writes=[dsl_i.r()])
    stok_d = nc.dram_tensor("stok_s", [NSLOT, 1], I32, kind="Internal").ap()
    sp_d = nc.dram_tensor("sp_s", [NSLOT, 1], F32, kind="Internal").ap()
    ini_i = sb("ini_i", [128, NSLOT // 128], I32)
    ini_f = sb("ini_f", [128, NSLOT // 128], F32)
    zrow = sb("zrow", [1, D], BF16)
    kb.op("dve", lambda e: e.memset(ini_i[:, :], T), writes=[ini_i.r()])
    kb.op("dve", lambda e: e.memset(ini_f[:, :], 0.0), writes=[ini_f.r()])
    kb.op("dve", lambda e: e.memset(zrow[:, :], 0.0), writes=[zrow.r()])
    kb.dma("sp", lambda q: q.dma_start(out=stok_d.rearrange("(p f) o -> p (f o)", p=128), in_=ini_i[:, :]), reads=[ini_i.r()], writes=[siR])
    kb.dma("sp", lambda q: q.dma_start(out=sp_d.rearrange("(p f) o -> p (f o)", p=128), in_=ini_f[:, :]), reads=[ini_f.r()], writes=[siR])
    kb.dma("sp", lambda q: q.dma_start(out=h2_d[T:T + 1, :], in_=zrow[0:1, :]), reads=[zrow.r()], writes=[h2R])
    IOA = bass.IndirectOffsetOnAxis
    for ti in range(TT_):
        for k in range(TOPK):
            c_ = ti * 4 + k
            kb.dma("pool", lambda q, ti=ti, c_=c_: q.indirect_dma_start(out=stok_d[:, :], out_offset=IOA(ap=dsl_i[:, c_:c_ + 1], axis=0), in_=tokid[:, ti:ti + 1], in_offset=None),
                   reads=[dsl_i.r(), tokid.r(), siR], writes=[siR])
            kb.dma("pool", lambda q, c_=c_: q.indirect_dma_start(out=sp_d[:, :], out_offset=IOA(ap=dsl_i[:, c_:c_ + 1], axis=0), in_=pk[:, c_:c_ + 1], in_offset=None),
                   reads=[dsl_i.r(), pk.r(), siR], writes=[siR])

    scA.__exit__()
    scB = scope()
    scB.__enter__()
    wgu_sb = [sb("wgu%d" % i, [128, KC, 2 * D], BF16) for i in range(2)]
    wdn_sb = [sb("wdn%d" % i, [128, KC, D], BF16) for i in range(2)]
    idxf = sb("idxf", [128, KC], F32)
    idxw = [sb("idxw%d" % i, [128, KC], I32) for i in range(2)]
    bgrow = [sb("bgrow", [2, 2 * D], F32)] * 2
    bdrow = [sb("bdrow", [2, D], F32)] * 2
    bcol = sb("bcol", [128, 16], F32)
    bdn_bc = sb("bdn_bc", [128, D], F32)
    si_t = [sb("si_t%d" % i, [128, 1], I32) for i in range(4)]
    sp_t = [sb("sp_t%d" % i, [128, 1], F32) for i in range(4)]
    xb = [sb("xb%d" % i, [128, D], BF16) for i in range(2)]
    xbT = sb("xbT", [128, KC, BLK], BF16)
    actT = sb("actT", [128, KC, BLK], BF16)
    gg_ = [sb("gg%d" % i_, [128, BLK], F32) for i_ in range(2)]
    ll_ = [sb("ll%d" % i_, [128, BLK], F32) for i_ in range(2)]
    sg_ = [sb("sg%d" % i_, [128, BLK], F32) for i_ in range(2)]
    yv = [sb("yv%d" % i, [128, D], BF16) for i in range(2)]
    tdn = sb("tdn", [128, 512], F32)

    def load_weights(blk):
        i = blk % 2
        kb.op("dve", lambda e: e.tensor_scalar(out=idxf[:, :], in0=mc[:, 128:136], scalar1=be_bc[:, blk:blk + 1], scalar2=None, op0=ALU.add), reads=[mc.r(), be_bc.r()], writes=[idxf.r()])
        kb.op("dve", lambda e: e.tensor_copy(out=idxw[i][:, :], in_=idxf[:, :]), reads=[idxf.r()], writes=[idxw[i].r()])
        for kc in range(KC):
            kb.dma("pool", lambda q, kc=kc: q.indirect_dma_start(out=wgu_sb[i][:, kc, :], out_offset=None, in_=wgu_d[:, :], in_offset=IOA(ap=idxw[i][:, kc:kc + 1], axis=0)),
                   reads=[idxw[i].r()], writes=[wgu_sb[i].r()])
            kb.dma("pool", lambda q, kc=kc: q.indirect_dma_start(out=wdn_sb[i][:, kc, :], out_offset=None, in_=wdn_d[:, :], in_offset=IOA(ap=idxw[i][:, kc:kc + 1], axis=0)),
                   reads=[idxw[i].r()], writes=[wdn_sb[i].r()])
        kb.dma("pool", lambda q: q.indirect_dma_start(out=bgrow[i][0:2, :], out_offset=None, in_=bgu_d[:, :], in_offset=IOA(ap=be_i[0:2, blk:blk + 1], axis=0)),
               reads=[be_i.r()], writes=[bgrow[i].r()])
        kb.dma("pool", lambda q: q.indirect_dma_start(out=bdrow[i][0:2, :], out_offset=None, in_=bdn_d[:, :], in_offset=IOA(ap=be_i[0:2, blk:blk + 1], axis=0)),
               reads=[be_i.r()], writes=[bdrow[i].r()])

    load_weights(0)
    for blk in range(NBLK):
        i = blk % 2
        row_to_cols(lambda j: bgrow[i][0:1, j * 128:(j + 1) * 128], 16, PS[0], bcol[:, :], [bgrow[i].r()], [bcol.r()])
        kb.op("dve", lambda e: e.tensor_scalar(out=bcol[:, 8:16], in0=bcol[:, 8:16], scalar1=1.0, scalar2=None, op0=ALU.add), reads=[bcol.r()], writes=[bcol.r()])
        for half in range(2):
            row_bcast(bdrow[i][0:1, half * 512:(half + 1) * 512], 512, PS[1], bdn_bc[:, half * 512:(half + 1) * 512], [bdrow[i].r()], [bdn_bc.r()])
        for s_ in range(4):
            slot0 = blk * BLK + s_ * 128
            kb.dma("sp", lambda q, s_=s_, slot0=slot0: q.dma_start(out=si_t[s_][:, :], in_=stok_d[slot0:slot0 + 128, :]), reads=[siR], writes=[si_t[s_].r()])
            kb.dma("sp", lambda q, s_=s_, slot0=slot0: q.dma_start(out=sp_t[s_][:, :], in_=sp_d[slot0:slot0 + 128, :]), reads=[siR], writes=[sp_t[s_].r()])
            xb_ = xb[s_ % 2]
            kb.dma("pool", lambda q, s_=s_, xb_=xb_: q.indirect_dma_start(out=xb_[:, :], out_offset=None, in_=h2_d[:, :], in_offset=IOA(ap=si_t[s_][:, 0:1], axis=0)),
                   reads=[si_t[s_].r(), h2R], writes=[xb_.r()])
            ps = PS[2 + (s_ % 2)]
            psb = ps[:, :].bitcast(BF16)
            for kc in range(KC):
                kb.op("pe", lambda e, kc=kc, xb_=xb_, psb=psb: e.transpose(psb[:, kc * 128:(kc + 1) * 128], xb_[:, kc * 128:(kc + 1) * 128], C("ident", bf=True)),
                      reads=[xb_.r()] + CRB, writes=[ps.r()], inc=(kc == KC - 1))
            for kc in range(KC):
                if False:
                    pass
                else:
                    kb.op("dve", lambda e, kc=kc, s_=s_, psb=psb: e.tensor_copy(out=xbT[:, kc, s_ * 128:(s_ + 1) * 128], in_=psb[:, kc * 128:(kc + 1) * 128]), reads=[ps.r()], writes=[xbT.r()])
        if blk + 1 < NBLK:
            load_weights(blk + 1)
        for j in range(KC):
            pg, pl = PS[4 + 2 * (j % 2)], PS[5 + 2 * (j % 2)]
            gg, ll, sg = gg_[j % 2], ll_[j % 2], sg_[j % 2]
            for kc in range(KC):
                mm(pg[:, :], wgu_sb[i][:, kc, j * 128:(j + 1) * 128], xbT[:, kc, :], kc == 0, kc == KC - 1, [wgu_sb[i].r(), xbT.r()], [pg.r()])
            for kc in range(KC):
                mm(pl[:, :], wgu_sb[i][:, kc, D + j * 128:D + (j + 1) * 128], xbT[:, kc, :], kc == 0, kc == KC - 1, [wgu_sb[i].r(), xbT.r()], [pl.r()])
            kb.op("dve", lambda e, j=j, pg=pg: e.tensor_scalar(out=gg[:, :], in0=pg[:, :], scalar1=bcol[:, j:j + 1], scalar2=LIMIT, op0=ALU.add, op1=ALU.min), reads=[pg.r(), bcol.r()], writes=[gg.r()])
            kb.op("dve", lambda e, j=j, pl=pl: e.tensor_scalar(out=ll[:, :], in0=pl[:, :], scalar1=bcol[:, 8 + j:9 + j], scalar2=LIMIT + 1.0, op0=ALU.add, op1=ALU.min), reads=[pl.r(), bcol.r()], writes=[ll.r()])
            kb.op("act", lambda e: e.activation(out=sg[:, :], in_=gg[:, :], func=AF.Sigmoid, scale=ALPHA), reads=[gg.r()], writes=[sg.r()])
            kb.op("dve", lambda e: e.tensor_tensor(out=gg[:, :], in0=gg[:, :], in1=sg[:, :], op=ALU.mult), reads=[gg.r(), sg.r()], writes=[gg.r()])
            kb.op("dve", lambda e, j=j: e.scalar_tensor_tensor(out=actT[:, j, :], in0=ll[:, :], scalar=-(LIMIT - 1.0), in1=gg[:, :], op0=ALU.max, op1=ALU.mult), reads=[gg.r(), ll.r()], writes=[actT.r()])
        for s_ in range(4):
            slot0 = blk * BLK + s_ * 128
            yv_ = yv[s_ % 2]
            for half in range(2):
                ps = PS[2 + half]
                for kc in range(KC):
                    mm(ps[:, :], actT[:, kc, s_ * 128:(s_ + 1) * 128], wdn_sb[i][:, kc, half * 512:(half + 1) * 512], kc == 0, kc == KC - 1, [actT.r(), wdn_sb[i].r()], [ps.r()])
                kb.op("dve", lambda e, ps=ps, half=half: e.tensor_tensor(out=tdn[:, :], in0=ps[:, :], in1=bdn_bc[:, half * 512:(half + 1) * 512], op=ALU.add), reads=[ps.r(), bdn_bc.r()], writes=[tdn.r()])
                kb.op("act", lambda e, half=half, yv_=yv_, s_=s_: e.activation(out=yv_[:, half * 512:(half + 1) * 512], in_=tdn[:, :], func=AF.Copy, scale=sp_t[s_][:, 0:1]), reads=[tdn.r(), sp_t[s_].r()], writes=[yv_.r()])
            kb.dma("sp", lambda q, slot0=slot0, yv_=yv_: q.dma_start(out=ys_d[slot0:slot0 + 128, :], in_=yv_[:, :]), reads=[yv_.r()], writes=[ysR])

    scB.__exit__()
    g2bc = [sb("g2bc%d" % b, [128, D], F32) for b in range(NB)]
    for b in range(NB):
        kb.dma("sp", lambda q, b=b: q.dma_start(out=g2bc[b][:, :], in_=modbc_d[b, :, 3 * D:4 * D]), writes=[g2bc[b].r()])
    yk = [sb("yk%d" % k, [128, D], BF16) for k in range(TOPK)]
    acc1 = sb("acc1", [128, D], F32)
    acc2 = sb("acc2", [128, D], F32)
    x1r = sb("x1r", [128, D], F32)
    ot = sb("ot", [128, D], F32)
    jk2 = sb("jk2", [128, D], BF16)
    st2 = sb("st2", [128, 4], F32)
    for ti in range(TT_):
        b = (ti * 128) // SEQ
        tok0 = ti * 128 - b * SEQ
        for k in range(TOPK):
            c_ = ti * 4 + k
            kb.dma("pool", lambda q, k=k, c_=c_: q.indirect_dma_start(out=yk[k][:, :], out_offset=None, in_=ys_d[:, :], in_offset=IOA(ap=dsl_i[:, c_:c_ + 1], axis=0)),
                   reads=[dsl_i.r(), ysR], writes=[yk[k].r()])
        kb.dma("sp", lambda q, ti=ti: q.dma_start(out=x1r[:, :], in_=x1_d[ti * 128:(ti + 1) * 128, :]), reads=[x1R], writes=[x1r.r()])
        kb.op("dve", lambda e: e.tensor_tensor(out=acc1[:, :], in0=yk[0][:, :], in1=yk[1][:, :], op=ALU.add), reads=[yk[0].r(), yk[1].r()], writes=[acc1.r()])
        kb.op("pool", lambda e: e.tensor_tensor(out=acc2[:, :], in0=yk[2][:, :], in1=yk[3][:, :], op=ALU.add), reads=[yk[2].r(), yk[3].r()], writes=[acc2.r()])
        kb.op("dve", lambda e: e.tensor_tensor(out=acc1[:, :], in0=acc1[:, :], in1=acc2[:, :], op=ALU.add), reads=[acc1.r(), acc2.r()], writes=[acc1.r()])
        kb.op("dve", lambda e, b=b: e.tensor_tensor(out=acc1[:, :], in0=acc1[:, :], in1=g2bc[b][:, :], op=ALU.mult), reads=[acc1.r(), g2bc[b].r()], writes=[acc1.r()])
        kb.op("pool", lambda e: e.tensor_tensor(out=acc1[:, :], in0=acc1[:, :], in1=x1r[:, :], op=ALU.add), reads=[acc1.r(), x1r.r()], writes=[acc1.r()])
        kb.op("act", lambda e: e.activation(out=jk2[:, :], in_=acc1[:, :], func=AF.Square, accum_out=st2[:, 0:1]), reads=[acc1.r()], writes=[jk2.r(), st2.r()])
        kb.op("act", lambda e: e.activation(out=st2[:, 1:2], in_=st2[:, 0:1], func=AF.Sqrt, scale=1.0 / D, bias=EPS), reads=[st2.r()], writes=[st2.r()])
        kb.op("dve", lambda e: e.reciprocal(out=st2[:, 2:3], in_=st2[:, 1:2]), reads=[st2.r()], writes=[st2.r()])
        kb.op("dve", lambda e: e.scalar_tensor_tensor(out=ot[:, :], in0=acc1[:, :], scalar=st2[:, 2:3], in1=fng_bc[:, :], op0=ALU.mult, op1=ALU.mult), reads=[acc1.r(), st2.r(), fng_bc.r(0), fng_bc.r(1)], writes=[ot.r()])
        kb.out_tokens.append(kb.dma("sp", lambda q, b=b, tok0=tok0: q.dma_start(out=out_d[b, tok0:tok0 + 128, :], in_=ot[:, :]), reads=[ot.r()]))
    return finish()


def _prep_inputs(inp, cores, nb):
    f = lambda a: np.ascontiguousarray(np.asarray(a, dtype=np.float32))
    consts = make_consts()
    shared = {
        "c_ctx": f(inp["c_ctx"]).reshape(1, D),
        "w_mod": f(inp["w_mod"][0]),
        "b_mod": f(inp["b_mod"][0]).reshape(1, -1),
        "norm_mix_g": f(inp["norm_mix_g"][0]).reshape(1, D),
        "norm_ffn_g": f(inp["norm_ffn_g"][0]).reshape(1, D),
        "w_in": f(inp["w_in"][0]),
        "conv_w": f(inp["conv_w"][0]).reshape(9, -1),
        "a_log": np.concatenate([f(inp["a_log_f"][0]), f(inp["a_log_b"][0])]).reshape(1, 16),
        "dt_bias": np.concatenate([f(inp["dt_bias_f"][0]), f(inp["dt_bias_b"][0])]).reshape(1, 16),
        "gdn_norm_g": f(inp["gdn_norm_g"][0]).reshape(1, 128),
        "gla_gate_w": np.stack([f(inp["gla_gate_w_f"][0]), f(inp["gla_gate_w_b"][0])]),
        "gla_gate_b": np.stack([f(inp["gla_gate_b_f"][0]), f(inp["gla_gate_b_b"][0])]),
        "gla_norm_g": f(inp["gla_norm_g"][0]).reshape(1, 256),
        "w_out_a": f(inp["w_out_a"][0]),
        "w_out_b": f(inp["w_out_b"][0]),
        "w_out": f(inp["w_out"][0]),
        "w_router": f(inp["w_router"][0]),
        "b_router": f(inp["b_router"][0]).reshape(1, N_EXP),
        "w_gu": f(inp["w_gu"][0]).reshape(N_EXP * D, 2 * D),
        "b_gu": f(inp["b_gu"][0]),
        "w_down": f(inp["w_down"][0]).reshape(N_EXP * D, D),
        "b_down": f(inp["b_down"][0]),
        "final_norm_g": f(inp["final_norm_g"]).reshape(1, D),
        "consts": np.concatenate([consts[k] for k in CONST_ORDER], axis=1),
        "sel16": consts["sel16"],
    }
    T_ = nb * inp["x"].shape[1]
    nblk = -(-(T_ * TOPK + N_EXP * 511) // 512)
    mcn = np.zeros((128, 128 + 8 + nblk), np.float32)
    ii = np.arange(128)
    mcn[:, 0:128] = (ii[:, None] < ii[None, :])
    mcn[:, 128:136] = np.arange(8)[None, :] * 128 + ii[:, None]
    mcn[:, 136:] = np.arange(nblk)[None, :] * 512
    shared["moe_c"] = mcn
    shared["tokid"] = (np.arange(T_ // 128)[None, :] * 128 + ii[:, None]).astype(np.int32)
    maps = []
    for i in range(cores):
        m = dict(shared)
        m["x"] = f(inp["x"][i * nb:(i + 1) * nb])
        m["c"] = f(inp["c"][i * nb:(i + 1) * nb])
        m["ctx"] = f(inp["ctx"][i * nb:(i + 1) * nb])
        maps.append(m)
    return maps


def kernel(**inputs):
    n = 8
    B, SEQ = inputs["x"].shape[0], inputs["x"].shape[1]
    CTX = inputs["ctx"].shape[1]
    nb = B // n
    cfg = {"NB": nb, "SEQ": SEQ, "CTX": CTX, "GW": 64}
    nc = build(cfg)
    maps = _prep_inputs(inputs, n, nb)
    res = run_bass_kernel_spmd(nc, maps, core_ids=list(range(n)))
    return np.concatenate([r["out"] for r in res.results], axis=0)
```

```python
import numpy as np
from contextlib import ExitStack
import concourse.bass as bass
import concourse.mybir as mybir
from concourse.bass_utils import run_bass_kernel_spmd

F32 = mybir.dt.float32
BF16 = mybir.dt.bfloat16
I32 = mybir.dt.int32
AF = mybir.ActivationFunctionType
ALU = mybir.AluOpType
AX = mybir.AxisListType

D = 1024
KC = 8
EPS = 1e-6
NH_A = 8
NH_B = 4
N_EXP = 32
TOPK = 4
LIMIT = 7.0
ALPHA = 1.702
SEM_LIMIT = 30000
OFF_QA, OFF_KA, OFF_VA, OFF_ZA = 0, 1024, 2048, 3072
OFF_BETA, OFF_DEC = 4096, 4112
OFF_QB, OFF_KB, OFF_VB, OFF_RB = 4128, 4640, 5152, 6176
OFF_LR, OFF_GATE = 7200, 7232
D_IN = 9280


class Res:
    __slots__ = ("name", "w", "r")

    def __init__(self, name):
        self.name = name
        self.w = None
        self.r = {}


class Eng:
    def __init__(self, kb, name, obj):
        self.kb, self.name, self.obj = kb, name, obj
        self.sem = None
        self.cnt = 0
        self.seen = {}
        self.nsem = 0

    def new_sem(self):
        self.sem = self.kb.es.enter_context(self.kb.nc.semaphore("e_%s_%d" % (self.name, self.nsem)))
        self.nsem += 1
        self.cnt = 0


class DSem:
    def __init__(self, sem):
        self.sem = sem
        self.total = 0


class KB:
    def __init__(self, nc, es, n_dsem=40):
        self.nc, self.es = nc, es
        self.eng = {
            "pe": Eng(self, "pe", nc.tensor),
            "act": Eng(self, "act", nc.scalar),
            "dve": Eng(self, "dve", nc.vector),
            "pool": Eng(self, "pool", nc.gpsimd),
            "sp": Eng(self, "sp", nc.sync),
        }
        for e in self.eng.values():
            e.new_sem()
        self.dsems_q = {"sp": [DSem(es.enter_context(nc.semaphore("dsp%d" % i))) for i in range(24)],
                        "pool": [DSem(es.enter_context(nc.semaphore("dpl%d" % i))) for i in range(24)]}
        self.dsems = self.dsems_q["sp"] + self.dsems_q["pool"]
        self.di = 0
        self.dq = {"sp": 0, "pool": 0}
        self.nres = 0
        self.out_tokens = []

    def res(self, name=None):
        self.nres += 1
        return Res(name or "r%d" % self.nres)

    def _waits(self, e, reads, writes, is_dma):
        deps = []
        for r in reads:
            if r.w is not None:
                deps.append((r.w, True))
        for r in writes:
            if r.w is not None:
                deps.append((r.w, False))
            for x in r.r.values():
                deps.append((x, False))
        for (sem, val, src), raw in deps:
            if (src is e) and not is_dma:
                if e.name == "pe":
                    continue
                assert val <= e.cnt or sem is not e.sem, "self-wait on pending token"
            if e.seen.get(id(sem), 0) >= val:
                continue
            e.obj.wait_ge(sem, val)
            e.seen[id(sem)] = val

    def op(self, en, fn, reads=(), writes=(), inc=True):
        e = self.eng[en]
        if e.cnt >= SEM_LIMIT:
            e.new_sem()
        self._waits(e, reads, writes, False)
        ins = fn(e.obj)
        if inc:
            e.cnt += 1
            ins.then_inc(e.sem, 1)
            tok = (e.sem, e.cnt, e)
        else:
            tok = (e.sem, e.cnt + 1, e)
        for r in writes:
            r.w = tok
            r.r = {}
        for r in reads:
            r.r[en] = tok
        return tok

    def dma(self, qn, fn, reads=(), writes=()):
        q = self.eng[qn]
        self._waits(q, reads, writes, True)
        pool_ = self.dsems_q[qn]
        s = pool_[self.dq[qn] % len(pool_)]
        self.dq[qn] += 1
        self.di += 1
        if s.total > 0 and q.seen.get(id(s.sem), 0) < s.total:
            q.obj.wait_ge(s.sem, s.total)
            q.seen[id(s.sem)] = s.total
        ins = fn(q.obj)
        s.total += 16
        assert s.total < 60000
        ins.then_inc(s.sem, 16)
        tok = (s.sem, s.total, None)
        for r in writes:
            r.w = tok
            r.r = {}
        for r in reads:
            r.r["dma%d" % (self.di % 64)] = tok
        return tok

    def barrier(self):
        toks = [(e.sem, e.cnt, e) for e in self.eng.values() if e.cnt > 0]
        toks += [(ds.sem, ds.total, None) for ds in self.dsems if ds.total > 0]
        for e in self.eng.values():
            for tok in toks:
                if tok[2] is not e:
                    self.wait_tok(e.name, tok)

    def wait_tok(self, en, tok):
        e = self.eng[en]
        sem, val, _ = tok
        if e.seen.get(id(sem), 0) < val:
            e.obj.wait_ge(sem, val)
            e.seen[id(sem)] = val


class Buf:
    def __init__(self, kb, t, name, single=False):
        self.kb, self.t, self.name = kb, t, name
        self._r = {}
        self.single = single

    def r(self, key=0):
        if self.single:
            key = 0
        if key not in self._r:
            self._r[key] = self.kb.res("%s/%s" % (self.name, key))
        return self._r[key]

    def __getitem__(self, idx):
        return self.t[idx]


def make_consts():
    c = {}
    idx = np.arange(128)
    same = (idx[:, None] // 64) == (idx[None, :] // 64)
    c["ident"] = np.eye(128, dtype=np.float32)
    c["tri_f"] = (same & (idx[:, None] <= idx[None, :])).astype(np.float32)
    c["tri_b"] = (same & (idx[:, None] >= idx[None, :])).astype(np.float32)
    c["blk"] = same.astype(np.float32)
    c["neg_f"] = np.where(c["tri_f"] > 0, 0.0, -30000.0).astype(np.float32)
    c["neg_b"] = np.where(c["tri_b"] > 0, 0.0, -30000.0).astype(np.float32)
    c["offd"] = (1.0 - np.eye(128)).astype(np.float32)
    c["ones"] = np.ones((128, 128), np.float32)
    sel = np.zeros((16, 16 * 128), np.float32)
    for r in range(16):
        sel[r, r * 128:(r + 1) * 128] = 1.0
    c["sel16"] = sel
    return c


CONST_ORDER = ["ident", "tri_f", "tri_b", "blk", "neg_f", "neg_b", "offd", "ones"]


def build(cfg):
    NB, SEQ, CTX, GW = cfg["NB"], cfg["SEQ"], cfg["CTX"], cfg["GW"]
    S = CTX + SEQ
    NT, NTC, NTL = S // 128, CTX // 128, SEQ // 128
    ROWS = SEQ // GW
    dbg = cfg.get("dbg", ())
    stop = cfg.get("stop", 99)
    nc = bass.Bass("TRN2", target_bir_lowering=False)

    def din(name, shape, dt=F32):
        return nc.dram_tensor(name, list(shape), dt, kind="ExternalInput").ap()

    x_d = din("x", [NB, SEQ, D])
    c_d = din("c", [NB, D])
    ctx_d = din("ctx", [NB, CTX, D])
    cctx_d = din("c_ctx", [1, D])
    wmod_d = din("w_mod", [D, 6 * D])
    bmod_d = din("b_mod", [1, 6 * D])
    gmix_d = din("norm_mix_g", [1, D])
    gffn_d = din("norm_ffn_g", [1, D])
    win_d = din("w_in", [D, D_IN])
    convw_d = din("conv_w", [9, 3 * D])
    alog_d = din("a_log", [1, 16])
    dtb_d = din("dt_bias", [1, 16])
    gdng_d = din("gdn_norm_g", [1, 128])
    glaw_d = din("gla_gate_w", [2, 16, 512])
    glab_d = din("gla_gate_b", [2, 512])
    glag_d = din("gla_norm_g", [1, 256])
    woa_d = din("w_out_a", [D, D])
    wob_d = din("w_out_b", [D, D])
    wo_d = din("w_out", [D, D])
    wr_d = din("w_router", [D, N_EXP])
    br_d = din("b_router", [1, N_EXP])
    wgu_d = din("w_gu", [N_EXP * D, 2 * D])
    bgu_d = din("b_gu", [N_EXP, 2 * D])
    wdn_d = din("w_down", [N_EXP * D, D])
    bdn_d = din("b_down", [N_EXP, D])
    fng_d = din("final_norm_g", [1, D])
    cst_d = din("consts", [128, len(CONST_ORDER) * 128])
    sel_d = din("sel16", [16, 16 * 128])
    _T = NB * SEQ
    _NBLK = -(-(_T * TOPK + N_EXP * 511) // 512)
    moec_d = din("moe_c", [128, 128 + 8 + _NBLK])
    tokid_d = din("tokid", [128, _T // 128], I32)
    out_d = nc.dram_tensor("out", [NB, SEQ, D], F32, kind="ExternalOutput").ap()
    dbg_d = {}
    for name, shape in cfg.get("dbg_shapes", {}).items():
        dbg_d[name] = nc.dram_tensor("dbg_" + name, list(shape), F32, kind="ExternalOutput").ap()

    es = ExitStack()
    kb = KB(nc, es)

    stk = [es]
    uniq = [0]

    def sb(name, shape, dt):
        uniq[0] += 1
        return Buf(kb, stk[-1].enter_context(nc.sbuf_tensor("s%d_%s" % (uniq[0], name), list(shape), dt)), name)

    class scope:
        def __enter__(self):
            self.s = ExitStack()
            stk.append(self.s)
            return self

        def __exit__(self, *a):
            kb.barrier()
            stk.pop()
            self.s.close()
            return False

    PS = [Buf(kb, es.enter_context(nc.psum_tensor("ps%d" % i, [128, 512], F32)), "ps%d" % i, single=True) for i in range(8)]

    cst = sb("cst", [128, len(CONST_ORDER) * 128], F32)
    cstb = sb("cstb", [128, len(CONST_ORDER) * 128], BF16)
    sel16 = sb("sel16", [16, 16 * 128], BF16)
    kb.dma("sp", lambda q: q.dma_start(out=cst[:, :], in_=cst_d), writes=[cst.r()])
    kb.dma("pool", lambda q: q.dma_start(out=cstb[:, :], in_=cst_d), writes=[cstb.r()])
    kb.dma("pool", lambda q: q.dma_start(out=sel16[:, :], in_=sel_d), writes=[sel16.r()])

    def C(name, bf=False, rows=slice(0, 128), cols=None):
        i = CONST_ORDER.index(name)
        t = cstb if bf else cst
        if cols is None:
            return t[rows, i * 128:(i + 1) * 128]
        return t[rows, i * 128 + cols.start:i * 128 + cols.stop]

    CR = [cst.r()]
    CRB = [cstb.r()]

    def mm(out, lhsT, rhs, start, stop_, reads, writes, inc=None):
        if inc is None:
            inc = stop_
        return kb.op("pe", lambda e: e.matmul(out, lhsT=lhsT, rhs=rhs, start=start, stop=stop_),
                     reads=reads, writes=writes, inc=inc)

    def row_to_cols(row_ap_fn, ncols, ps, dst_ap, reads, writes):
        for j in range(ncols):
            mm(ps[:, j:j + 1], row_ap_fn(j), C("ones", rows=slice(0, 1), cols=slice(0, 1)), True, True,
               reads + CR, [ps.r()])
        kb.op("dve", lambda e: e.tensor_copy(out=dst_ap, in_=ps[:, 0:ncols]), reads=[ps.r()], writes=writes)

    def row_bcast(row_ap, n, ps, dst_ap, reads, writes, eng="dve"):
        mm(ps[:, 0:n], C("ones", rows=slice(0, 1)), row_ap, True, True, reads + CR, [ps.r()])
        if eng == "dve":
            kb.op("dve", lambda e: e.tensor_copy(out=dst_ap, in_=ps[:, 0:n]), reads=[ps.r()], writes=writes)
        else:
            kb.op("act", lambda e: e.activation(out=dst_ap, in_=ps[:, 0:n], func=AF.Copy), reads=[ps.r()], writes=writes)

    bis = cfg.get("bis", 0)

    def early():
        for ds in kb.dsems:
            if ds.total > 0:
                kb.wait_tok("sp", (ds.sem, ds.total, None))
        while len(stk) > 1:
            stk.pop().close()
        es.close()
        return nc

    if bis == 1:
        return early()
    NSRC = NB + 1
    modcol = sb("modcol", [128, NSRC * 2 * KC], F32)
    gsc1 = sb("gsc1", [128, NSRC * KC], F32)
    fng_bc = sb("fng_bc", [128, D], F32)
    modbc_d = nc.dram_tensor("modbc_s", [NB, 128, 4 * D], F32, kind="Internal").ap()
    sc0 = scope()
    sc0.__enter__()
    rows = sb("rows", [1, 3 * 1024], F32)
    crow = sb("crow", [1, NSRC * D], F32)
    for b in range(NB):
        kb.dma("sp", lambda q, b=b: q.dma_start(out=crow[0:1, b * D:(b + 1) * D], in_=c_d[b:b + 1, :]), writes=[crow.r(b)])
    kb.dma("sp", lambda q: q.dma_start(out=crow[0:1, NB * D:(NB + 1) * D], in_=cctx_d), writes=[crow.r(NB)])
    ccol = sb("ccol", [128, NSRC * KC], F32)
    scol = sb("scol", [128, NSRC * KC], BF16)
    for s_ in range(NSRC):
        row_to_cols(lambda j, s_=s_: crow[0:1, s_ * D + j * 128:s_ * D + (j + 1) * 128], KC, PS[0],
                    ccol[:, s_ * KC:(s_ + 1) * KC], [crow.r(s_)], [ccol.r(s_)])
        kb.op("act", lambda e, s_=s_: e.activation(out=scol[:, s_ * KC:(s_ + 1) * KC], in_=ccol[:, s_ * KC:(s_ + 1) * KC], func=AF.Silu),
              reads=[ccol.r(s_)], writes=[scol.r(s_)])
    if bis == 2:
        return early()
    bmod = sb("bmod", [1, 6 * D], F32)
    kb.dma("sp", lambda q: q.dma_start(out=bmod[0:1, :], in_=bmod_d), writes=[bmod.r()])
    kb.dma("sp", lambda q: q.dma_start(out=rows[0:1, 0:D], in_=gmix_d), writes=[rows.r(0)])
    kb.dma("sp", lambda q: q.dma_start(out=rows[0:1, D:2 * D], in_=gffn_d), writes=[rows.r(1)])
    kb.dma("sp", lambda q: q.dma_start(out=rows[0:1, 2 * D:3 * D], in_=fng_d), writes=[rows.r(2)])
    gmixc = sb("gmixc", [128, KC], F32)
    row_to_cols(lambda j: rows[0:1, j * 128:(j + 1) * 128], KC, PS[0], gmixc[:, :], [rows.r(0)], [gmixc.r()])
    gffn_bc = sb("gffn_bc", [128, D], F32)
    for h in range(2):
        row_bcast(rows[0:1, D + h * 512:D + (h + 1) * 512], 512, PS[1], gffn_bc[:, h * 512:(h + 1) * 512], [rows.r(1)], [gffn_bc.r(h)])
        row_bcast(rows[0:1, 2 * D + h * 512:2 * D + (h + 1) * 512], 512, PS[1], fng_bc[:, h * 512:(h + 1) * 512], [rows.r(2)], [fng_bc.r(h)])

    modbc = [sb("modbc%d" % b, [128, 4 * D], F32) for b in range(NB)]
    wm = [sb("wm%d" % i, [128, KC, 512], BF16) for i in range(2)]
    mrow = [sb("mrow%d" % i, [1, 512], F32) for i in range(2)]
    wmod_v = wmod_d.rearrange("(kc p) n -> p kc n", p=128)
    for cb in range(12):
        w_ = wm[cb % 2]
        kb.dma("pool", lambda q, cb=cb, w_=w_: q.dma_start(out=w_[:, :, :], in_=wmod_v[:, :, cb * 512:(cb + 1) * 512]), writes=[w_.r()])
        v, half = cb // 2, cb % 2
        for s_ in range(NSRC):
            if v >= 2 and s_ == NB:
                continue
            ps = PS[2 + (s_ % 2)]
            for kc in range(KC):
                mm(ps[0:1, :], scol[:, s_ * KC + kc:s_ * KC + kc + 1], w_[:, kc, :], kc == 0, kc == KC - 1,
                   [scol.r(s_), w_.r()], [ps.r()])
            mr = mrow[s_ % 2]
            kb.op("dve", lambda e, ps=ps, mr=mr, cb=cb: e.tensor_tensor(out=mr[0:1, :], in0=ps[0:1, :], in1=bmod[0:1, cb * 512:(cb + 1) * 512], op=ALU.add),
                  reads=[ps.r(), bmod.r()], writes=[mr.r()])
            if v < 2:
                row_to_cols(lambda j, mr=mr: mr[0:1, j * 128:(j + 1) * 128], 4, PS[0],
                            modcol[:, s_ * 16 + v * 8 + half * 4:s_ * 16 + v * 8 + half * 4 + 4], [mr.r()], [modcol.r((s_, v, half))])
            else:
                row_bcast(mr[0:1, :], 512, PS[1], modbc[s_][:, (v - 2) * D + half * 512:(v - 2) * D + (half + 1) * 512],
                          [mr.r()], [modbc[s_].r((v - 2, half))])
    for s_ in range(NSRC):
        kb.op("dve", lambda e, s_=s_: e.scalar_tensor_tensor(out=gsc1[:, s_ * KC:(s_ + 1) * KC], in0=modcol[:, s_ * 16 + 8:s_ * 16 + 16], scalar=1.0,
                                                             in1=gmixc[:, :], op0=ALU.add, op1=ALU.mult),
              reads=[modcol.r((s_, 1, 0)), modcol.r((s_, 1, 1)), gmixc.r()], writes=[gsc1.r(s_)])
    for b in range(NB):
        for h in range(2):
            kb.op("dve", lambda e, b=b, h=h: e.scalar_tensor_tensor(out=modbc[b][:, 2 * D + h * 512:2 * D + (h + 1) * 512],
                                                                    in0=modbc[b][:, 2 * D + h * 512:2 * D + (h + 1) * 512], scalar=1.0,
                                                                    in1=gffn_bc[:, h * 512:(h + 1) * 512], op0=ALU.add, op1=ALU.mult),
                  reads=[modbc[b].r((2, h)), gffn_bc.r(h)], writes=[modbc[b].r((2, h))])

    def finish():
        for tok in kb.out_tokens:
            kb.wait_tok("sp", tok)
        for ds in kb.dsems:
            if ds.total > 0:
                kb.wait_tok("sp", (ds.sem, ds.total, None))
        es.close()
        return nc

    def dbg_out(name, src_ap, reads):
        if name in dbg_d:
            kb.out_tokens.append(kb.dma("pool", lambda q: q.dma_start(out=dbg_d[name], in_=src_ap), reads=reads))

    dbg_out("gsc1", gsc1[:, :], [gsc1.r(s_) for s_ in range(NSRC)])
    dbg_out("modbc0", modbc[0][:, :], [modbc[0].r((v, h)) for v in range(4) for h in range(2)])
    if bis == 3:
        return early()
    modbc_tok = []
    for b in range(NB):
        modbc_tok.append(kb.dma("sp", lambda q, b=b: q.dma_start(out=modbc_d[b], in_=modbc[b][:, :]),
                                reads=[modbc[b].r((v, h)) for v in range(4) for h in range(2)]))
    if bis == 4:
        return early()
    if bis == 5:
        kb.barrier()
        return early()
    sc0.__exit__()
    if stop <= 0:
        return finish()
    cwc = sb("cwc", [128, 24 * 9], F32)
    sc_cw = scope()
    sc_cw.__enter__()
    cwrow = sb("cwrow", [9, 3 * D], F32)
    kb.dma("sp", lambda q: q.dma_start(out=cwrow[:, :], in_=convw_d), writes=[cwrow.r()])
    for g_ in range(3):
        ps = PS[0]
        for j in range(8):
            ch = g_ * 8 + j
            mm(ps[:, j * 9:(j + 1) * 9], cwrow[0:9, ch * 128:(ch + 1) * 128], C("ident", rows=slice(0, 9), cols=slice(0, 9)), True, True,
               [cwrow.r()] + CR, [ps.r()])
        kb.op("dve", lambda e, g_=g_, ps=ps: e.tensor_copy(out=cwc[:, g_ * 72:(g_ + 1) * 72], in_=ps[:, 0:72]), reads=[ps.r()], writes=[cwc.r(g_)])
    sc_cw.__exit__()
    srow = sb("srow", [1, 32], F32)
    kb.dma("sp", lambda q: q.dma_start(out=srow[0:1, 0:16], in_=alog_d), writes=[srow.r(0)])
    kb.dma("sp", lambda q: q.dma_start(out=srow[0:1, 16:32], in_=dtb_d), writes=[srow.r(1)])
    gconst = sb("gconst", [128, 32], F32)
    row_bcast(srow[0:1, 0:32], 32, PS[1], gconst[:, :], [srow.r(0), srow.r(1)], [gconst.r()])
    kb.op("act", lambda e: e.activation(out=gconst[:, 0:16], in_=gconst[:, 0:16], func=AF.Exp), reads=[gconst.r()], writes=[gconst.r()])
    kb.op("dve", lambda e: e.tensor_scalar(out=gconst[:, 0:16], in0=gconst[:, 0:16], scalar1=-1.0, scalar2=None, op0=ALU.mult), reads=[gconst.r()], writes=[gconst.r()])
    nrow = sb("nrow", [1, 384], F32)
    kb.dma("sp", lambda q: q.dma_start(out=nrow[0:1, 0:128], in_=gdng_d), writes=[nrow.r(0)])
    kb.dma("sp", lambda q: q.dma_start(out=nrow[0:1, 128:384], in_=glag_d), writes=[nrow.r(1)])
    ngc = sb("ngc", [128, 3], F32)
    row_to_cols(lambda j: nrow[0:1, j * 128:(j + 1) * 128], 3, PS[0], ngc[:, :], [nrow.r(0), nrow.r(1)], [ngc.r()])
    win_v = win_d.rearrange("(kc p) n -> p kc n", p=128)
    wbd = sb("wbd", [128, KC, 32], BF16)
    wlr = sb("wlr", [128, KC, 32], BF16)
    kb.dma("pool", lambda q: q.dma_start(out=wbd[:, :, :], in_=win_v[:, :, OFF_BETA:OFF_BETA + 32]), writes=[wbd.r()])
    kb.dma("pool", lambda q: q.dma_start(out=wlr[:, :, :], in_=win_v[:, :, OFF_LR:OFF_LR + 32]), writes=[wlr.r()])

    if bis == 6:
        return early()
    gt_tmp = sb("gt_tmp", [128, 16], F32)

    TB = [(t0, min(512, S - t0)) for t0 in range(0, S, 512)]

    def tile_src(b, t):
        if t < NTC:
            return ctx_d[b, t * 128:(t + 1) * 128, :]
        return x_d[b, (t - NTC) * 128:(t - NTC + 1) * 128, :]

    def stage1(b):
        xt = [sb("xt%d" % i, [128, D], F32) for i in range(2)]
        xh = [sb("xh%d" % i, [128, D], BF16) for i in range(2)]
        junk = sb("junk", [128, D], BF16)
        st1 = [sb("st1_%d" % i, [128, 4], F32) for i in range(2)]
        for t in range(NT):
            i = t % 2
            src = NB if t < NTC else b
            kb.dma("sp", lambda q, t=t, i=i: q.dma_start(out=xt[i][:, :], in_=tile_src(b, t)), writes=[xt[i].r()])
            kb.op("act", lambda e, i=i: e.activation(out=junk[:, :], in_=xt[i][:, :], func=AF.Square, accum_out=st1[i][:, 0:1]),
                  reads=[xt[i].r()], writes=[junk.r(), st1[i].r()])
            kb.op("act", lambda e, i=i: e.activation(out=st1[i][:, 1:2], in_=st1[i][:, 0:1], func=AF.Sqrt, scale=1.0 / D, bias=EPS),
                  reads=[st1[i].r()], writes=[st1[i].r()])
            kb.op("dve", lambda e, i=i: e.reciprocal(out=st1[i][:, 2:3], in_=st1[i][:, 1:2]),
                  reads=[st1[i].r()], writes=[st1[i].r()])
            kb.op("act", lambda e, i=i: e.activation(out=xh[i][:, :], in_=xt[i][:, :], func=AF.Copy, scale=st1[i][:, 2:3]),
                  reads=[xt[i].r(), st1[i].r()], writes=[xh[i].r()])
            ps = PS[2 + i]
            psb = ps[:, :].bitcast(BF16)
            for kc in range(KC):
                kb.op("pe", lambda e, kc=kc, i=i, psb=psb: e.transpose(psb[:, kc * 128:(kc + 1) * 128], xh[i][:, kc * 128:(kc + 1) * 128], C("ident", bf=True)),
                      reads=[xh[i].r()] + CRB, writes=[ps.r()], inc=(kc == KC - 1))
            for kc in range(KC):
                sc_ = gsc1[:, src * KC + kc:src * KC + kc + 1]
                sh_ = modcol[:, src * 16 + kc:src * 16 + kc + 1]
                rds = [ps.r(), gsc1.r(src), modcol.r((src, 0, 0)), modcol.r((src, 0, 1))]
                if True:
                    kb.op("dve", lambda e, kc=kc, t=t, psb=psb, sc_=sc_, sh_=sh_: e.tensor_scalar(out=hT[:, kc, t * 128:(t + 1) * 128], in0=psb[:, kc * 128:(kc + 1) * 128],
                                                                                 scalar1=sc_, scalar2=sh_, op0=ALU.mult, op1=ALU.add),
                          reads=rds, writes=[hT.r((kc, t))])
                else:
                    kb.op("act", lambda e, kc=kc, t=t, psb=psb, sc_=sc_, sh_=sh_: e.activation(out=hT[:, kc, t * 128:(t + 1) * 128], in_=psb[:, kc * 128:(kc + 1) * 128],
                                                                              func=AF.Identity, scale=sc_, bias=sh_),
                          reads=rds, writes=[hT.r((kc, t))])

    def hT_reads(t0, n):
        return [hT.r((kc, t)) for kc in range(KC) for t in range(t0 // 128, (t0 + n) // 128)]

    def stage2_small(b):
        for t in range(NT):
            ps = PS[0]
            for kc in range(KC):
                mm(ps[:, 0:32], hT[:, kc, t * 128:(t + 1) * 128], wbd[:, kc, :], kc == 0, kc == KC - 1, hT_reads(t * 128, 128) + [wbd.r()], [ps.r()])
            kb.op("act", lambda e, t=t, ps=ps: e.activation(out=Gtok[:, t, 0:16], in_=ps[:, 0:16], func=AF.Sigmoid), reads=[ps.r()], writes=[Gtok.r(t)])
            kb.op("dve", lambda e, ps=ps: e.tensor_tensor(out=gt_tmp[:, :], in0=ps[:, 16:32], in1=gconst[:, 16:32], op=ALU.add), reads=[ps.r(), gconst.r()], writes=[gt_tmp.r()])
            kb.op("act", lambda e: e.activation(out=gt_tmp[:, :], in_=gt_tmp[:, :], func=AF.Exp), reads=[gt_tmp.r()], writes=[gt_tmp.r()])
            kb.op("act", lambda e: e.activation(out=gt_tmp[:, :], in_=gt_tmp[:, :], func=AF.Ln, bias=1.0), reads=[gt_tmp.r()], writes=[gt_tmp.r()])
            kb.op("dve", lambda e, t=t: e.tensor_tensor(out=Gtok[:, t, 16:32], in0=gt_tmp[:, :], in1=gconst[:, 0:16], op=ALU.mult), reads=[gt_tmp.r(), gconst.r()], writes=[Gtok.r(t)])

    def compute_betaT(b, betaT):
        for (t0, n) in TB:
            ps = PS[1]
            for kc in range(KC):
                mm(ps[0:16, 0:n], wbd[:, kc, 0:16], hT[:, kc, t0:t0 + n], kc == 0, kc == KC - 1, hT_reads(t0, n) + [wbd.r()], [ps.r()])
            kb.op("act", lambda e, t0=t0, n=n, ps=ps: e.activation(out=betaT[0:16, t0:t0 + n], in_=ps[0:16, 0:n], func=AF.Sigmoid), reads=[ps.r()], writes=[betaT.r()])

    def compute_lrT(b, lrT):
        for (t0, n) in TB:
            for d in range(2):
                ps = PS[d]
                for kc in range(KC):
                    mm(ps[0:16, 0:n], wlr[:, kc, d * 16:(d + 1) * 16], hT[:, kc, t0:t0 + n], kc == 0, kc == KC - 1, hT_reads(t0, n) + [wlr.r()], [ps.r()])
                kb.op("dve", lambda e, t0=t0, n=n, ps=ps, d=d: e.tensor_copy(out=lrT[d][0:16, t0:t0 + n], in_=ps[0:16, 0:n]), reads=[ps.r()], writes=[lrT[d].r()])

    ya_d = nc.dram_tensor("ya_s", [NB, D, SEQ], BF16, kind="Internal").ap()
    yb_d = nc.dram_tensor("yb_s", [NB, D, SEQ], BF16, kind="Internal").ap()
    LTB = [(t0, min(512, SEQ - t0)) for t0 in range(0, SEQ, 512)]

    def proj_chunk(wt, widx, dst, col0, ncols, tok0=0):
        k = 0
        for c0 in range(col0, col0 + ncols, 512):
            n = min(512, col0 + ncols - c0)
            ps = PS[2 + (k % 2)]
            for kc in range(KC):
                mm(ps[:, 0:n], wt[:, kc, widx, :], hT[:, kc, tok0 + c0:tok0 + c0 + n], kc == 0, kc == KC - 1,
                   hT_reads(tok0 + c0, n) + [wt.r(widx)], [ps.r()])
            if k % 2 == 0:
                kb.op("act", lambda e, ps=ps, c0=c0, n=n: e.activation(out=dst[:, c0:c0 + n], in_=ps[:, 0:n], func=AF.Copy), reads=[ps.r()], writes=[dst.r()])
            else:
                kb.op("dve", lambda e, ps=ps, c0=c0, n=n: e.tensor_copy(out=dst[:, c0:c0 + n], in_=ps[:, 0:n]), reads=[ps.r()], writes=[dst.r()])
            k += 1

    def conv(en, ch, pre, acc):
        cw = lambda tap: cwc[:, ch * 9 + tap:ch * 9 + tap + 1]
        rd = [pre.r(), acc.r(), cwc.r(ch // 8)]
        wr = [acc.r()]
        kb.op(en, lambda e: e.tensor_scalar(out=acc[:, 0:S], in0=pre[:, 0:S], scalar1=cw(4), scalar2=None, op0=ALU.mult), reads=rd, writes=wr)
        kb.op(en, lambda e: e.scalar_tensor_tensor(out=acc[:, 1:CTX], in0=pre[:, 0:CTX - 1], scalar=cw(3), in1=acc[:, 1:CTX], op0=ALU.mult, op1=ALU.add), reads=rd, writes=wr)
        kb.op(en, lambda e: e.scalar_tensor_tensor(out=acc[:, 0:CTX - 1], in0=pre[:, 1:CTX], scalar=cw(5), in1=acc[:, 0:CTX - 1], op0=ALU.mult, op1=ALU.add), reads=rd, writes=wr)
        P3 = pre[:, CTX:S].rearrange("p (r c) -> p r c", c=GW)
        A3 = acc[:, CTX:S].rearrange("p (r c) -> p r c", c=GW)
        for a in range(3):
            for bb in range(3):
                if a == 1 and bb == 1:
                    continue
                dr, dc = a - 1, bb - 1
                r0, r1 = max(0, -dr), ROWS - max(0, dr)
                c0, c1 = max(0, -dc), GW - max(0, dc)
                kb.op(en, lambda e, a=a, bb=bb, dr=dr, dc=dc, r0=r0, r1=r1, c0=c0, c1=c1: e.scalar_tensor_tensor(
                    out=A3[:, r0:r1, c0:c1], in0=P3[:, r0 + dr:r1 + dr, c0 + dc:c1 + dc], scalar=cw(a * 3 + bb), in1=A3[:, r0:r1, c0:c1],
                    op0=ALU.mult, op1=ALU.add), reads=rd, writes=wr)

    def l2norm(dst, scale, tmpb, tmpf):
        for (t0, n) in TB:
            kb.op("act", lambda e, t0=t0, n=n: e.activation(out=tmpb[:, 0:n], in_=dst[:, t0:t0 + n], func=AF.Square), reads=[dst.r()], writes=[tmpb.r()])
            ps = PS[2]
            mm(ps[:, 0:n], C("ones", bf=True), tmpb[:, 0:n], True, True, [tmpb.r()] + CRB, [ps.r()])
            kb.op("act", lambda e, n=n, ps=ps: e.activation(out=tmpf[:, 0:n], in_=ps[:, 0:n], func=AF.Ln, bias=EPS), reads=[ps.r()], writes=[tmpf.r()])
            kb.op("act", lambda e, n=n: e.activation(out=tmpf[:, 0:n], in_=tmpf[:, 0:n], func=AF.Exp, scale=-0.5), reads=[tmpf.r()], writes=[tmpf.r()])
            kb.op("dve", lambda e, t0=t0, n=n: e.scalar_tensor_tensor(out=dst[:, t0:t0 + n], in0=dst[:, t0:t0 + n], scalar=scale, in1=tmpf[:, 0:n], op0=ALU.mult, op1=ALU.mult),
                  reads=[dst.r(), tmpf.r()], writes=[dst.r()])

    def interleave(gens):
        gens = [g for g in gens if g is not None]
        while gens:
            for g in list(gens):
                try:
                    next(g)
                except StopIteration:
                    gens.remove(g)

    def gdn_all(b):
        betaT = sb("betaT", [16, S], BF16)
        compute_betaT(b, betaT)
        wch = sb("wch", [128, KC, 3, 128], BF16)
        wz = sb("wz", [128, KC, 1, 128], BF16)
        pre = sb("pre", [128, S], BF16)
        pre2 = sb("pre2", [128, S], BF16)
        oT = sb("oTa", [128, SEQ], BF16)
        qT = sb("qT", [128, S], BF16)
        kT = sb("kT", [128, S], BF16)
        vT = sb("vT", [128, S], BF16)
        zT = qT
        kbT = pre
        tmpb = sb("tmpb", [128, 512], BF16)
        tmpf = sb("tmpf", [128, 512], F32)
        tmpg = sb("tmpg", [128, 512], F32)
        Sf = [sb("Sf%d" % d_, [128, 128], F32) for d_ in range(2)]
        Sb = [sb("Sb%d" % d_, [128, 128], BF16) for d_ in range(2)]
        rbf = [sb("rbf%d" % d_, [128, 128], BF16) for d_ in range(2)]
        vnb = [sb("vnb%d" % d_, [128, 128], BF16) for d_ in range(2)]
        kbT1 = sb("kbT1", [128, S], BF16)
        WK = []
        for i in range(4):
            WK.append(dict(
                grep=sb("grep%d" % i, [128, 128], F32), tmp=sb("wtmp%d" % i, [128, 128], F32),
                E=sb("E%d" % i, [128, 128], BF16), Es=sb("Es%d" % i, [128, 128], BF16), erow=sb("erow%d" % i, [128, 128], BF16),
                L=[sb("L%d_%d" % (i, j), [128, 384], BF16) for j in range(2)],
                attnT=sb("attnT%d" % i, [128, 128], BF16), qgT=sb("qgT%d" % i, [128, 128], BF16), nkbgT=sb("nkbgT%d" % i, [128, 128], BF16),
                kd=sb("kd%d" % i, [128, 128], BF16), vb=sb("vb%d" % i, [128, 128], BF16), cols=sb("cols%d" % i, [128, 8], F32),
                TT=None))

        def load_qkv_w(hh):
            for j, off in enumerate((OFF_QA, OFF_KA, OFF_VA)):
                kb.dma("pool", lambda q, j=j, off=off: q.dma_start(out=wch[:, :, j, :], in_=win_v[:, :, off + hh * 128:off + (hh + 1) * 128]), writes=[wch.r(j)])

        load_qkv_w(0)
        for h in range(NH_A):
            kb.dma("pool", lambda q: q.dma_start(out=wz[:, :, 0, :], in_=win_v[:, :, OFF_ZA + h * 128:OFF_ZA + (h + 1) * 128]), writes=[wz.r(0)])
            pres = (pre, kbT1, pre2)
            for j in range(3):
                proj_chunk(wch, j, pres[j], 0, S)
            if h + 1 < NH_A:
                load_qkv_w(h + 1)
            for j, dst in enumerate((qT, kT, vT)):
                conv("dve", j * 8 + h, pres[j], dst)
                kb.op("act", lambda e, dst=dst: e.activation(out=dst[:, 0:S], in_=dst[:, 0:S], func=AF.Silu), reads=[dst.r()], writes=[dst.r()])
            l2norm(qT, float(128 ** -0.5), tmpb, tmpf)
            l2norm(kT, 1.0, tmpb, tmpf)
            if b == 0 and h == 0:
                dbg_out("qT", qT[:, :], [qT.r()])
                dbg_out("kT", kT[:, :], [kT.r()])
                dbg_out("vT", vT[:, :], [vT.r()])
            owr = set()
            kbTs = [kbT, kbT1]
            interleave([gdn_scan(b, h, d, locals()) for d in range(2)])
            proj_chunk(wz, 0, zT, 0, SEQ, tok0=CTX)
            for (t0, n) in LTB:
                kb.op("act", lambda e, t0=t0, n=n: e.activation(out=tmpb[:, 0:n], in_=oT[:, t0:t0 + n], func=AF.Square), reads=[oT.r()], writes=[tmpb.r()])
                ps = PS[2]
                mm(ps[:, 0:n], C("ones", bf=True), tmpb[:, 0:n], True, True, [tmpb.r()] + CRB, [ps.r()])
                kb.op("act", lambda e, n=n, ps=ps: e.activation(out=tmpf[:, 0:n], in_=ps[:, 0:n], func=AF.Ln, scale=1.0 / 128, bias=EPS), reads=[ps.r()], writes=[tmpf.r()])
                kb.op("act", lambda e, n=n: e.activation(out=tmpf[:, 0:n], in_=tmpf[:, 0:n], func=AF.Exp, scale=-0.5), reads=[tmpf.r()], writes=[tmpf.r()])
                kb.op("dve", lambda e, t0=t0, n=n: e.scalar_tensor_tensor(out=tmpf[:, 0:n], in0=oT[:, t0:t0 + n], scalar=ngc[:, 0:1], in1=tmpf[:, 0:n], op0=ALU.mult, op1=ALU.mult),
                      reads=[oT.r(), tmpf.r(), ngc.r()], writes=[tmpf.r()])
                kb.op("act", lambda e, t0=t0, n=n: e.activation(out=tmpg[:, 0:n], in_=zT[:, t0:t0 + n], func=AF.Silu), reads=[zT.r()], writes=[tmpg.r()])
                kb.op("dve", lambda e, n=n: e.tensor_tensor(out=tmpb[:, 0:n], in0=tmpf[:, 0:n], in1=tmpg[:, 0:n], op=ALU.mult), reads=[tmpf.r(), tmpg.r()], writes=[tmpb.r()])
                kb.dma("sp", lambda q, t0=t0, n=n: q.dma_start(out=ya_d[b, h * 128:(h + 1) * 128, t0:t0 + n], in_=tmpb[:, 0:n]), reads=[tmpb.r()], writes=[yaR])
            if b == 0:
                dbg_out("oT%d" % h, oT[:, 0:SEQ], [oT.r()])

    yaR = kb.res("ya_dram")
    ybR = kb.res("yb_dram")

    def gdn_scan(b, h, d, L_):
        betaT, qT, kT, vT, oT, owr = (L_[k] for k in ("betaT", "qT", "kT", "vT", "oT", "owr"))
        kbT = L_["kbTs"][d]
        WK = L_["WK"][2 * d:2 * d + 2]
        Sf, Sb, rbf, vnb = L_["Sf"][d], L_["Sb"][d], L_["rbf"][d], L_["vnb"][d]
        tri, neg = ("tri_f", "neg_f") if d == 0 else ("tri_b", "neg_b")
        if d == 0:
            order = list(range(NT))
        else:
            order = [NTC - 1 - t for t in range(NTC)] + [NT - 1 - t for t in range(NTL)]
        r = d * 8 + h
        for (t0, n) in TB:
            ps = PS[3]
            mm(ps[:, 0:n], sel16[0:16, r * 128:(r + 1) * 128], betaT[0:16, t0:t0 + n], True, True, [sel16.r(), betaT.r()], [ps.r()])
            kb.op("dve", lambda e, t0=t0, n=n, ps=ps: e.tensor_tensor(out=kbT[:, t0:t0 + n], in0=kT[:, t0:t0 + n], in1=ps[:, 0:n], op=ALU.mult), reads=[kT.r(), ps.r()], writes=[kbT.r()])
        kb.op("dve", lambda e: e.memset(Sf[:, :], 0.0), writes=[Sf.r()])
        kb.op("dve", lambda e: e.memset(Sb[:, :], 0.0), writes=[Sb.r()])

        def pre_gen(idx, t):
            W = WK[idx % 2]
            ts_ = slice(t * 128, (t + 1) * 128)
            gcol = Gtok[:, t, 16 + r:17 + r]
            bcol = Gtok[:, t, r:r + 1]
            cols, grep, tmp, E, Es, erow = W["cols"], W["grep"], W["tmp"], W["E"], W["Es"], W["erow"]
            bX, bY = (PS[2], PS[3]) if d == 0 else (PS[5], PS[6])
            kb.op("act", lambda e: e.activation(out=grep[:, :], in_=C("ones"), func=AF.Copy, scale=gcol), reads=[Gtok.r(t)] + CR, writes=[grep.r()])
            mm(bY[:, 448:449], C(tri), gcol, True, True, [Gtok.r(t)] + CR, [bY.r()])
            mm(bY[:, 449:450], C("blk"), gcol, True, True, [Gtok.r(t)] + CR, [bY.r()])
            mm(bX[:, 0:128], grep[:, :], C(tri), True, True, [grep.r()] + CR, [bX.r()])
            mm(bY[:, 450:451], grep[:, :], C("blk", cols=slice(0, 1)), True, True, [grep.r()] + CR, [bY.r()])
            mm(bY[:, 451:452], grep[:, :], C("blk", cols=slice(64, 65)), True, True, [grep.r()] + CR, [bY.r()])
            kb.op("dve", lambda e: e.tensor_copy(out=cols[:, 0:2], in_=bY[:, 448:450]), reads=[bY.r()], writes=[cols.r()])
            kb.op("dve", lambda e: e.tensor_tensor(out=cols[:, 2:3], in0=cols[:, 1:2], in1=cols[:, 0:1], op=ALU.subtract), reads=[cols.r()], writes=[cols.r()])
            kb.op("act", lambda e: e.activation(out=cols[:, 2:4], in_=cols[:, 2:4], func=AF.Exp) if False else e.activation(out=cols[:, 2:3], in_=cols[:, 2:3], func=AF.Exp), reads=[cols.r()], writes=[cols.r()])
            kb.op("act", lambda e: e.activation(out=cols[:, 3:4], in_=cols[:, 0:1], func=AF.Exp), reads=[cols.r()], writes=[cols.r()])
            kb.op("dve", lambda e: e.tensor_tensor(out=cols[:, 4:5], in0=cols[:, 3:4], in1=bcol, op=ALU.mult), reads=[cols.r(), Gtok.r(t)], writes=[cols.r()])
            kb.op("act", lambda e: e.activation(out=cols[:, 5:7], in_=bY[:, 450:452], func=AF.Exp), reads=[bY.r()], writes=[cols.r()])
            kb.op("dve", lambda e: e.scalar_tensor_tensor(out=tmp[:, :], in0=bX[:, 0:128], scalar=cols[:, 0:1], in1=C(neg), op0=ALU.subtract, op1=ALU.add),
                  reads=[bX.r(), cols.r()] + CR, writes=[tmp.r()])
            kb.op("act", lambda e: e.activation(out=E[:, :], in_=tmp[:, :], func=AF.Exp), reads=[tmp.r()], writes=[E.r()])
            kb.op("pool", lambda e: e.tensor_tensor(out=Es[:, :], in0=E[:, :], in1=C("offd", bf=True), op=ALU.mult), reads=[E.r()] + CRB, writes=[Es.r()])
            kb.op("act", lambda e: e.activation(out=erow[:, :], in_=bX[:, 0:128], func=AF.Exp), reads=[bX.r()], writes=[erow.r()])
            yield
            mm(bX[:, 128:256], kT[:, ts_], kbT[:, ts_], True, True, [kT.r(), kbT.r()], [bX.r()])
            mm(bX[:, 256:384], kT[:, ts_], qT[:, ts_], True, True, [kT.r(), qT.r()], [bX.r()])
            L0 = W["L"][0]
            kb.op("dve", lambda e: e.scalar_tensor_tensor(out=L0[:, 128:256], in0=bX[:, 128:256], scalar=-1.0, in1=Es[:, :], op0=ALU.mult, op1=ALU.mult),
                  reads=[bX.r(), Es.r()], writes=[L0.r("x")])
            kb.op("dve", lambda e: e.tensor_tensor(out=W["attnT"][:, :], in0=bX[:, 256:384], in1=E[:, :], op=ALU.mult), reads=[bX.r(), E.r()], writes=[W["attnT"].r()])
            bXb = bX[:, :].bitcast(BF16)
            bYb = bY[:, :].bitcast(BF16)
            kb.op("pe", lambda e: e.transpose(bYb[:, 768:896], L0[:, 128:256], C("ident", bf=True)), reads=[L0.r("x")] + CRB, writes=[bY.r()])
            kb.op("pe", lambda e: e.transpose(bXb[:, 768:896], kT[:, ts_], C("ident", bf=True)), reads=[kT.r()] + CRB, writes=[bX.r()])
            kb.op("pe", lambda e: e.transpose(bXb[:, 896:1024], vT[:, ts_], C("ident", bf=True)), reads=[vT.r()] + CRB, writes=[bX.r()])
            kb.op("dve", lambda e: e.tensor_copy(out=L0[:, 256:384], in_=bYb[:, 768:896]), reads=[bY.r()], writes=[L0.r("w")])
            kb.op("pool", lambda e: e.tensor_copy(out=L0[:, 0:128], in_=C("ident", bf=True)), reads=CRB, writes=[L0.r("q")])
            kb.op("dve", lambda e: e.tensor_scalar(out=W["kd"][:, :], in0=bXb[:, 768:896], scalar1=cols[:, 2:3], scalar2=None, op0=ALU.mult), reads=[bX.r(), cols.r()], writes=[W["kd"].r()])
            kb.op("dve", lambda e: e.tensor_scalar(out=W["vb"][:, :], in0=bXb[:, 896:1024], scalar1=bcol, scalar2=None, op0=ALU.mult), reads=[bX.r(), Gtok.r(t)], writes=[W["vb"].r()])
            kb.op("pool", lambda e: e.tensor_tensor(out=W["qgT"][:, :], in0=qT[:, ts_], in1=erow[:, :], op=ALU.mult), reads=[qT.r(), erow.r()], writes=[W["qgT"].r()])
            kb.op("dve", lambda e: e.scalar_tensor_tensor(out=W["nkbgT"][:, :], in0=kbT[:, ts_], scalar=-1.0, in1=erow[:, :], op0=ALU.mult, op1=ALU.mult),
                  reads=[kbT.r(), erow.r()], writes=[W["nkbgT"].r()])
            yield
            for hop in range(6):
                Li, Lo = W["L"][hop % 2], W["L"][(hop + 1) % 2]
                lr_ = [Li.r("q"), Li.r("x"), Li.r("w")]
                mm(bY[:, 0:128], C("ident", bf=True), Li[:, 0:128], True, False, lr_ + CRB, [bY.r()], inc=False)
                mm(bY[:, 0:128], Li[:, 256:384], Li[:, 0:128], False, True, lr_, [bY.r()], inc=(hop == 5))
                if hop < 5:
                    mm(bY[:, 256:384], Li[:, 128:256], Li[:, 256:384], True, True, lr_, [bY.r()], inc=(hop == 4))
                if hop < 4:
                    mm(bY[:, 128:256], Li[:, 256:384], Li[:, 128:256], True, True, lr_, [bY.r()], inc=True)
                hi = 128 if hop == 5 else 384
                wr = [Lo.r("q"), Lo.r("x"), Lo.r("w")]
                if hop % 2 == 0:
                    kb.op("act", lambda e, Lo=Lo, hi=hi: e.activation(out=Lo[:, 0:hi], in_=bY[:, 0:hi], func=AF.Copy), reads=[bY.r()], writes=wr)
                else:
                    kb.op("dve", lambda e, Lo=Lo, hi=hi: e.tensor_copy(out=Lo[:, 0:hi], in_=bY[:, 0:hi]), reads=[bY.r()], writes=wr)
                yield
            W["TT"] = W["L"][0]

        def scan_gen(idx, t):
            W = WK[idx % 2]
            TT = W["L"][0]
            cols = W["cols"]
            pS = PS[4] if d == 0 else PS[7]
            for c in ((0, 1) if d == 0 else (1, 0)):
                sl = slice(c * 64, c * 64 + 64)
                mm(pS[:, 0:128], C("ident", bf=True), W["vb"][:, :], True, False, [W["vb"].r()] + CRB, [pS.r("r")], inc=False)
                mm(pS[:, 0:128], W["nkbgT"][:, :], Sb[:, :], False, True, [W["nkbgT"].r(), Sb.r()], [pS.r("r")])
                kb.op("act", lambda e, sl=sl: e.activation(out=rbf[sl, :], in_=pS[sl, 0:128], func=AF.Copy), reads=[pS.r("r")], writes=[rbf.r()])
                yield
                mm(pS[:, 128:256], TT[sl, 0:128], rbf[sl, :], True, True, [TT.r("q"), rbf.r()], [pS.r("v")])
                kb.op("dve", lambda e, sl=sl: e.tensor_copy(out=vnb[sl, :], in_=pS[sl, 128:256]), reads=[pS.r("v")], writes=[vnb.r()])
                yield
                if t >= NTC:
                    pO = pS
                    mm(pO[:, 256:320], Sb[:, :], W["qgT"][:, sl], True, False, [Sb.r(), W["qgT"].r()], [pO.r()], inc=False)
                    mm(pO[:, 256:320], vnb[sl, :], W["attnT"][sl, sl], False, True, [vnb.r(), W["attnT"].r()], [pO.r()])
                    o0 = (t - NTC) * 128 + c * 64
                    if o0 not in owr:
                        owr.add(o0)
                        kb.op("act", lambda e, o0=o0: e.activation(out=oT[:, o0:o0 + 64], in_=pO[:, 256:320], func=AF.Copy), reads=[pO.r()], writes=[oT.r()])
                    else:
                        kb.op("dve", lambda e, o0=o0: e.tensor_tensor(out=oT[:, o0:o0 + 64], in0=pO[:, 256:320], in1=oT[:, o0:o0 + 64], op=ALU.add), reads=[pO.r(), oT.r()], writes=[oT.r()])
                mm(pS[:, 320:448], W["kd"][sl, :], vnb[sl, :], True, True, [W["kd"].r(), vnb.r()], [pS.r("s")])
                kb.op("dve", lambda e, c=c: e.scalar_tensor_tensor(out=Sf[:, :], in0=Sf[:, :], scalar=cols[:, 5 + c:6 + c], in1=pS[:, 320:448], op0=ALU.mult, op1=ALU.add),
                      reads=[Sf.r(), cols.r(), pS.r("s")], writes=[Sf.r()])
                kb.op("act", lambda e: e.activation(out=Sb[:, :], in_=Sf[:, :], func=AF.Copy), reads=[Sf.r()], writes=[Sb.r()])
                yield

        yield
        for _ in pre_gen(0, order[0]):
            yield
        for idx, t in enumerate(order):
            nxt = pre_gen(idx + 1, order[idx + 1]) if idx + 1 < len(order) else None
            gens = [g for g in (scan_gen(idx, t), nxt) if g is not None]
            while gens:
                for g in list(gens):
                    try:
                        next(g)
                        yield
                    except StopIteration:
                        gens.remove(g)

    def gla_all(b):
        gwb_t = sb("gwb", [48, 512], BF16)

        class _Rows:
            def __init__(s_, buf, base): s_.buf, s_.base = buf, base
            def __getitem__(s_, idx): return s_.buf[slice(idx[0].start + s_.base, idx[0].stop + s_.base), idx[1]]
            def r(s_, key=0): return s_.buf.r()
        gwb = [_Rows(gwb_t, 0), _Rows(gwb_t, 32)]
        gb_bc = [sb("gb_bc%d" % d, [128, 512], F32) for d in range(2)]
        with scope():
            gbrow = sb("gbrow", [1, 1024], F32)
            for d in range(2):
                kb.dma("pool", lambda q, d=d: q.dma_start(out=gwb[d][0:16, 0:512], in_=glaw_d[d]), writes=[gwb[d].r()])
                kb.dma("sp", lambda q, d=d: q.dma_start(out=gbrow[0:1, d * 512:(d + 1) * 512], in_=glab_d[d:d + 1, :]), writes=[gbrow.r(d)])
                row_bcast(gbrow[0:1, d * 512:(d + 1) * 512], 512, PS[1], gb_bc[d][:, :], [gbrow.r(d)], [gb_bc[d].r()])
        lrT_t = sb("lrT", [48, S], BF16)
        lrT = [_Rows(lrT_t, 0), _Rows(lrT_t, 32)]
        compute_lrT(b, lrT)
        wch = sb("wchb", [128, KC, 6, 128], BF16)
        qT = sb("qbT", [128, S], BF16)
        kT = sb("kbT_", [128, S], BF16)
        vT = sb("vbT", [128, 2 * S], BF16)
        rT = vT
        oT = sb("oTb", [128, 2 * SEQ], BF16)
        tmpb = sb("g_tmpb", [128, 512], BF16)
        tmpb2 = sb("g_tmpb2", [128, 512], BF16)
        tmpf = sb("g_tmpf", [128, 512], F32)
        tmpg = sb("g_tmpg", [128, 512], F32)
        Sf = [sb("g_Sf%d" % d_, [128, 256], F32) for d_ in range(2)]
        Sb = [sb("g_Sb%d" % d_, [128, 256], BF16) for d_ in range(2)]
        WK = []
        for i in range(4):
            WK.append(dict(
                la=sb("la%d" % i, [128, 128], F32), tmpx=sb("tmpx%d" % i, [128, 128], F32), edk=sb("edk%d" % i, [128, 128], F32),
                gcT=sb("gcT%d" % i, [128, 128], F32), eg=sb("eg%d" % i, [128, 128], BF16), glc=sb("glc%d" % i, [128, 2], F32),
                tq=sb("tq%d" % i, [128, 128], F32), eq=sb("eq%d" % i, [128, 128], BF16), ek=sb("ek%d" % i, [128, 128], BF16),
                qgT=sb("gqgT%d" % i, [128, 128], BF16), qrT=sb("qrT%d" % i, [128, 128], BF16), krT=sb("krT%d" % i, [128, 128], BF16),
                attnT=sb("gattnT%d" % i, [128, 128], BF16), kd=sb("gkd%d" % i, [128, 128], BF16), vtok=sb("vtok%d" % i, [128, 256], BF16)))
        for hb in range(NH_B):
            offs = [OFF_QB + hb * 128, OFF_KB + hb * 128, OFF_VB + hb * 256, OFF_VB + hb * 256 + 128, OFF_RB + hb * 256, OFF_RB + hb * 256 + 128]
            for j, off in enumerate(offs):
                kb.dma("pool", lambda q, j=j, off=off: q.dma_start(out=wch[:, :, j, :], in_=win_v[:, :, off:off + 128]), writes=[wch.r(j)])
            proj_chunk(wch, 0, qT, 0, S)
            kb.op("dve", lambda e: e.tensor_scalar(out=qT[:, 0:S], in0=qT[:, 0:S], scalar1=float(128 ** -0.5), scalar2=None, op0=ALU.mult), reads=[qT.r()], writes=[qT.r()])
            proj_chunk(wch, 1, kT, 0, S)
            vT0 = Buf(kb, vT.t, "vT0"); vT0._r = vT._r
            for hv in range(2):
                class _V:
                    def __init__(s_, base): s_.base = base
                    def __getitem__(s_, idx): return vT[idx[0], slice(idx[1].start + s_.base, idx[1].stop + s_.base)]
                    def r(s_, key=0): return vT.r()
                proj_chunk(wch, 2 + hv, _V(hv * S), 0, S)
            owr = set()
            interleave([gla_scan(b, hb, d, locals()) for d in range(2)])
            for hv in range(2):
                class _R:
                    def __init__(s_, base): s_.base = base
                    def __getitem__(s_, idx): return rT[idx[0], slice(idx[1].start + s_.base, idx[1].stop + s_.base)]
                    def r(s_, key=0): return rT.r()
                proj_chunk(wch, 4 + hv, _R(hv * SEQ), 0, SEQ, tok0=CTX)
            for (t0, n) in LTB:
                ps = PS[2]
                tb_ = (tmpb, tmpb2)
                for hv in range(2):
                    kb.op("act", lambda e, t0=t0, n=n, hv=hv: e.activation(out=tb_[hv][:, 0:n], in_=oT[:, hv * SEQ + t0:hv * SEQ + t0 + n], func=AF.Square), reads=[oT.r()], writes=[tb_[hv].r()])
                for hv in range(2):
                    mm(ps[:, 0:n], C("ones", bf=True), tb_[hv][:, 0:n], hv == 0, hv == 1, [tb_[hv].r()] + CRB, [ps.r()])
                kb.op("act", lambda e, n=n, ps=ps: e.activation(out=tmpf[:, 0:n], in_=ps[:, 0:n], func=AF.Ln, scale=1.0 / 256, bias=EPS), reads=[ps.r()], writes=[tmpf.r()])
                kb.op("act", lambda e, n=n: e.activation(out=tmpf[:, 0:n], in_=tmpf[:, 0:n], func=AF.Exp, scale=-0.5), reads=[tmpf.r()], writes=[tmpf.r()])
                for hv in range(2):
                    kb.op("dve", lambda e, t0=t0, n=n, hv=hv: e.scalar_tensor_tensor(out=tmpg[:, 0:n], in0=oT[:, hv * SEQ + t0:hv * SEQ + t0 + n], scalar=ngc[:, 1 + hv:2 + hv], in1=tmpf[:, 0:n], op0=ALU.mult, op1=ALU.mult),
                          reads=[oT.r(), tmpf.r(), ngc.r()], writes=[tmpg.r()])
                    kb.op("act", lambda e, t0=t0, n=n, hv=hv: e.activation(out=tb_[hv][:, 0:n], in_=rT[:, hv * SEQ + t0:hv * SEQ + t0 + n], func=AF.Silu), reads=[rT.r()], writes=[tb_[hv].r()])
                    kb.op("dve", lambda e, n=n, hv=hv: e.tensor_tensor(out=tb_[hv][:, 0:n], in0=tmpg[:, 0:n], in1=tb_[hv][:, 0:n], op=ALU.mult), reads=[tmpg.r(), tb_[hv].r()], writes=[tb_[hv].r()])
                    kb.dma("sp", lambda q, t0=t0, n=n, hv=hv: q.dma_start(out=yb_d[b, hb * 256 + hv * 128:hb * 256 + (hv + 1) * 128, t0:t0 + n], in_=tb_[hv][:, 0:n]), reads=[tb_[hv].r()], writes=[ybR])
            if b == 0:
                dbg_out("obT%d" % hb, oT[:, :], [oT.r()])

    def gla_scan(b, hb, d, L_):
        lrT, qT, kT, vT, oT, gwb, gb_bc, owr = (L_[k] for k in ("lrT", "qT", "kT", "vT", "oT", "gwb", "gb_bc", "owr"))
        WK = L_["WK"][2 * d:2 * d + 2]
        Sf, Sb = L_["Sf"][d], L_["Sb"][d]
        tri = "tri_f" if d == 0 else "tri_b"
        if d == 0:
            order = list(range(NT))
        else:
            order = [NTC - 1 - t for t in range(NTC)] + [NT - 1 - t for t in range(NTL)]
        kb.op("dve", lambda e: e.memset(Sf[:, :], 0.0), writes=[Sf.r()])
        kb.op("dve", lambda e: e.memset(Sb[:, :], 0.0), writes=[Sb.r()])
        pA, pC = (PS[2], PS[3]) if d == 0 else (PS[5], PS[6])
        pT = pC

        def pre_gen(idx, t):
            W = WK[idx % 2]
            ts_ = slice(t * 128, (t + 1) * 128)
            la = W["la"]
            mm(pA[:, 0:128], lrT[d][0:16, ts_], gwb[d][0:16, hb * 128:(hb + 1) * 128], True, True, [lrT[d].r(), gwb[d].r()], [pA.r()])
            kb.op("dve", lambda e: e.tensor_tensor(out=la[:, :], in0=pA[:, 0:128], in1=gb_bc[d][:, hb * 128:(hb + 1) * 128], op=ALU.add), reads=[pA.r(), gb_bc[d].r()], writes=[la.r()])
            kb.op("act", lambda e: e.activation(out=la[:, :], in_=la[:, :], func=AF.Exp, scale=-1.0), reads=[la.r()], writes=[la.r()])
            kb.op("act", lambda e: e.activation(out=la[:, :], in_=la[:, :], func=AF.Ln, bias=1.0), reads=[la.r()], writes=[la.r()])
            kb.op("dve", lambda e: e.tensor_scalar(out=la[:, :], in0=la[:, :], scalar1=-1.0 / 16.0, scalar2=None, op0=ALU.mult), reads=[la.r()], writes=[la.r()])
            yield
            mm(pA[:, 0:128], C(tri), la[:, :], True, True, [la.r()] + CR, [pA.r()])
            mm(pA[:, 128:256], C("blk"), la[:, :], True, True, [la.r()] + CR, [pA.r()])
            mm(pA[:, 256:384], la[:, :], C(tri), True, True, [la.r()] + CR, [pA.r()])
            mm(pA[:, 384:385], la[:, :], C("blk", cols=slice(0, 1)), True, True, [la.r()] + CR, [pA.r()])
            mm(pA[:, 385:386], la[:, :], C("blk", cols=slice(64, 65)), True, True, [la.r()] + CR, [pA.r()])
            kb.op("act", lambda e: e.activation(out=W["tmpx"][:, :], in_=pA[:, 0:128], func=AF.Copy), reads=[pA.r()], writes=[W["tmpx"].r()])
            kb.op("dve", lambda e: e.tensor_tensor(out=W["tmpx"][:, :], in0=pA[:, 128:256], in1=W["tmpx"][:, :], op=ALU.subtract), reads=[pA.r(), W["tmpx"].r()], writes=[W["tmpx"].r()])
            kb.op("act", lambda e: e.activation(out=W["edk"][:, :], in_=W["tmpx"][:, :], func=AF.Exp), reads=[W["tmpx"].r()], writes=[W["edk"].r()])
            kb.op("dve", lambda e: e.tensor_copy(out=W["gcT"][:, :], in_=pA[:, 256:384]), reads=[pA.r()], writes=[W["gcT"].r()])
            kb.op("act", lambda e: e.activation(out=W["eg"][:, :], in_=pA[:, 256:384], func=AF.Exp), reads=[pA.r()], writes=[W["eg"].r()])
            kb.op("act", lambda e: e.activation(out=W["glc"][:, 0:2], in_=pA[:, 384:386], func=AF.Exp), reads=[pA.r()], writes=[W["glc"].r()])
            yield
            kb.op("pool", lambda e: e.tensor_tensor(out=W["qgT"][:, :], in0=qT[:, ts_], in1=W["eg"][:, :], op=ALU.mult), reads=[qT.r(), W["eg"].r()], writes=[W["qgT"].r()])
            for c in range(2):
                sl = slice(c * 64, c * 64 + 64)
                ref_ = W["gcT"][:, c * 64 + 32:c * 64 + 33]
                kb.op("dve", lambda e, sl=sl, ref_=ref_: e.tensor_scalar(out=W["tq"][:, sl], in0=W["gcT"][:, sl], scalar1=ref_, scalar2=None, op0=ALU.subtract), reads=[W["gcT"].r()], writes=[W["tq"].r()])
            kb.op("act", lambda e: e.activation(out=W["eq"][:, :], in_=W["tq"][:, :], func=AF.Exp), reads=[W["tq"].r()], writes=[W["eq"].r()])
            kb.op("act", lambda e: e.activation(out=W["ek"][:, :], in_=W["tq"][:, :], func=AF.Exp, scale=-1.0), reads=[W["tq"].r()], writes=[W["ek"].r()])
            kb.op("pool", lambda e: e.tensor_tensor(out=W["qrT"][:, :], in0=qT[:, ts_], in1=W["eq"][:, :], op=ALU.mult), reads=[qT.r(), W["eq"].r()], writes=[W["qrT"].r()])
            kb.op("pool", lambda e: e.tensor_tensor(out=W["krT"][:, :], in0=kT[:, ts_], in1=W["ek"][:, :], op=ALU.mult), reads=[kT.r(), W["ek"].r()], writes=[W["krT"].r()])
            yield
            mm(pC[:, 0:128], W["krT"][:, :], W["qrT"][:, :], True, True, [W["krT"].r(), W["qrT"].r()], [pC.r()])
            pTb = pT[:, :].bitcast(BF16)
            kb.op("pe", lambda e: e.transpose(pTb[:, 256:384], kT[:, ts_], C("ident", bf=True)), reads=[kT.r()] + CRB, writes=[pT.r()])
            for hv in range(2):
                kb.op("pe", lambda e, hv=hv: e.transpose(pTb[:, 384 + hv * 128:512 + hv * 128], vT[:, hv * S + t * 128:hv * S + (t + 1) * 128], C("ident", bf=True)), reads=[vT.r()] + CRB, writes=[pT.r()])
            kb.op("dve", lambda e: e.tensor_tensor(out=W["attnT"][:, :], in0=pC[:, 0:128], in1=C(tri, bf=True), op=ALU.mult), reads=[pC.r()] + CRB, writes=[W["attnT"].r()])
            kb.op("dve", lambda e: e.tensor_tensor(out=W["kd"][:, :], in0=pTb[:, 256:384], in1=W["edk"][:, :], op=ALU.mult), reads=[pT.r(), W["edk"].r()], writes=[W["kd"].r()])
            kb.op("dve", lambda e: e.tensor_copy(out=W["vtok"][:, :], in_=pTb[:, 384:640]), reads=[pT.r()], writes=[W["vtok"].r()])
            yield

        def scan_gen(idx, t):
            W = WK[idx % 2]
            pS = PS[4] if d == 0 else PS[7]
            pO = pS
            for c in ((0, 1) if d == 0 else (1, 0)):
                sl = slice(c * 64, c * 64 + 64)
                if t >= NTC:
                    for hv in range(2):
                        mm(pO[:, 256 + hv * 64:256 + (hv + 1) * 64], Sb[:, hv * 128:(hv + 1) * 128], W["qgT"][:, sl], True, False, [Sb.r(), W["qgT"].r()], [pO.r()], inc=False)
                        mm(pO[:, 256 + hv * 64:256 + (hv + 1) * 64], W["vtok"][sl, hv * 128:(hv + 1) * 128], W["attnT"][sl, sl], False, True, [W["vtok"].r(), W["attnT"].r()], [pO.r()])
                    o0 = (t - NTC) * 128 + c * 64
                    for hv in range(2):
                        if (o0, hv) not in owr:
                            owr.add((o0, hv))
                            kb.op("act", lambda e, o0=o0, hv=hv: e.activation(out=oT[:, hv * SEQ + o0:hv * SEQ + o0 + 64], in_=pO[:, 256 + hv * 64:256 + (hv + 1) * 64], func=AF.Copy), reads=[pO.r()], writes=[oT.r()])
                        else:
                            kb.op("dve", lambda e, o0=o0, hv=hv: e.tensor_tensor(out=oT[:, hv * SEQ + o0:hv * SEQ + o0 + 64], in0=pO[:, 256 + hv * 64:256 + (hv + 1) * 64], in1=oT[:, hv * SEQ + o0:hv * SEQ + o0 + 64], op=ALU.add),
                                  reads=[pO.r(), oT.r()], writes=[oT.r()])
                mm(pS[:, 0:256], W["kd"][sl, :], W["vtok"][sl, :], True, True, [W["kd"].r(), W["vtok"].r()], [pS.r()])
                kb.op("dve", lambda e, c=c: e.scalar_tensor_tensor(out=Sf[:, :], in0=Sf[:, :], scalar=W["glc"][:, c:c + 1], in1=pS[:, 0:256], op0=ALU.mult, op1=ALU.add),
                      reads=[Sf.r(), W["glc"].r(), pS.r()], writes=[Sf.r()])
                kb.op("act", lambda e: e.activation(out=Sb[:, :], in_=Sf[:, :], func=AF.Copy), reads=[Sf.r()], writes=[Sb.r()])
                yield

        yield
        for _ in pre_gen(0, order[0]):
            yield
        for idx, t in enumerate(order):
            nxt = pre_gen(idx + 1, order[idx + 1]) if idx + 1 < len(order) else None
            gens = [g_ for g_ in (scan_gen(idx, t), nxt) if g_ is not None]
            while gens:
                for g_ in list(gens):
                    try:
                        next(g_)
                        yield
                    except StopIteration:
                        gens.remove(g_)

    T = NB * SEQ
    TT_ = T // 128
    BLK = 512
    NBLK = -(-(T * TOPK + N_EXP * (BLK - 1)) // BLK)
    NSLOT = NBLK * BLK
    x1_d = nc.dram_tensor("x1_s", [T, D], F32, kind="Internal").ap()
    h2_d = nc.dram_tensor("h2_s", [T + 1, D], BF16, kind="Internal").ap()
    ys_d = nc.dram_tensor("ys_s", [NSLOT, D], BF16, kind="Internal").ap()
    sinfo_d = nc.dram_tensor("sinfo_s", [NSLOT, 2], I32, kind="Internal").ap()
    x1R, h2R, ysR, siR = kb.res("x1"), kb.res("h2"), kb.res("ys"), kb.res("si")
    lg_all = sb("lg_all", [128, TT_, N_EXP], F32)
    wr_sb = sb("wr_sb", [128, KC, N_EXP], F32)
    kb.dma("sp", lambda q: q.dma_start(out=wr_sb[:, :, :], in_=wr_d.rearrange("(kc p) n -> p kc n", p=128)), writes=[wr_sb.r()])
    brrow = sb("brrow", [1, N_EXP], F32)
    kb.dma("sp", lambda q: q.dma_start(out=brrow[0:1, :], in_=br_d), writes=[brrow.r()])
    br_bc = sb("br_bc", [128, N_EXP], F32)
    row_bcast(brrow[0:1, :], N_EXP, PS[1], br_bc[:, :], [brrow.r()], [br_bc.r()])

    ym_d = nc.dram_tensor("ym_s", [NB, D, SEQ], BF16, kind="Internal").ap()
    ymR = kb.res("ym_dram")

    def merge_seq(b):
        wo = sb("wo", [128, KC, D], BF16)
        for kc in range(KC):
            kb.dma("pool", lambda q, kc=kc: q.dma_start(out=wo[:, kc, :], in_=wo_d[kc * 128:(kc + 1) * 128, :]), writes=[wo.r()])
        woa_v = woa_d.rearrange("(kc p) n -> p kc n", p=128)
        wob_v = wob_d.rearrange("(kc p) n -> p kc n", p=128)
        mbc = sb("mbc", [128, 3 * D], F32)
        kb.dma("sp", lambda q: q.dma_start(out=mbc[:, :], in_=modbc_d[b, :, 0:3 * D]), reads=[], writes=[mbc.r()])
        ya_v = ya_d[b].rearrange("(kc p) t -> p kc t", p=128)
        yb_v = yb_d[b].rearrange("(kc p) t -> p kc t", p=128)
        ym_v = ym_d[b].rearrange("(kc p) t -> p kc t", p=128)
        with scope():
            wmm = [sb("wmm%d" % i, [128, KC, 4, 128], BF16) for i in range(2)]
            ya_sb = [sb("ya_sb%d" % i, [128, KC, 512], BF16) for i in range(2)]
            yb_sb = [sb("yb_sb%d" % i, [128, KC, 512], BF16) for i in range(2)]
            ga = sb("ga", [128, 512], F32)
            gb = sb("gb", [128, 512], F32)
            t1 = sb("t1", [128, 512], F32)
            t2 = sb("t2", [128, 512], F32)
            ymc = [sb("ymc%d" % i, [128, 512], BF16) for i in range(2)]
            it = 0
            for m in range(KC):
                wm_ = wmm[m % 2]
                kb.dma("pool", lambda q, m=m, wm_=wm_: q.dma_start(out=wm_[:, :, 0, :], in_=win_v[:, :, OFF_GATE + m * 128:OFF_GATE + (m + 1) * 128]), writes=[wm_.r()])
                kb.dma("pool", lambda q, m=m, wm_=wm_: q.dma_start(out=wm_[:, :, 1, :], in_=win_v[:, :, OFF_GATE + D + m * 128:OFF_GATE + D + (m + 1) * 128]), writes=[wm_.r()])
                kb.dma("pool", lambda q, m=m, wm_=wm_: q.dma_start(out=wm_[:, :, 2, :], in_=woa_v[:, :, m * 128:(m + 1) * 128]), writes=[wm_.r()])
                kb.dma("pool", lambda q, m=m, wm_=wm_: q.dma_start(out=wm_[:, :, 3, :], in_=wob_v[:, :, m * 128:(m + 1) * 128]), writes=[wm_.r()])
                for (t0, n) in LTB:
                    ya_, yb_, ymc_ = ya_sb[it % 2], yb_sb[it % 2], ymc[it % 2]
                    it += 1
                    kb.dma("sp", lambda q, t0=t0, n=n, ya_=ya_: q.dma_start(out=ya_[:, :, 0:n], in_=ya_v[:, :, t0:t0 + n]), reads=[yaR], writes=[ya_.r()])
                    kb.dma("sp", lambda q, t0=t0, n=n, yb_=yb_: q.dma_start(out=yb_[:, :, 0:n], in_=yb_v[:, :, t0:t0 + n]), reads=[ybR], writes=[yb_.r()])
                    hr = hT_reads(CTX + t0, n)
                    pa, pb, pga, pgb = PS[4], PS[5], PS[6], PS[7]
                    for kc in range(KC):
                        mm(pga[:, 0:n], wm_[:, kc, 0, :], hT[:, kc, CTX + t0:CTX + t0 + n], kc == 0, kc == KC - 1, hr + [wm_.r()], [pga.r()])
                    for kc in range(KC):
                        mm(pgb[:, 0:n], wm_[:, kc, 1, :], hT[:, kc, CTX + t0:CTX + t0 + n], kc == 0, kc == KC - 1, hr + [wm_.r()], [pgb.r()])
                    for kc in range(KC):
                        mm(pa[:, 0:n], wm_[:, kc, 2, :], ya_[:, kc, 0:n], kc == 0, kc == KC - 1, [wm_.r(), ya_.r()], [pa.r()])
                    for kc in range(KC):
                        mm(pb[:, 0:n], wm_[:, kc, 3, :], yb_[:, kc, 0:n], kc == 0, kc == KC - 1, [wm_.r(), yb_.r()], [pb.r()])
                    kb.op("act", lambda e, n=n, pga=pga: e.activation(out=ga[:, 0:n], in_=pga[:, 0:n], func=AF.Sigmoid), reads=[pga.r()], writes=[ga.r()])
                    kb.op("act", lambda e, n=n, pgb=pgb: e.activation(out=gb[:, 0:n], in_=pgb[:, 0:n], func=AF.Sigmoid), reads=[pgb.r()], writes=[gb.r()])
                    kb.op("dve", lambda e, n=n, pa=pa: e.tensor_tensor(out=t1[:, 0:n], in0=pa[:, 0:n], in1=ga[:, 0:n], op=ALU.mult), reads=[pa.r(), ga.r()], writes=[t1.r()])
                    kb.op("dve", lambda e, n=n, pb=pb: e.tensor_tensor(out=t2[:, 0:n], in0=pb[:, 0:n], in1=gb[:, 0:n], op=ALU.mult), reads=[pb.r(), gb.r()], writes=[t2.r()])
                    kb.op("pool", lambda e, n=n, ymc_=ymc_: e.tensor_tensor(out=ymc_[:, 0:n], in0=t1[:, 0:n], in1=t2[:, 0:n], op=ALU.add), reads=[t1.r(), t2.r()], writes=[ymc_.r()])
                    kb.dma("sp", lambda q, m=m, t0=t0, n=n, ymc_=ymc_: q.dma_start(out=ym_d[b, m * 128:(m + 1) * 128, t0:t0 + n], in_=ymc_[:, 0:n]), reads=[ymc_.r()], writes=[ymR])
        ymT2 = [sb("ymT%d" % i, [128, KC, 512], BF16) for i in range(2)]
        xr = sb("xr", [128, D], F32)
        x1t = sb("x1t", [128, D], F32)
        h2f = sb("h2f", [128, D], F32)
        h2b = sb("h2b", [128, D], BF16)
        h2T = sb("h2T", [128, D], F32)
        jk = sb("jk", [128, D], BF16)
        st = sb("st", [128, 4], F32)
        for bi, (t0, n) in enumerate(LTB):
            ymT = ymT2[bi % 2]
            kb.dma("sp", lambda q, t0=t0, n=n, ymT=ymT: q.dma_start(out=ymT[:, :, 0:n], in_=ym_v[:, :, t0:t0 + n]), reads=[ymR], writes=[ymT.r()])
            for tt in range(n // 128):
                tok0 = t0 + tt * 128
                gt = b * SEQ + tok0
                tile_i = gt // 128
                kb.dma("sp", lambda q, tok0=tok0: q.dma_start(out=xr[:, :], in_=x_d[b, tok0:tok0 + 128, :]), writes=[xr.r()])
                for half in range(2):
                    ps = PS[2 + half]
                    for kc in range(KC):
                        mm(ps[:, :], ymT[:, kc, tt * 128:(tt + 1) * 128], wo[:, kc, half * 512:(half + 1) * 512], kc == 0, kc == KC - 1, [ymT.r(), wo.r()], [ps.r()])
                    kb.op("dve", lambda e, ps=ps, half=half: e.tensor_tensor(out=x1t[:, half * 512:(half + 1) * 512], in0=ps[:, :], in1=mbc[:, half * 512:(half + 1) * 512], op=ALU.mult),
                          reads=[ps.r(), mbc.r()], writes=[x1t.r()])
                kb.op("pool", lambda e: e.tensor_tensor(out=x1t[:, :], in0=x1t[:, :], in1=xr[:, :], op=ALU.add), reads=[x1t.r(), xr.r()], writes=[x1t.r()])
                kb.dma("sp", lambda q, gt=gt: q.dma_start(out=x1_d[gt:gt + 128, :], in_=x1t[:, :]), reads=[x1t.r()], writes=[x1R])
                kb.op("act", lambda e: e.activation(out=jk[:, :], in_=x1t[:, :], func=AF.Square, accum_out=st[:, 0:1]), reads=[x1t.r()], writes=[jk.r(), st.r()])
                kb.op("act", lambda e: e.activation(out=st[:, 1:2], in_=st[:, 0:1], func=AF.Sqrt, scale=1.0 / D, bias=EPS), reads=[st.r()], writes=[st.r()])
                kb.op("dve", lambda e: e.reciprocal(out=st[:, 2:3], in_=st[:, 1:2]), reads=[st.r()], writes=[st.r()])
                kb.op("dve", lambda e: e.scalar_tensor_tensor(out=h2f[:, :], in0=x1t[:, :], scalar=st[:, 2:3], in1=mbc[:, 2 * D:3 * D], op0=ALU.mult, op1=ALU.mult),
                      reads=[x1t.r(), st.r(), mbc.r()], writes=[h2f.r()])
                kb.op("pool", lambda e: e.tensor_tensor(out=h2f[:, :], in0=h2f[:, :], in1=mbc[:, D:2 * D], op=ALU.add), reads=[h2f.r(), mbc.r()], writes=[h2f.r()])
                kb.op("act", lambda e: e.activation(out=h2b[:, :], in_=h2f[:, :], func=AF.Copy), reads=[h2f.r()], writes=[h2b.r()])
                kb.dma("sp", lambda q, gt=gt: q.dma_start(out=h2_d[gt:gt + 128, :], in_=h2b[:, :]), reads=[h2b.r()], writes=[h2R])
                for half in range(2):
                    ps = PS[half]
                    for j in range(4):
                        kc = half * 4 + j
                        kb.op("pe", lambda e, ps=ps, j=j, kc=kc: e.transpose(ps[:, j * 128:(j + 1) * 128], h2f[:, kc * 128:(kc + 1) * 128], C("ident")),
                              reads=[h2f.r()] + CR, writes=[ps.r()], inc=(j == 3))
                    kb.op("act" if half == 0 else "dve", (lambda e, ps=ps, half=half: e.activation(out=h2T[:, half * 512:(half + 1) * 512], in_=ps[:, :], func=AF.Copy)) if half == 0 else
                          (lambda e, ps=ps, half=half: e.tensor_copy(out=h2T[:, half * 512:(half + 1) * 512], in_=ps[:, :])), reads=[ps.r()], writes=[h2T.r()])
                ps = PS[2]
                for kc in range(KC):
                    mm(ps[:, 0:N_EXP], h2T[:, kc * 128:(kc + 1) * 128], wr_sb[:, kc, :], kc == 0, kc == KC - 1, [h2T.r(), wr_sb.r()], [ps.r()])
                kb.op("dve", lambda e, ps=ps, tile_i=tile_i: e.tensor_tensor(out=lg_all[:, tile_i, :], in0=ps[:, 0:N_EXP], in1=br_bc[:, :], op=ALU.add), reads=[ps.r(), br_bc.r()], writes=[lg_all.r(tile_i)])

    seq_scope = scope()
    seq_scope.__enter__()
    hT = sb("hT", [128, KC, S], BF16)
    Gtok = sb("Gtok", [128, NT, 32], F32)
    for b in range(NB):
        with scope():
            stage1(b)
        if bis == 7:
            return early()
        stage2_small(b)
        if bis == 8:
            return early()
        if b == 0:
            dbg_out("hT", hT[:, 0, :], [hT.r((0, t)) for t in range(NT)])
            dbg_out("Gtok", Gtok[:, :, :], [Gtok.r(t) for t in range(NT)])
        if stop <= 1:
            continue
        with scope():
            gdn_all(b)
        if stop <= 2:
            continue
        with scope():
            gla_all(b)
        if stop <= 3:
            continue
        with scope():
            merge_seq(b)
    seq_scope.__exit__()
    dbg_out("lg", lg_all[:, :, :], [lg_all.r(i) for i in range(TT_)])
    if "x1" in dbg_d:
        kb.out_tokens.append(kb.dma("sp", lambda q: q.dma_start(out=dbg_d["x1"], in_=x1_d), reads=[x1R]))
    if stop <= 4:
        return finish()
    NMC = 128 + 8 + NBLK
    mc = sb("mc", [128, NMC], F32)
    kb.dma("sp", lambda q: q.dma_start(out=mc[:, :], in_=moec_d), writes=[mc.r()])
    ustr = sb("ustr", [128, 128], BF16)
    kb.op("dve", lambda e: e.tensor_copy(out=ustr[:, :], in_=mc[:, 0:128]), reads=[mc.r()], writes=[ustr.r()])
    tokid = sb("tokid", [128, TT_], I32)
    kb.dma("sp", lambda q: q.dma_start(out=tokid[:, :], in_=tokid_d), writes=[tokid.r()])
    dsl_i = sb("dsl_i", [128, TT_ * 4], I32)
    be_bc = sb("be_bc", [128, NBLK], F32)
    be_i = sb("be_i", [128, NBLK], I32)
    scA = scope()
    scA.__enter__()
    sel_all = sb("sel_all", [128, TT_, N_EXP], F32)
    P_all = sb("P_all", [128, TT_, N_EXP], F32)
    dest_all = sb("dest_all", [128, TT_, N_EXP], F32)
    top8 = sb("top8", [128, TT_, 8], F32)
    dsl = sb("dsl", [128, TT_ * 4], F32)
    pk = sb("pk", [128, TT_ * 4], F32)
    run = sb("run", [128, N_EXP], F32)
    t32 = sb("t32", [128, N_EXP], F32)
    selb = sb("selb", [128, N_EXP], BF16)
    den = sb("den", [128, 2], F32)
    kb.op("dve", lambda e: e.memset(run[:, :], 0.0), writes=[run.r()])
    for ti in range(TT_):
        lg = lg_all[:, ti, :]
        rl = [lg_all.r(ti)]
        kb.op("dve", lambda e, ti=ti, lg=lg: e.max(out=top8[:, ti, :], in_=lg), reads=rl, writes=[top8.r(ti)])
        kb.op("dve", lambda e, ti=ti, lg=lg: e.tensor_scalar(out=sel_all[:, ti, :], in0=lg, scalar1=top8[:, ti, 3:4], scalar2=None, op0=ALU.is_ge), reads=rl + [top8.r(ti)], writes=[sel_all.r(ti)])
        kb.op("dve", lambda e, ti=ti, lg=lg: e.tensor_scalar(out=t32[:, :], in0=lg, scalar1=top8[:, ti, 0:1], scalar2=None, op0=ALU.subtract), reads=rl + [top8.r(ti)], writes=[t32.r()])
        kb.op("act", lambda e: e.activation(out=t32[:, :], in_=t32[:, :], func=AF.Exp), reads=[t32.r()], writes=[t32.r()])
        kb.op("dve", lambda e, ti=ti: e.tensor_tensor(out=t32[:, :], in0=t32[:, :], in1=sel_all[:, ti, :], op=ALU.mult), reads=[t32.r(), sel_all.r(ti)], writes=[t32.r()])
        kb.op("dve", lambda e: e.reduce_sum(out=den[:, 0:1], in_=t32[:, :], axis=AX.X), reads=[t32.r()], writes=[den.r()])
        kb.op("dve", lambda e: e.reciprocal(out=den[:, 1:2], in_=den[:, 0:1]), reads=[den.r()], writes=[den.r()])
        kb.op("dve", lambda e, ti=ti: e.tensor_scalar(out=P_all[:, ti, :], in0=t32[:, :], scalar1=den[:, 1:2], scalar2=None, op0=ALU.mult), reads=[t32.r(), den.r()], writes=[P_all.r(ti)])
        kb.op("act", lambda e, ti=ti: e.activation(out=selb[:, :], in_=sel_all[:, ti, :], func=AF.Copy), reads=[sel_all.r(ti)], writes=[selb.r()])
        ps = PS[ti % 2]
        mm(ps[:, 0:32], ustr[:, :], selb[:, :], True, True, [ustr.r(), selb.r()], [ps.r()])
        mm(ps[:, 32:64], C("ones", bf=True), selb[:, :], True, True, [selb.r()] + CRB, [ps.r()])
        kb.op("dve", lambda e, ti=ti, ps=ps: e.tensor_tensor(out=dest_all[:, ti, :], in0=ps[:, 0:32], in1=run[:, :], op=ALU.add), reads=[ps.r(), run.r()], writes=[dest_all.r(ti)])
        kb.op("dve", lambda e, ps=ps: e.tensor_tensor(out=run[:, :], in0=ps[:, 32:64], in1=run[:, :], op=ALU.add), reads=[ps.r(), run.r()], writes=[run.r()])
    padded = sb("padded", [128, N_EXP], F32)
    pstart = sb("pstart", [128, N_EXP], F32)
    pend = sb("pend", [128, N_EXP], F32)
    padT = sb("padT", [32, 128], F32)
    pendT = sb("pendT", [32, 128], F32)
    cmpb = sb("cmpb", [32, NBLK], F32)
    kb.op("dve", lambda e: e.tensor_scalar(out=padded[:, :], in0=run[:, :], scalar1=0.0, scalar2=float(BLK), op0=ALU.is_gt, op1=ALU.mult), reads=[run.r()], writes=[padded.r()])
    for j_ in range(1, -(-T // BLK)):
        kb.op("dve", lambda e, j_=j_: e.tensor_scalar(out=t32[:, :], in0=run[:, :], scalar1=float(j_ * BLK), scalar2=float(BLK), op0=ALU.is_gt, op1=ALU.mult), reads=[run.r()], writes=[t32.r()])
        kb.op("dve", lambda e: e.tensor_tensor(out=padded[:, :], in0=padded[:, :], in1=t32[:, :], op=ALU.add), reads=[padded.r(), t32.r()], writes=[padded.r()])
    ps = PS[0]
    kb.op("pe", lambda e: e.transpose(ps[0:32, 0:128], padded[:, :], C("ident")), reads=[padded.r()] + CR, writes=[ps.r()])
    kb.op("dve", lambda e: e.tensor_copy(out=padT[:, :], in_=ps[0:32, 0:128]), reads=[ps.r()], writes=[padT.r()])
    mm(ps[:, 0:32], padT[0:32, :], C("tri_f", rows=slice(0, 32), cols=slice(0, 32)), True, True, [padT.r()] + CR, [ps.r()])
    kb.op("dve", lambda e: e.tensor_copy(out=pend[:, :], in_=ps[:, 0:32]), reads=[ps.r()], writes=[pend.r()])
    kb.op("dve", lambda e: e.tensor_tensor(out=pstart[:, :], in0=pend[:, :], in1=padded[:, :], op=ALU.subtract), reads=[pend.r(), padded.r()], writes=[pstart.r()])
    kb.op("pe", lambda e: e.transpose(ps[0:32, 0:128], pend[:, :], C("ident")), reads=[pend.r()] + CR, writes=[ps.r()])
    kb.op("dve", lambda e: e.tensor_copy(out=pendT[:, :], in_=ps[0:32, 0:128]), reads=[ps.r()], writes=[pendT.r()])
    kb.op("dve", lambda e: e.tensor_scalar(out=cmpb[:, :], in0=mc[0:32, 136:136 + NBLK], scalar1=pendT[0:32, 0:1], scalar2=None, op0=ALU.is_ge), reads=[mc.r(), pendT.r()], writes=[cmpb.r()])
    mm(ps[:, 0:NBLK], C("ones", rows=slice(0, 32)), cmpb[0:32, :], True, True, [cmpb.r()] + CR, [ps.r()])
    kb.op("dve", lambda e: e.tensor_scalar(out=be_bc[:, :], in0=ps[:, 0:NBLK], scalar1=float(N_EXP - 1), scalar2=None, op0=ALU.min), reads=[ps.r()], writes=[be_bc.r()])
    kb.op("dve", lambda e: e.tensor_copy(out=be_i[:, :], in_=be_bc[:, :]), reads=[be_bc.r()], writes=[be_i.r()])
    kb.op("dve", lambda e: e.tensor_scalar(out=be_bc[:, :], in0=be_bc[:, :], scalar1=float(D), scalar2=None, op0=ALU.mult), reads=[be_bc.r(), be_i.r()], writes=[be_bc.r()])
    for ti in range(TT_):
        kb.op("dve", lambda e, ti=ti: e.tensor_tensor(out=dest_all[:, ti, :], in0=dest_all[:, ti, :], in1=pstart[:, :], op=ALU.add), reads=[dest_all.r(ti), pstart.r()], writes=[dest_all.r(ti)])
        for k in range(TOPK):
            kb.op("dve", lambda e, ti=ti, k=k: e.scalar_tensor_tensor(out=t32[:, :], in0=lg_all[:, ti, :], scalar=top8[:, ti, k:k + 1], in1=dest_all[:, ti, :], op0=ALU.is_equal, op1=ALU.mult,
                                                                      accum_out=dsl[:, ti * 4 + k:ti * 4 + k + 1]), reads=[lg_all.r(ti), top8.r(ti), dest_all.r(ti)], writes=[t32.r(), dsl.r()])
            kb.op("dve", lambda e, ti=ti, k=k: e.scalar_tensor_tensor(out=t32[:, :], in0=lg_all[:, ti, :], scalar=top8[:, ti, k:k + 1], in1=P_all[:, ti, :], op0=ALU.is_equal, op1=ALU.mult,
                                                                      accum_out=pk[:, ti * 4 + k:ti * 4 + k + 1]), reads=[lg_all.r(ti), top8.r(ti), P_all.r(ti)], writes=[t32.r(), pk.r()])
    kb.op("dve", lambda e: e.tensor_copy(out=dsl_i[:, :], in_=dsl[:, :]), reads=[dsl.r()], writes=[dsl_i.r()])
    stok_d = nc.dram_tensor("stok_s", [NSLOT, 1], I32, kind="Internal").ap()
    sp_d = nc.dram_tensor("sp_s", [NSLOT, 1], F32, kind="Internal").ap()
    ini_i = sb("ini_i", [128, NSLOT // 128], I32)
    ini_f = sb("ini_f", [128, NSLOT // 128], F32)
    zrow = sb("zrow", [1, D], BF16)
    kb.op("dve", lambda e: e.memset(ini_i[:, :], T), writes=[ini_i.r()])
    kb.op("dve", lambda e: e.memset(ini_f[:, :], 0.0), writes=[ini_f.r()])
    kb.op("dve", lambda e: e.memset(zrow[:, :], 0.0), writes=[zrow.r()])
    kb.dma("sp", lambda q: q.dma_start(out=stok_d.rearrange("(p f) o -> p (f o)", p=128), in_=ini_i[:, :]), reads=[ini_i.r()], writes=[siR])
    kb.dma("sp", lambda q: q.dma_start(out=sp_d.rearrange("(p f) o -> p (f o)", p=128), in_=ini_f[:, :]), reads=[ini_f.r()], writes=[siR])
    kb.dma("sp", lambda q: q.dma_start(out=h2_d[T:T + 1, :], in_=zrow[0:1, :]), reads=[zrow.r()], writes=[h2R])
    IOA = bass.IndirectOffsetOnAxis
    for ti in range(TT_):
        for k in range(TOPK):
            c_ = ti * 4 + k
            kb.dma("pool", lambda q, ti=ti, c_=c_: q.indirect_dma_start(out=stok_d[:, :], out_offset=IOA(ap=dsl_i[:, c_:c_ + 1], axis=0), in_=tokid[:, ti:ti + 1], in_offset=None),
                   reads=[dsl_i.r(), tokid.r(), siR], writes=[siR])
            kb.dma("pool", lambda q, c_=c_: q.indirect_dma_start(out=sp_d[:, :], out_offset=IOA(ap=dsl_i[:, c_:c_ + 1], axis=0), in_=pk[:, c_:c_ + 1], in_offset=None),
                   reads=[dsl_i.r(), pk.r(), siR], writes=[siR])

    scA.__exit__()
    scB = scope()
    scB.__enter__()
    wgu_sb = [sb("wgu%d" % i, [128, KC, 2 * D], BF16) for i in range(2)]
    wdn_sb = [sb("wdn%d" % i, [128, KC, D], BF16) for i in range(2)]
    idxf = sb("idxf", [128, KC], F32)
    idxw = [sb("idxw%d" % i, [128, KC], I32) for i in range(2)]
    bgrow = [sb("bgrow", [2, 2 * D], F32)] * 2
    bdrow = [sb("bdrow", [2, D], F32)] * 2
    bcol = sb("bcol", [128, 16], F32)
    bdn_bc = sb("bdn_bc", [128, D], F32)
    si_t = [sb("si_t%d" % i, [128, 1], I32) for i in range(4)]
    sp_t = [sb("sp_t%d" % i, [128, 1], F32) for i in range(4)]
    xb = [sb("xb%d" % i, [128, D], BF16) for i in range(2)]
    xbT = sb("xbT", [128, KC, BLK], BF16)
    actT = sb("actT", [128, KC, BLK], BF16)
    gg_ = [sb("gg%d" % i_, [128, BLK], F32) for i_ in range(2)]
    ll_ = [sb("ll%d" % i_, [128, BLK], F32) for i_ in range(2)]
    sg_ = [sb("sg%d" % i_, [128, BLK], F32) for i_ in range(2)]
    yv = [sb("yv%d" % i, [128, D], BF16) for i in range(2)]
    tdn = sb("tdn", [128, 512], F32)

    def load_weights(blk):
        i = blk % 2
        kb.op("dve", lambda e: e.tensor_scalar(out=idxf[:, :], in0=mc[:, 128:136], scalar1=be_bc[:, blk:blk + 1], scalar2=None, op0=ALU.add), reads=[mc.r(), be_bc.r()], writes=[idxf.r()])
        kb.op("dve", lambda e: e.tensor_copy(out=idxw[i][:, :], in_=idxf[:, :]), reads=[idxf.r()], writes=[idxw[i].r()])
        for kc in range(KC):
            kb.dma("pool", lambda q, kc=kc: q.indirect_dma_start(out=wgu_sb[i][:, kc, :], out_offset=None, in_=wgu_d[:, :], in_offset=IOA(ap=idxw[i][:, kc:kc + 1], axis=0)),
                   reads=[idxw[i].r()], writes=[wgu_sb[i].r()])
            kb.dma("pool", lambda q, kc=kc: q.indirect_dma_start(out=wdn_sb[i][:, kc, :], out_offset=None, in_=wdn_d[:, :], in_offset=IOA(ap=idxw[i][:, kc:kc + 1], axis=0)),
                   reads=[idxw[i].r()], writes=[wdn_sb[i].r()])
        kb.dma("pool", lambda q: q.indirect_dma_start(out=bgrow[i][0:2, :], out_offset=None, in_=bgu_d[:, :], in_offset=IOA(ap=be_i[0:2, blk:blk + 1], axis=0)),
               reads=[be_i.r()], writes=[bgrow[i].r()])
        kb.dma("pool", lambda q: q.indirect_dma_start(out=bdrow[i][0:2, :], out_offset=None, in_=bdn_d[:, :], in_offset=IOA(ap=be_i[0:2, blk:blk + 1], axis=0)),
               reads=[be_i.r()], writes=[bdrow[i].r()])

    load_weights(0)
    for blk in range(NBLK):
        i = blk % 2
        row_to_cols(lambda j: bgrow[i][0:1, j * 128:(j + 1) * 128], 16, PS[0], bcol[:, :], [bgrow[i].r()], [bcol.r()])
        kb.op("dve", lambda e: e.tensor_scalar(out=bcol[:, 8:16], in0=bcol[:, 8:16], scalar1=1.0, scalar2=None, op0=ALU.add), reads=[bcol.r()], writes=[bcol.r()])
        for half in range(2):
            row_bcast(bdrow[i][0:1, half * 512:(half + 1) * 512], 512, PS[1], bdn_bc[:, half * 512:(half + 1) * 512], [bdrow[i].r()], [bdn_bc.r()])
        for s_ in range(4):
            slot0 = blk * BLK + s_ * 128
            kb.dma("sp", lambda q, s_=s_, slot0=slot0: q.dma_start(out=si_t[s_][:, :], in_=stok_d[slot0:slot0 + 128, :]), reads=[siR], writes=[si_t[s_].r()])
            kb.dma("sp", lambda q, s_=s_, slot0=slot0: q.dma_start(out=sp_t[s_][:, :], in_=sp_d[slot0:slot0 + 128, :]), reads=[siR], writes=[sp_t[s_].r()])
            xb_ = xb[s_ % 2]
            kb.dma("pool", lambda q, s_=s_, xb_=xb_: q.indirect_dma_start(out=xb_[:, :], out_offset=None, in_=h2_d[:, :], in_offset=IOA(ap=si_t[s_][:, 0:1], axis=0)),
                   reads=[si_t[s_].r(), h2R], writes=[xb_.r()])
            ps = PS[2 + (s_ % 2)]
            psb = ps[:, :].bitcast(BF16)
            for kc in range(KC):
                kb.op("pe", lambda e, kc=kc, xb_=xb_, psb=psb: e.transpose(psb[:, kc * 128:(kc + 1) * 128], xb_[:, kc * 128:(kc + 1) * 128], C("ident", bf=True)),
                      reads=[xb_.r()] + CRB, writes=[ps.r()], inc=(kc == KC - 1))
            kb.op("dve", lambda e, s_=s_, psb=psb: e.tensor_copy(out=xbT[:, :, s_ * 128:(s_ + 1) * 128], in_=psb[:, 0:1024].rearrange("p (k t) -> p k t", t=128)),
                  reads=[ps.r()], writes=[xbT.r()])
        if blk + 1 < NBLK:
            load_weights(blk + 1)
        for j in range(KC):
            pg, pl = PS[4 + 2 * (j % 2)], PS[5 + 2 * (j % 2)]
            gg, ll, sg = gg_[j % 2], ll_[j % 2], sg_[j % 2]
            for kc in range(KC):
                mm(pg[:, :], wgu_sb[i][:, kc, j * 128:(j + 1) * 128], xbT[:, kc, :], kc == 0, kc == KC - 1, [wgu_sb[i].r(), xbT.r()], [pg.r()])
            for kc in range(KC):
                mm(pl[:, :], wgu_sb[i][:, kc, D + j * 128:D + (j + 1) * 128], xbT[:, kc, :], kc == 0, kc == KC - 1, [wgu_sb[i].r(), xbT.r()], [pl.r()])
            kb.op("dve", lambda e, j=j, pg=pg: e.tensor_scalar(out=gg[:, :], in0=pg[:, :], scalar1=bcol[:, j:j + 1], scalar2=LIMIT, op0=ALU.add, op1=ALU.min), reads=[pg.r(), bcol.r()], writes=[gg.r()])
            kb.op("dve", lambda e, j=j, pl=pl: e.tensor_scalar(out=ll[:, :], in0=pl[:, :], scalar1=bcol[:, 8 + j:9 + j], scalar2=LIMIT + 1.0, op0=ALU.add, op1=ALU.min), reads=[pl.r(), bcol.r()], writes=[ll.r()])
            kb.op("act", lambda e: e.activation(out=sg[:, :], in_=gg[:, :], func=AF.Sigmoid, scale=ALPHA), reads=[gg.r()], writes=[sg.r()])
            kb.op("dve", lambda e: e.tensor_tensor(out=gg[:, :], in0=gg[:, :], in1=sg[:, :], op=ALU.mult), reads=[gg.r(), sg.r()], writes=[gg.r()])
            kb.op("dve", lambda e, j=j: e.scalar_tensor_tensor(out=actT[:, j, :], in0=ll[:, :], scalar=-(LIMIT - 1.0), in1=gg[:, :], op0=ALU.max, op1=ALU.mult), reads=[gg.r(), ll.r()], writes=[actT.r()])
        for s_ in range(4):
            slot0 = blk * BLK + s_ * 128
            yv_ = yv[s_ % 2]
            for half in range(2):
                ps = PS[2 + half]
                for kc in range(KC):
                    mm(ps[:, :], actT[:, kc, s_ * 128:(s_ + 1) * 128], wdn_sb[i][:, kc, half * 512:(half + 1) * 512], kc == 0, kc == KC - 1, [actT.r(), wdn_sb[i].r()], [ps.r()])
                kb.op("dve", lambda e, ps=ps, half=half: e.tensor_tensor(out=tdn[:, :], in0=ps[:, :], in1=bdn_bc[:, half * 512:(half + 1) * 512], op=ALU.add), reads=[ps.r(), bdn_bc.r()], writes=[tdn.r()])
                kb.op("act", lambda e, half=half, yv_=yv_, s_=s_: e.activation(out=yv_[:, half * 512:(half + 1) * 512], in_=tdn[:, :], func=AF.Copy, scale=sp_t[s_][:, 0:1]), reads=[tdn.r(), sp_t[s_].r()], writes=[yv_.r()])
            kb.dma("sp", lambda q, slot0=slot0, yv_=yv_: q.dma_start(out=ys_d[slot0:slot0 + 128, :], in_=yv_[:, :]), reads=[yv_.r()], writes=[ysR])

    scB.__exit__()
    g2bc = [sb("g2bc%d" % b, [128, D], F32) for b in range(NB)]
    for b in range(NB):
        kb.dma("sp", lambda q, b=b: q.dma_start(out=g2bc[b][:, :], in_=modbc_d[b, :, 3 * D:4 * D]), writes=[g2bc[b].r()])
    yk = [sb("yk%d" % k, [128, D], BF16) for k in range(TOPK)]
    acc1 = sb("acc1", [128, D], F32)
    acc2 = sb("acc2", [128, D], F32)
    x1r = sb("x1r", [128, D], F32)
    ot = sb("ot", [128, D], F32)
    jk2 = sb("jk2", [128, D], BF16)
    st2 = sb("st2", [128, 4], F32)
    for ti in range(TT_):
        b = (ti * 128) // SEQ
        tok0 = ti * 128 - b * SEQ
        for k in range(TOPK):
            c_ = ti * 4 + k
            kb.dma("pool", lambda q, k=k, c_=c_: q.indirect_dma_start(out=yk[k][:, :], out_offset=None, in_=ys_d[:, :], in_offset=IOA(ap=dsl_i[:, c_:c_ + 1], axis=0)),
                   reads=[dsl_i.r(), ysR], writes=[yk[k].r()])
        kb.dma("sp", lambda q, ti=ti: q.dma_start(out=x1r[:, :], in_=x1_d[ti * 128:(ti + 1) * 128, :]), reads=[x1R], writes=[x1r.r()])
        kb.op("dve", lambda e: e.tensor_tensor(out=acc1[:, :], in0=yk[0][:, :], in1=yk[1][:, :], op=ALU.add), reads=[yk[0].r(), yk[1].r()], writes=[acc1.r()])
        kb.op("pool", lambda e: e.tensor_tensor(out=acc2[:, :], in0=yk[2][:, :], in1=yk[3][:, :], op=ALU.add), reads=[yk[2].r(), yk[3].r()], writes=[acc2.r()])
        kb.op("dve", lambda e: e.tensor_tensor(out=acc1[:, :], in0=acc1[:, :], in1=acc2[:, :], op=ALU.add), reads=[acc1.r(), acc2.r()], writes=[acc1.r()])
        kb.op("dve", lambda e, b=b: e.tensor_tensor(out=acc1[:, :], in0=acc1[:, :], in1=g2bc[b][:, :], op=ALU.mult), reads=[acc1.r(), g2bc[b].r()], writes=[acc1.r()])
        kb.op("pool", lambda e: e.tensor_tensor(out=acc1[:, :], in0=acc1[:, :], in1=x1r[:, :], op=ALU.add), reads=[acc1.r(), x1r.r()], writes=[acc1.r()])
        kb.op("act", lambda e: e.activation(out=jk2[:, :], in_=acc1[:, :], func=AF.Square, accum_out=st2[:, 0:1]), reads=[acc1.r()], writes=[jk2.r(), st2.r()])
        kb.op("act", lambda e: e.activation(out=st2[:, 1:2], in_=st2[:, 0:1], func=AF.Sqrt, scale=1.0 / D, bias=EPS), reads=[st2.r()], writes=[st2.r()])
        kb.op("dve", lambda e: e.reciprocal(out=st2[:, 2:3], in_=st2[:, 1:2]), reads=[st2.r()], writes=[st2.r()])
        kb.op("dve", lambda e: e.scalar_tensor_tensor(out=ot[:, :], in0=acc1[:, :], scalar=st2[:, 2:3], in1=fng_bc[:, :], op0=ALU.mult, op1=ALU.mult), reads=[acc1.r(), st2.r(), fng_bc.r(0), fng_bc.r(1)], writes=[ot.r()])
        kb.out_tokens.append(kb.dma("sp", lambda q, b=b, tok0=tok0: q.dma_start(out=out_d[b, tok0:tok0 + 128, :], in_=ot[:, :]), reads=[ot.r()]))
    return finish()


def _prep_inputs(inp, cores, nb):
    f = lambda a: np.ascontiguousarray(np.asarray(a, dtype=np.float32))
    consts = make_consts()
    shared = {
        "c_ctx": f(inp["c_ctx"]).reshape(1, D),
        "w_mod": f(inp["w_mod"][0]),
        "b_mod": f(inp["b_mod"][0]).reshape(1, -1),
        "norm_mix_g": f(inp["norm_mix_g"][0]).reshape(1, D),
        "norm_ffn_g": f(inp["norm_ffn_g"][0]).reshape(1, D),
        "w_in": f(inp["w_in"][0]),
        "conv_w": f(inp["conv_w"][0]).reshape(9, -1),
        "a_log": np.concatenate([f(inp["a_log_f"][0]), f(inp["a_log_b"][0])]).reshape(1, 16),
        "dt_bias": np.concatenate([f(inp["dt_bias_f"][0]), f(inp["dt_bias_b"][0])]).reshape(1, 16),
        "gdn_norm_g": f(inp["gdn_norm_g"][0]).reshape(1, 128),
        "gla_gate_w": np.stack([f(inp["gla_gate_w_f"][0]), f(inp["gla_gate_w_b"][0])]),
        "gla_gate_b": np.stack([f(inp["gla_gate_b_f"][0]), f(inp["gla_gate_b_b"][0])]),
        "gla_norm_g": f(inp["gla_norm_g"][0]).reshape(1, 256),
        "w_out_a": f(inp["w_out_a"][0]),
        "w_out_b": f(inp["w_out_b"][0]),
        "w_out": f(inp["w_out"][0]),
        "w_router": f(inp["w_router"][0]),
        "b_router": f(inp["b_router"][0]).reshape(1, N_EXP),
        "w_gu": f(inp["w_gu"][0]).reshape(N_EXP * D, 2 * D),
        "b_gu": f(inp["b_gu"][0]),
        "w_down": f(inp["w_down"][0]).reshape(N_EXP * D, D),
        "b_down": f(inp["b_down"][0]),
        "final_norm_g": f(inp["final_norm_g"]).reshape(1, D),
        "consts": np.concatenate([consts[k] for k in CONST_ORDER], axis=1),
        "sel16": consts["sel16"],
    }
    T_ = nb * inp["x"].shape[1]
    nblk = -(-(T_ * TOPK + N_EXP * 511) // 512)
    mcn = np.zeros((128, 128 + 8 + nblk), np.float32)
    ii = np.arange(128)
    mcn[:, 0:128] = (ii[:, None] < ii[None, :])
    mcn[:, 128:136] = np.arange(8)[None, :] * 128 + ii[:, None]
    mcn[:, 136:] = np.arange(nblk)[None, :] * 512
    shared["moe_c"] = mcn
    shared["tokid"] = (np.arange(T_ // 128)[None, :] * 128 + ii[:, None]).astype(np.int32)
    maps = []
    for i in range(cores):
        m = dict(shared)
        m["x"] = f(inp["x"][i * nb:(i + 1) * nb])
        m["c"] = f(inp["c"][i * nb:(i + 1) * nb])
        m["ctx"] = f(inp["ctx"][i * nb:(i + 1) * nb])
        maps.append(m)
    return maps


def kernel(**inputs):
    n = 8
    B, SEQ = inputs["x"].shape[0], inputs["x"].shape[1]
    CTX = inputs["ctx"].shape[1]
    nb = B // n
    cfg = {"NB": nb, "SEQ": SEQ, "CTX": CTX, "GW": 64}
    nc = build(cfg)
    maps = _prep_inputs(inputs, n, nb)
    res = run_bass_kernel_spmd(nc, maps, core_ids=list(range(n)))
    return np.concatenate([r["out"] for r in res.results], axis=0)
```
